# Optimizing a Trainium2 kernel written in Bass

```python
import math
import jax, jax.numpy as jnp
from jax import lax
import numpy as np

D_MODEL = 1024
BATCH = 16
SEQ = 4096
DEPTH = 2

GRID_W = 64
CTX_LEN = 256
EPS = 1e-6
ROPE_BASE = 10000.0
NEG_INF = -1e30

MLA_HEADS = 4
MLA_NOPE = 64
MLA_ROPE = 32
MLA_V = 64
MLA_Q_RANK = 256
MLA_KV_RANK = 128
MLA_SCALE = (MLA_NOPE + MLA_ROPE) ** -0.5
SWA_HEADS = 4
SWA_KV_HEADS = 2
SWA_HEAD_DIM = 64
WINDOW = 128
BLOCK = 128
LRU_WIDTH = 512
LRU_BLOCKS = 8
LRU_BW = LRU_WIDTH // LRU_BLOCKS
CONV_W = 4
CONV_LEFT = CONV_W // 2
CONV_RIGHT = CONV_W - 1 - CONV_W // 2
LRU_C = 8.0
N_GROUPS = 4
EXPERTS_PER_GROUP = 8
N_EXPERTS = N_GROUPS * EXPERTS_PER_GROUP
TOP_K = 2
D_EXPERT = 256

MLA_OUT = MLA_HEADS * MLA_V
SWA_OUT = SWA_HEADS * SWA_HEAD_DIM
MIX_WIDTH = MLA_OUT + SWA_OUT + LRU_WIDTH
IN_SIZES = (MLA_Q_RANK, MLA_KV_RANK, MLA_ROPE, SWA_HEADS * SWA_HEAD_DIM, SWA_KV_HEADS * SWA_HEAD_DIM, SWA_KV_HEADS * SWA_HEAD_DIM, LRU_WIDTH, LRU_WIDTH)
IN_COLS = sum(IN_SIZES)

kernel_name = 'hybrid_mla_swa_rglru_hmoe_dit'


def rmsnorm(x, g):
    xf = x.astype(jnp.float32)
    y = xf * lax.rsqrt(jnp.mean(xf * xf, axis=-1, keepdims=True) + EPS)
    return (y * g.astype(jnp.float32)).astype(x.dtype)


def rope_1d(x, pos):
    half = x.shape[-1] // 2
    freqs = ROPE_BASE ** (-jnp.arange(half, dtype=jnp.float32) / half)
    ang = pos.astype(jnp.float32)[:, None] * freqs[None, :]
    cos = jnp.cos(ang)[:, None, :]
    sin = jnp.sin(ang)[:, None, :]
    xf = x.astype(jnp.float32)
    x1, x2 = xf[..., :half], xf[..., half:]
    return jnp.concatenate([x1 * cos - x2 * sin, x2 * cos + x1 * sin], axis=-1).astype(x.dtype)


def rope_2d(x, rows, cols):
    d = x.shape[-1] // 2
    return jnp.concatenate([rope_1d(x[..., :d], rows), rope_1d(x[..., d:], cols)], axis=-1)


def split_cols(z):
    out = []
    o = 0
    for n in IN_SIZES:
        out.append(z[..., o:o + n])
        o += n
    return out


def mla_q(z_cq, g_cq, w_uq, rows, cols):
    B, S = z_cq.shape[:2]
    q = (rmsnorm(z_cq, g_cq) @ w_uq).reshape(B, S, MLA_HEADS, MLA_NOPE + MLA_ROPE)
    q_nope, q_rope = q[..., :MLA_NOPE], q[..., MLA_NOPE:]
    if rows is not None:
        q_rope = rope_2d(q_rope, rows, cols)
    return jnp.concatenate([q_nope, q_rope], axis=-1)


def mla_kv(z_ckv, z_kr, g_ckv, w_ukv, rows, cols):
    B, S = z_ckv.shape[:2]
    kv = (rmsnorm(z_ckv, g_ckv) @ w_ukv).reshape(B, S, MLA_HEADS, MLA_NOPE + MLA_V)
    k_nope, v = kv[..., :MLA_NOPE], kv[..., MLA_NOPE:]
    k_rope = z_kr[:, :, None, :]
    if rows is not None:
        k_rope = rope_2d(k_rope, rows, cols)
    k = jnp.concatenate([k_nope, jnp.broadcast_to(k_rope, (B, S, MLA_HEADS, MLA_ROPE))], axis=-1)
    return k, v


def dense_attention(q, k, v, scale):
    B, L, H, _ = q.shape
    s = jnp.einsum('blhd,bjhd->bhlj', q, k).astype(jnp.float32) * scale
    p = jax.nn.softmax(s, axis=-1).astype(v.dtype)
    return jnp.einsum('bhlj,bjhd->blhd', p, v).reshape(B, L, H * v.shape[-1])


def blockwise_attention(q, k, v, scale):
    B, S, H, dq = q.shape
    nb = S // BLOCK
    qb = jnp.moveaxis(q.reshape(B, nb, BLOCK, H, dq), 1, 0)

    def one(qblk):
        s = jnp.einsum('bqhd,bjhd->bhqj', qblk, k).astype(jnp.float32) * scale
        p = jax.nn.softmax(s, axis=-1).astype(v.dtype)
        return jnp.einsum('bhqj,bjhd->bqhd', p, v)

    o = lax.map(one, qb)
    return jnp.moveaxis(o, 0, 1).reshape(B, S, H * v.shape[-1])


def sink_attention(q, k, v, sink):
    B, L, Hq, hd = q.shape
    KV = k.shape[2]
    G = Hq // KV
    qg = q.reshape(B, L, KV, G, hd)
    s = jnp.einsum('blkgd,bjkd->bkglj', qg, k).astype(jnp.float32) * (hd ** -0.5)
    s_sink = jnp.broadcast_to(sink.astype(jnp.float32).reshape(KV, G)[None, :, :, None, None], s.shape[:-1] + (1,))
    p = jax.nn.softmax(jnp.concatenate([s, s_sink], axis=-1), axis=-1)[..., :-1].astype(v.dtype)
    return jnp.einsum('bkglj,bjkd->blkgd', p, v).reshape(B, L, Hq * hd)


def banded_sink_attention(q, k, v, k_ctx, v_ctx, sink):
    B, S, Hq, hd = q.shape
    KV = k.shape[2]
    G = Hq // KV
    L = k_ctx.shape[1]
    nb = S // BLOCK
    span = BLOCK + 2 * WINDOW
    scale = hd ** -0.5
    pad = ((0, 0), (WINDOW, WINDOW), (0, 0), (0, 0))
    kpad = jnp.pad(k, pad)
    vpad = jnp.pad(v, pad)
    qb = jnp.moveaxis(q.reshape(B, nb, BLOCK, KV, G, hd), 1, 0)
    sink_kg = sink.astype(jnp.float32).reshape(KV, G)

    def one(args):
        n, qblk = args
        start = n * BLOCK
        kw = lax.dynamic_slice_in_dim(kpad, start, span, axis=1)
        vw = lax.dynamic_slice_in_dim(vpad, start, span, axis=1)
        qpos = start + jnp.arange(BLOCK)
        kpos = start - WINDOW + jnp.arange(span)
        valid = (jnp.abs(qpos[:, None] - kpos[None, :]) <= WINDOW) & (kpos >= 0)[None, :] & (kpos < S)[None, :]
        s_loc = jnp.einsum('bqkgd,bjkd->bkgqj', qblk, kw).astype(jnp.float32) * scale
        s_loc = jnp.where(valid, s_loc, NEG_INF)
        s_ctx = jnp.einsum('bqkgd,bjkd->bkgqj', qblk, k_ctx).astype(jnp.float32) * scale
        s_sink = jnp.broadcast_to(sink_kg[None, :, :, None, None], s_loc.shape[:-1] + (1,))
        p = jax.nn.softmax(jnp.concatenate([s_loc, s_ctx, s_sink], axis=-1), axis=-1).astype(v.dtype)
        return (jnp.einsum('bkgqj,bjkd->bqkgd', p[..., :span], vw)
                + jnp.einsum('bkgqj,bjkd->bqkgd', p[..., span:span + L], v_ctx))

    o = lax.map(one, (jnp.arange(nb), qb))
    return jnp.moveaxis(o, 0, 1).reshape(B, S, Hq * hd)


def centred_conv(z, w, b):
    S = z.shape[1]
    zp = jnp.pad(z, ((0, 0), (CONV_LEFT, CONV_RIGHT), (0, 0)))
    y = b
    for tap in range(CONV_W):
        y = y + zp[:, tap:tap + S] * w[tap]
    return y


def linear_scan(a, b, h0, reverse):
    def comb(l, r):
        return (l[0] * r[0], r[0] * l[1] + r[1])
    A, Bc = lax.associative_scan(comb, (a, b), axis=1, reverse=reverse)
    return A * h0[:, None, :] + Bc


def rglru_direction(u, wa, ba, wx, bx, lam, h0, reverse):
    B, S, W = u.shape
    ub = u.reshape(B, S, LRU_BLOCKS, LRU_BW)
    r = jax.nn.sigmoid((jnp.einsum('bsnc,ncd->bsnd', ub, wa).reshape(B, S, W) + ba).astype(jnp.float32))
    i = jax.nn.sigmoid((jnp.einsum('bsnc,ncd->bsnd', ub, wx).reshape(B, S, W) + bx).astype(jnp.float32))
    log_a = -LRU_C * r * jax.nn.softplus(-lam.astype(jnp.float32))
    a = jnp.exp(log_a)
    bt = jnp.sqrt(-jnp.expm1(2.0 * log_a)) * (i * u.astype(jnp.float32))
    return linear_scan(a, bt, h0, reverse)


def merge_groups(o_mla, o_swa, o_lru, g):
    return jnp.concatenate([
        rmsnorm(o_mla, g[:MLA_OUT]),
        rmsnorm(o_swa, g[MLA_OUT:MLA_OUT + SWA_OUT]),
        rmsnorm(o_lru, g[MLA_OUT + SWA_OUT:]),
    ], axis=-1)


def mixing_layer(h, hc, rows, cols, w_in, g_cq, w_uq, g_ckv, w_ukv, sink, conv_w, conv_b,
                 wa, ba, wx, bx, lam, g_grp, w_out, need_ctx_out):
    B, S, _ = h.shape
    L = hc.shape[1]
    z = split_cols(h @ w_in)
    zc = split_cols(hc @ w_in)
    k_c, v_c = mla_kv(zc[1], zc[2], g_ckv, w_ukv, None, None)
    sk_c = zc[4].reshape(B, L, SWA_KV_HEADS, SWA_HEAD_DIM)
    sv_c = zc[5].reshape(B, L, SWA_KV_HEADS, SWA_HEAD_DIM)
    u_c = centred_conv(zc[6], conv_w, conv_b)
    h0 = jnp.zeros((B, LRU_WIDTH), jnp.float32)
    hf_c = rglru_direction(u_c, wa[0], ba[0], wx[0], bx[0], lam[0], h0, False)
    hb_c = rglru_direction(u_c, wa[1], ba[1], wx[1], bx[1], lam[1], h0, True)
    q = mla_q(z[0], g_cq, w_uq, rows, cols)
    k, v = mla_kv(z[1], z[2], g_ckv, w_ukv, rows, cols)
    o_mla = blockwise_attention(q, jnp.concatenate([k_c, k], axis=1), jnp.concatenate([v_c, v], axis=1), MLA_SCALE)
    sq = rope_2d(z[3].reshape(B, S, SWA_HEADS, SWA_HEAD_DIM), rows, cols)
    sk = rope_2d(z[4].reshape(B, S, SWA_KV_HEADS, SWA_HEAD_DIM), rows, cols)
    sv = z[5].reshape(B, S, SWA_KV_HEADS, SWA_HEAD_DIM)
    o_swa = banded_sink_attention(sq, sk, sv, sk_c, sv_c, sink)
    u = centred_conv(z[6], conv_w, conv_b)
    hf = rglru_direction(u, wa[0], ba[0], wx[0], bx[0], lam[0], hf_c[:, -1], False)
    hb = rglru_direction(u, wa[1], ba[1], wx[1], bx[1], lam[1], hb_c[:, 0], True)
    o_lru = (hf + hb).astype(h.dtype) * jax.nn.gelu(z[7])
    out = merge_groups(o_mla, o_swa, o_lru, g_grp) @ w_out
    if not need_ctx_out:
        return out, None
    q_c = mla_q(zc[0], g_cq, w_uq, None, None)
    oc_mla = dense_attention(q_c, k_c, v_c, MLA_SCALE)
    oc_swa = sink_attention(zc[3].reshape(B, L, SWA_HEADS, SWA_HEAD_DIM), sk_c, sv_c, sink)
    oc_lru = (hf_c + hb_c).astype(hc.dtype) * jax.nn.gelu(zc[7])
    out_c = merge_groups(oc_mla, oc_swa, oc_lru, g_grp) @ w_out
    return out, out_c


def hmoe(h, w_g1, b_g1, w_g2, b_g2, w_eg, w_eu, w_ed):
    shp = h.shape
    t = h.reshape(-1, shp[-1])
    T = t.shape[0]
    pg = jax.nn.softmax((t @ w_g1 + b_g1).astype(jnp.float32), axis=-1)
    pg_top, g_idx = lax.top_k(pg, 1)
    le = jnp.einsum('td,gde->tge', t, w_g2) + b_g2
    le_sel = le[jnp.arange(T), g_idx[:, 0]]
    pe = jax.nn.softmax(le_sel.astype(jnp.float32), axis=-1)
    pe_top, e_idx = lax.top_k(pe, TOP_K)
    w = pg_top * pe_top / jnp.sum(pe_top, axis=-1, keepdims=True)
    eid = g_idx * EXPERTS_PER_GROUP + e_idx
    combine = jnp.sum(jax.nn.one_hot(eid, N_EXPERTS, dtype=jnp.float32) * w[..., None], axis=1).astype(t.dtype)
    y = jnp.zeros_like(t)
    for e in range(N_EXPERTS):
        act = jax.nn.silu(t @ w_eg[e]) * (t @ w_eu[e])
        y = y + (combine[:, e:e + 1] * act) @ w_ed[e]
    return y.reshape(shp)


def setup_inputs(seed: int = 0) -> dict:
    key = jax.random.key(seed)
    ks = iter(jax.random.split(key, 40))
    f32 = jnp.float32

    def nrm(shape, scale):
        return jax.random.normal(next(ks), shape, f32) * scale

    def gain(shape):
        return 1.0 + nrm(shape, 0.01)

    u = jax.random.uniform(next(ks), (DEPTH, 2, LRU_WIDTH), f32, minval=0.9, maxval=0.999)
    a0 = u ** (1.0 / LRU_C)
    lru_lam = jnp.log(a0) - jnp.log1p(-a0)
    return {
        'x': nrm((BATCH, SEQ, D_MODEL), 1.0),
        'c': nrm((BATCH, D_MODEL), 1.0),
        'ctx': nrm((BATCH, CTX_LEN, D_MODEL), 1.0),
        'c_ctx': nrm((D_MODEL,), 1.0),
        'w_ada': nrm((DEPTH, D_MODEL, 6 * D_MODEL), 0.5 * D_MODEL ** -0.5),
        'b_ada': nrm((DEPTH, 6 * D_MODEL), 0.01),
        'g_norm1': gain((DEPTH, D_MODEL)),
        'g_norm2': gain((DEPTH, D_MODEL)),
        'w_in': nrm((DEPTH, D_MODEL, IN_COLS), D_MODEL ** -0.5),
        'g_cq': gain((DEPTH, MLA_Q_RANK)),
        'w_uq': nrm((DEPTH, MLA_Q_RANK, MLA_HEADS * (MLA_NOPE + MLA_ROPE)), MLA_Q_RANK ** -0.5),
        'g_ckv': gain((DEPTH, MLA_KV_RANK)),
        'w_ukv': nrm((DEPTH, MLA_KV_RANK, MLA_HEADS * (MLA_NOPE + MLA_V)), MLA_KV_RANK ** -0.5),
        'swa_sink': nrm((DEPTH, SWA_HEADS), 1.0),
        'conv_w': nrm((DEPTH, CONV_W, LRU_WIDTH), CONV_W ** -0.5),
        'conv_b': nrm((DEPTH, LRU_WIDTH), 0.01),
        'lru_wa': nrm((DEPTH, 2, LRU_BLOCKS, LRU_BW, LRU_BW), LRU_BW ** -0.5),
        'lru_ba': nrm((DEPTH, 2, LRU_WIDTH), 0.01),
        'lru_wx': nrm((DEPTH, 2, LRU_BLOCKS, LRU_BW, LRU_BW), LRU_BW ** -0.5),
        'lru_bx': nrm((DEPTH, 2, LRU_WIDTH), 0.01),
        'lru_lam': lru_lam,
        'g_grp': gain((DEPTH, MIX_WIDTH)),
        'w_out': nrm((DEPTH, MIX_WIDTH, D_MODEL), MIX_WIDTH ** -0.5),
        'w_g1': nrm((DEPTH, D_MODEL, N_GROUPS), D_MODEL ** -0.5),
        'b_g1': nrm((DEPTH, N_GROUPS), 0.01),
        'w_g2': nrm((DEPTH, N_GROUPS, D_MODEL, EXPERTS_PER_GROUP), D_MODEL ** -0.5),
        'b_g2': nrm((DEPTH, N_GROUPS, EXPERTS_PER_GROUP), 0.01),
        'w_e_gate': nrm((DEPTH, N_EXPERTS, D_MODEL, D_EXPERT), D_MODEL ** -0.5),
        'w_e_up': nrm((DEPTH, N_EXPERTS, D_MODEL, D_EXPERT), D_MODEL ** -0.5),
        'w_e_down': nrm((DEPTH, N_EXPERTS, D_EXPERT, D_MODEL), D_EXPERT ** -0.5),
        'g_final': gain((D_MODEL,)),
    }


def reference(x, c, ctx, c_ctx, w_ada, b_ada, g_norm1, g_norm2, w_in, g_cq, w_uq, g_ckv, w_ukv,
              swa_sink, conv_w, conv_b, lru_wa, lru_ba, lru_wx, lru_bx, lru_lam, g_grp, w_out,
              w_g1, b_g1, w_g2, b_g2, w_e_gate, w_e_up, w_e_down, g_final):
    S = x.shape[1]
    ROWS = S // GRID_W
    rows = jnp.repeat(jnp.arange(ROWS, dtype=jnp.int32), GRID_W)
    cols = jnp.tile(jnp.arange(GRID_W, dtype=jnp.int32), ROWS)
    xc = ctx
    for l in range(DEPTH):
        last = l == DEPTH - 1
        mod = (jax.nn.silu(c) @ w_ada[l] + b_ada[l])[:, None, :]
        mod_c = jax.nn.silu(c_ctx) @ w_ada[l] + b_ada[l]
        sh1, sc1, gt1, sh2, sc2, gt2 = jnp.split(mod, 6, axis=-1)
        sh1c, sc1c, gt1c, sh2c, sc2c, gt2c = jnp.split(mod_c, 6, axis=-1)
        h = rmsnorm(x, g_norm1[l]) * (1.0 + sc1) + sh1
        hc = rmsnorm(xc, g_norm1[l]) * (1.0 + sc1c) + sh1c
        mix, mix_c = mixing_layer(h, hc, rows, cols, w_in[l], g_cq[l], w_uq[l], g_ckv[l], w_ukv[l],
                                  swa_sink[l], conv_w[l], conv_b[l], lru_wa[l], lru_ba[l], lru_wx[l],
                                  lru_bx[l], lru_lam[l], g_grp[l], w_out[l], not last)
        x = x + gt1 * mix
        h2 = rmsnorm(x, g_norm2[l]) * (1.0 + sc2) + sh2
        x = x + gt2 * hmoe(h2, w_g1[l], b_g1[l], w_g2[l], b_g2[l], w_e_gate[l], w_e_up[l], w_e_down[l])
        if not last:
            xc = xc + gt1c * mix_c
            h2c = rmsnorm(xc, g_norm2[l]) * (1.0 + sc2c) + sh2c
            xc = xc + gt2c * hmoe(h2c, w_g1[l], b_g1[l], w_g2[l], b_g2[l], w_e_gate[l], w_e_up[l], w_e_down[l])
    return rmsnorm(x, g_final)
```

```python
import numpy as np
from contextlib import ExitStack
import concourse.bass as bass
import concourse.mybir as mybir
from concourse.bass_utils import run_bass_kernel_spmd

F32 = mybir.dt.float32
BF16 = mybir.dt.bfloat16
AF = mybir.ActivationFunctionType
ALU = mybir.AluOpType
AX = mybir.AxisListType

D = 1024
KD = 8
LCTX = 256
EPS = 1e-6
NEXP = 32
DEXP = 256
MLA_SCALE = 96 ** -0.5
SWA_SCALE = 0.125

COMPUTE = ("pe", "act", "dve", "pool")
DMAQ = ("sp", "pool")
NDMASEM = 12


class Buf:
    __slots__ = ("w", "r", "name", "excl")

    def __init__(self, name="", excl=False):
        self.w = None
        self.r = {}
        self.name = name
        self.excl = excl


class V:
    __slots__ = ("ap", "bufs")

    def __init__(self, ap, bufs):
        self.ap = ap
        self.bufs = bufs

    def __getitem__(self, idx):
        return V(self.ap[idx], self.bufs)

    def re(self, pat, **kw):
        return V(self.ap.rearrange(pat, **kw), self.bufs)

    def bcast(self, axis, shape):
        return V(self.ap.unsqueeze(axis).broadcast_to(list(shape)), self.bufs)

    def sub(self, buf):
        return V(self.ap, [buf])


class Op:
    __slots__ = ("eng", "fn", "deps", "sig", "cnt", "dma", "dsem", "dtgt", "idx")

    def __init__(self, eng, fn, dma):
        self.eng = eng
        self.fn = fn
        self.dma = dma
        self.deps = None
        self.sig = False
        self.cnt = 0
        self.dsem = None
        self.dtgt = 0


class Sched:
    def __init__(self, nc, es):
        self.nc = nc
        self.es = es
        self.ops = {e: [] for e in ("pe", "act", "dve", "pool", "sp")}
        self.dma_hist = {q: [None] * NDMASEM for q in DMAQ}
        self.dma_cnt = {q: [0] * NDMASEM for q in DMAQ}
        self.dma_rr = {q: 0 for q in DMAQ}
        self.sems = {}
        self.dsems = {}
        self.out_dmas = []
        self.bar = set()
        self.last = {e: None for e in COMPUTE}
        self.nadd = 0
        self.limit = None
        self.trace = None

    def dram(self, name, shape, dtype, kind="Internal"):
        t = self.nc.dram_tensor(name, list(shape), dtype, kind=kind)
        return V(t.ap(), [])

    def barrier(self):
        b = set()
        for e in COMPUTE:
            if self.last[e] is not None:
                b.add(self.last[e])
        for q in DMAQ:
            for o in self.dma_hist[q]:
                if o is not None:
                    b.add(o)
        for o in b:
            o.sig = True
        self.bar = b

    def add(self, eng, fn, reads=(), writes=(), dma=False):
        op = Op(eng, fn, dma)
        self.nadd += 1
        op.idx = self.nadd
        if self.trace is not None and self.trace[0] <= self.nadd <= self.trace[1]:
            import traceback
            fr = [f for f in traceback.extract_stack() if f.name not in ("add",)][-2:]
            print("OP", self.nadd, eng, "dma" if dma else "", [(f.lineno, f.line) for f in fr][-1])
        if self.limit is not None and self.nadd > self.limit:
            op.deps = set()
            return op
        deps = set(self.bar)
        for v in reads:
            for b in v.bufs:
                if b.w is not None:
                    deps.add(b.w)
                if b.excl:
                    for key, r in b.r.items():
                        if key != eng and not isinstance(r, list):
                            deps.add(r)
        for v in writes:
            for b in v.bufs:
                if b.w is not None:
                    deps.add(b.w)
                for r in b.r.values():
                    if isinstance(r, list):
                        deps.update(r)
                    elif r.eng != eng or r.dma or dma:
                        deps.add(r)
        if dma:
            q = eng
            slot = self.dma_rr[q] % NDMASEM
            self.dma_rr[q] += 1
            prev = self.dma_hist[q][slot]
            if prev is not None:
                deps.add(prev)
            self.dma_cnt[q][slot] += 1
            op.dsem = (q, slot)
            op.dtgt = 16 * self.dma_cnt[q][slot]
            self.dma_hist[q][slot] = op
        deps.discard(op)
        if eng == "pe":
            deps = {d for d in deps if d.dma or d.eng != "pe"}
        op.deps = deps
        for d in deps:
            d.sig = True
        for v in reads:
            for b in v.bufs:
                if dma:
                    b.r.setdefault(("dma", eng), []).append(op)
                else:
                    b.r[eng] = op
        for v in writes:
            for b in v.bufs:
                b.w = op
                b.r = {}
        self.ops[eng].append(op)
        if not dma:
            self.last[eng] = op
        return op

    def emit(self):
        nc = self.nc
        es = self.es
        import os as _os2
        for _i in range(int(_os2.environ.get("KDUMMYSEM", "0"))):
            es.enter_context(nc.semaphore("dummy%d" % _i))
        for e in COMPUTE:
            self.sems[e] = es.enter_context(nc.semaphore("s_" + e))
        for q in DMAQ:
            for i in range(NDMASEM):
                self.dsems[(q, i)] = es.enter_context(nc.semaphore("d_%s%d" % (q, i)))
        for e in COMPUTE:
            c = 0
            for op in self.ops[e]:
                if op.dma:
                    continue
                if op.sig:
                    c += 1
                    op.cnt = c
            assert c < 65000, (e, c)
        final_waits = list(self.out_dmas)
        block = es.enter_context(nc.Block())
        sched = self

        def run(ename, eng):
            waited = {e: 0 for e in COMPUTE}
            dwaited = {}
            for op in sched.ops[ename]:
                need = {}
                if sched.trace is not None and sched.trace[0] <= op.idx <= sched.trace[1]:
                    print("EMIT", op.idx, ename, "cnt", op.cnt, "sig", op.sig, "deps", sorted((d.eng, d.idx, d.cnt, d.dtgt) for d in op.deps))
                for d in op.deps:
                    if d.dma:
                        if dwaited.get(d.dsem, 0) < d.dtgt:
                            dwaited[d.dsem] = d.dtgt
                            eng.wait_ge(sched.dsems[d.dsem], d.dtgt)
                    else:
                        if d.cnt > need.get(d.eng, 0):
                            need[d.eng] = d.cnt
                for se, c in need.items():
                    if c > waited[se]:
                        waited[se] = c
                        eng.wait_ge(sched.sems[se], c)
                inst = op.fn(eng)
                if op.dma:
                    inst.then_inc(sched.dsems[op.dsem], 16)
                elif op.sig:
                    inst.then_inc(sched.sems[ename], 1)
            if ename == "sp":
                for d in final_waits:
                    eng.wait_ge(sched.dsems[d.dsem], d.dtgt)

        @block.tensor
        def _(eng):
            run("pe", eng)

        @block.scalar
        def _(eng):
            run("act", eng)

        @block.vector
        def _(eng):
            run("dve", eng)

        @block.gpsimd
        def _(eng):
            run("pool", eng)

        @block.sync
        def _(eng):
            run("sp", eng)

    def dma(self, out, in_, q="sp", is_out=False):
        op = self.add(q, lambda e: e.dma_start(out=out.ap, in_=in_.ap), reads=[in_], writes=[out], dma=True)
        if is_out:
            self.out_dmas.append(op)
        return op

    def mm(self, out, lhsT, rhs, start=True, stop=True):
        return self.add("pe", lambda e: e.matmul(out.ap, lhsT.ap, rhs.ap, start=start, stop=stop),
                        reads=[lhsT, rhs], writes=[out])

    def transpose(self, out, in_, ident):
        return self.add("pe", lambda e: e.transpose(out.ap, in_.ap, ident.ap), reads=[in_, ident], writes=[out])

    def act(self, out, in_, func, bias=None, scale=None, accum=None):
        reads = [in_]
        kw = {}
        if bias is not None:
            if isinstance(bias, V):
                reads.append(bias)
                kw["bias"] = bias.ap
            else:
                kw["bias"] = bias
        if scale is not None:
            if isinstance(scale, V):
                reads.append(scale)
                kw["scale"] = scale.ap
            else:
                kw["scale"] = scale
        writes = [out]
        if accum is not None:
            kw["accum_out"] = accum.ap
            writes.append(accum)
        return self.add("act", lambda e: e.activation(out.ap, in_.ap, func, **kw), reads=reads, writes=writes)

    def tt(self, out, a, b, op, eng="dve"):
        return self.add(eng, lambda e: e.tensor_tensor(out.ap, a.ap, b.ap, op), reads=[a, b], writes=[out])

    def ts(self, out, a, s1, s2, op0, op1=None, eng="dve"):
        reads = [a]
        a1 = s1.ap if isinstance(s1, V) else s1
        a2 = s2.ap if isinstance(s2, V) else s2
        if isinstance(s1, V):
            reads.append(s1)
        if isinstance(s2, V):
            reads.append(s2)
        if op1 is None:
            return self.add(eng, lambda e: e.tensor_scalar(out.ap, a.ap, a1, None, op0), reads=reads, writes=[out])
        return self.add(eng, lambda e: e.tensor_scalar(out.ap, a.ap, a1, a2, op0, op1), reads=reads, writes=[out])

    def stt(self, out, a, s, b, op0, op1):
        reads = [a, b]
        a1 = s.ap if isinstance(s, V) else s
        if isinstance(s, V):
            reads.append(s)
        return self.add("dve", lambda e: e.scalar_tensor_tensor(out.ap, a.ap, a1, b.ap, op0, op1),
                        reads=reads, writes=[out])

    def copy(self, out, in_, eng="dve"):
        if eng == "act":
            return self.add("act", lambda e: e.copy(out.ap, in_.ap), reads=[in_], writes=[out])
        return self.add(eng, lambda e: e.tensor_copy(out.ap, in_.ap), reads=[in_], writes=[out])

    def memset(self, out, val, eng="pool"):
        return self.add(eng, lambda e: e.memset(out.ap, val), reads=[], writes=[out])

    def scan(self, out, d0, d1, init):
        reads = [d0, d1]
        i = init.ap if isinstance(init, V) else init
        if isinstance(init, V):
            reads.append(init)
        return self.add("dve", lambda e: e.tensor_tensor_scan(out.ap, d0.ap, d1.ap, i, ALU.mult, ALU.add),
                        reads=reads, writes=[out])

    def recip(self, out, in_):
        return self.add("dve", lambda e: e.reciprocal(out.ap, in_.ap), reads=[in_], writes=[out])

    def reduce(self, out, in_, op):
        return self.add("dve", lambda e: e.tensor_reduce(out.ap, in_.ap, AX.X, op), reads=[in_], writes=[out])

    def max8(self, out, in_):
        return self.add("dve", lambda e: e.max(out.ap, in_.ap), reads=[in_], writes=[out])


class Region:
    def __init__(self, arena_ap, start, end, cached=False):
        self.ap = arena_ap
        self.cached = cached
        self.cache = {}
        self.start = start
        self.end = end
        self.off = start

    def reset(self):
        if not self.cached:
            self.off = self.start

    def alloc(self, name, free_shape, dtype):
        if self.cached and name in self.cache:
            return self.cache[name]
        v = self._alloc(name, free_shape, dtype)
        if self.cached:
            self.cache[name] = v
        return v

    def _alloc(self, name, free_shape, dtype):
        n = 1
        for s in free_shape:
            n *= s
        esz = 4 if dtype == F32 else 2
        nb = (n * esz + 63) // 64 * 64
        assert self.off + nb <= self.end, ("sbuf region overflow", name, self.off, nb, self.end)
        a = self.ap[:, self.off // 4:(self.off + nb) // 4]
        self.off += nb
        if dtype != F32:
            a = a.bitcast(dtype)
        a = a[:, 0:n]
        if len(free_shape) == 2:
            a = a.rearrange("p (a b) -> p a b", a=free_shape[0])
        elif len(free_shape) == 3:
            a = a.rearrange("p (a b c) -> p a b c", a=free_shape[0], b=free_shape[1])
        return V(a, [Buf(name)])


def rms_rstd(S, R, ss, n, width, tag):
    r = R.alloc("rstd_" + tag, [width], F32)
    S.act(r, ss, AF.Sqrt, bias=EPS, scale=1.0 / n)
    S.recip(r, r)
    return r


def build(SEQ, NB, depth=2, debug=False, plan="ABSCD", nlayers=None):
    T = LCTX + SEQ
    NT = T // 128
    NLT = SEQ // 128
    nc = bass.Bass("TRN2", target_bir_lowering=False)
    es = ExitStack()
    with es:
        S = Sched(nc, es)
        import os as _os
        if _os.environ.get("KTRACE"):
            S.trace = tuple(int(v) for v in _os.environ["KTRACE"].split(","))
        if _os.environ.get("KLIMIT"):
            S.limit = int(_os.environ["KLIMIT"])
        okind = "ExternalOutput" if debug else "Internal"
        x_d = S.dram("x", [NB, SEQ, D], F32, "ExternalInput")
        ctx_d = S.dram("ctx", [NB, LCTX, D], F32, "ExternalInput")
        cT_d = S.dram("cT", [128, KD, 3], F32, "ExternalInput")
        w_ada_d = S.dram("w_ada", [depth, D, 6 * D], F32, "ExternalInput")
        b_adaT_d = S.dram("b_adaT", [depth, 128, 48], F32, "ExternalInput")
        b_ada_d = S.dram("b_ada", [depth, 6 * D], F32, "ExternalInput")
        g1T_d = S.dram("g1T", [depth, 128, KD], F32, "ExternalInput")
        g2T_d = S.dram("g2T", [depth, 128, KD], F32, "ExternalInput")
        w_in_d = S.dram("w_in_p", [depth, D, 1952], F32, "ExternalInput")
        g_cqT_d = S.dram("g_cqT", [depth, 128, 2], F32, "ExternalInput")
        g_ckvT_d = S.dram("g_ckvT", [depth, 128, 1], F32, "ExternalInput")
        w_uq_d = S.dram("w_uq_p", [depth, 256, 384], F32, "ExternalInput")
        w_ukv_d = S.dram("w_ukv_p", [depth, 128, 512], F32, "ExternalInput")
        sink_d = S.dram("sink", [depth, 4], F32, "ExternalInput")
        conv_wT_d = S.dram("conv_wT", [depth, 128, 4, 4], F32, "ExternalInput")
        conv_bT_d = S.dram("conv_bT", [depth, 128, 4], F32, "ExternalInput")
        lru_wa_d = S.dram("lru_wa", [depth, 2, 8, 64, 64], F32, "ExternalInput")
        lru_wx_d = S.dram("lru_wx", [depth, 2, 8, 64, 64], F32, "ExternalInput")
        lru_vT_d = S.dram("lru_vT", [depth, 128, 3, 2, 4], F32, "ExternalInput")
        g_grpT_d = S.dram("g_grpT", [depth, 128, KD], F32, "ExternalInput")
        w_out_d = S.dram("w_out", [depth, D, D], F32, "ExternalInput")
        wr_d = S.dram("wr", [depth, D, 36], F32, "ExternalInput")
        rb_d = S.dram("rb", [depth, 36], F32, "ExternalInput")
        weg_d = S.dram("w_e_gate", [depth, NEXP, D, DEXP], F32, "ExternalInput")
        weu_d = S.dram("w_e_up", [depth, NEXP, D, DEXP], F32, "ExternalInput")
        wed_d = S.dram("w_e_down", [depth, NEXP, DEXP, D], F32, "ExternalInput")
        gfin_d = S.dram("g_final", [D], F32, "ExternalInput")
        ident_d = S.dram("ident", [128, 128], F32, "ExternalInput")
        masks_d = S.dram("masks", [128, 2, 128], F32, "ExternalInput")
        rope_d = S.dram("rope", [NLT, 128, 96], F32, "ExternalInput")
        out_d = S.dram("out", [NB, SEQ, D], F32, "ExternalOutput")
        GATES = S.dram("s_gates", [depth, 3, 2, D], F32, okind)
        XS = S.dram("s_xs", [NB, T, D], F32, okind)
        X1S = S.dram("s_x1s", [NB, T, D], F32, okind)
        ZL = S.dram("s_zl", [NB, 1024, T], F32, okind)
        QT = S.dram("s_qt", [NB, 96, 4, T], BF16, okind)
        KT = S.dram("s_kt", [NB, 96, 4, T], BF16, okind)
        VA = S.dram("s_va", [NB, T, 260], BF16, okind)
        SQKT = S.dram("s_sqkt", [NB, 64, 6, T], BF16, okind)
        SVA = S.dram("s_sva", [NB, T, 130], BF16, okind)
        OMIX = S.dram("s_omix", [NB, T, 512], F32, okind)
        OLRU = S.dram("s_olru", [NB, 512, T], BF16, okind)

        ARENA_BYTES = 190 * 1024
        arena_t = es.enter_context(nc.sbuf_tensor("arena", [128, ARENA_BYTES // 4], F32))
        arena = arena_t[:]
        PERS_BYTES = 12 * 1024
        P = Region(arena, 0, PERS_BYTES)
        R = Region(arena, PERS_BYTES, ARENA_BYTES)
        psum = []
        for i in range(4):
            t = es.enter_context(nc.psum_tensor("ps%d" % i, [128, 1024], F32))
            a = t[:]
            b0, b1 = Buf("ps%da" % i, excl=True), Buf("ps%db" % i, excl=True)
            psum.append((V(a, [b0, b1]), V(a[:, 0:512], [b0]), V(a[:, 512:1024], [b1])))
        PB = []
        for pr in psum:
            PB.append(pr[1])
            PB.append(pr[2])

        def pbf(bank):
            return V(bank.ap.bitcast(BF16), bank.bufs)

        identF = P.alloc("identF", [128], F32)
        identB = P.alloc("identB", [128], BF16)
        maskB = P.alloc("maskB", [2, 128], BF16)
        ones1 = P.alloc("ones1", [1], F32)
        modT = P.alloc("modT", [depth, 48, 3], F32)
        AB = P.alloc("AB", [depth * 3, 4, KD], F32)
        sT = P.alloc("sT", [KD, 3], F32)
        S.dma(identF, ident_d)
        S.copy(identB, identF, eng="dve")
        S.dma(sT, cT_d)
        S.memset(ones1, 1.0, eng="dve")
        R.reset()
        mtmp = R.alloc("mtmp", [2, 128], F32)
        S.dma(mtmp, masks_d)
        S.copy(maskB, mtmp, eng="dve")
        S.act(sT, sT, AF.Silu)
        wab = [R.alloc("wab%d" % i, [KD, 512], F32) for i in range(2)]
        brow = [R.alloc("brow%d" % i, [512], F32) for i in range(2)]
        grow = [R.alloc("grow%d" % i, [512], F32) for i in range(2)]
        badaT = R.alloc("badaT", [depth, 48], F32)
        gT = R.alloc("gT", [depth, 2, KD], F32)
        for l in range(depth):
            S.dma(badaT[:, l, :], b_adaT_d[l])
            S.dma(gT[:, l, 0, :], g1T_d[l])
            S.dma(gT[:, l, 1, :], g2T_d[l])
        it = 0
        for l in range(depth):
            for j in range(12):
                w = wab[it % 2]
                S.dma(w, w_ada_d[l][:, j * 512:(j + 1) * 512].re("(k p) n -> p k n", p=128))
                vec = j // 2
                if vec in (2, 5):
                    br = brow[it % 2]
                    S.dma(br[0:3, :], V(b_ada_d.ap[l, j * 512:(j + 1) * 512].partition_broadcast(3), []))
                    pm = PB[it % 2]
                    for k in range(KD):
                        S.mm(pm[0:3, :], sT[:, k, :], w[:, k, :], start=(k == 0), stop=(k == KD - 1))
                    gr = grow[it % 2]
                    S.tt(gr[0:3, :], pm[0:3, :], br[0:3, :], ALU.add)
                    S.dma(GATES[l, :, 0 if vec == 2 else 1, (j % 2) * 512:(j % 2 + 1) * 512], gr[0:3, :])
                else:
                    for m in range(4):
                        pm = PB[2 + (m % 2)]
                        for k in range(KD):
                            S.mm(pm[:, 0:3], w[:, k, m * 128:(m + 1) * 128], sT[:, k, :],
                                 start=(k == 0), stop=(k == KD - 1))
                        S.act(modT[:, l, j * 4 + m, :], pm[:, 0:3], AF.Identity,
                              bias=badaT[:, l, j * 4 + m:j * 4 + m + 1], scale=1.0)
                it += 1
            for r in range(3):
                ab = AB[:, l * 3 + r]
                S.ts(ab[:, 0, :], modT[:, l, 8:16, r], 1.0, None, ALU.add)
                S.tt(ab[:, 0, :], ab[:, 0, :], gT[:, l, 0, :], ALU.mult)
                S.copy(ab[:, 1, :], modT[:, l, 0:8, r])
                S.ts(ab[:, 2, :], modT[:, l, 32:40, r], 1.0, None, ALU.add)
                S.tt(ab[:, 2, :], ab[:, 2, :], gT[:, l, 1, :], ALU.mult)
                S.copy(ab[:, 3, :], modT[:, l, 24:32, r])
        S.barrier()
        if debug:
            print("ops after prologue", S.nadd)

        def x_src(b, l, t):
            if l == 0:
                if t < 2:
                    return ctx_d[b, t * 128:(t + 1) * 128, :]
                return x_d[b, (t - 2) * 128:(t - 1) * 128, :]
            return XS[b, t * 128:(t + 1) * 128, :]

        def norm_transpose(xt, A, Bc, hT_dst, tmpR, junk, pbanks, hTf_dst=None):
            ss = tmpR.alloc("ss", [1], F32)
            S.act(junk, xt, AF.Square, accum=ss)
            rstd = rms_rstd(S, tmpR, ss, D, 1, "x")
            xn = tmpR.alloc("xn", [D], F32)
            S.ts(xn, xt, rstd, None, ALU.mult, eng="pool")
            for half in range(2):
                pb = pbanks[half]
                for kk in range(4):
                    k = half * 4 + kk
                    S.transpose(pb[:, kk * 128:(kk + 1) * 128], xn[:, k * 128:(k + 1) * 128], identF)
                for kk in range(4):
                    k = half * 4 + kk
                    S.act(hT_dst[:, k, :], pb[:, kk * 128:(kk + 1) * 128], AF.Identity,
                          bias=Bc[:, k:k + 1], scale=A[:, k:k + 1])
                    if hTf_dst is not None:
                        S.ts(hTf_dst[:, k, :], pb[:, kk * 128:(kk + 1) * 128], A[:, k:k + 1], Bc[:, k:k + 1],
                             ALU.mult, ALU.add)

        def phase_A(b, l):
            R.reset()
            rowl, rowc = l * 3 + b, l * 3 + 2
            w_in = R.alloc("w_in", [KD, 1952], BF16)
            for k in range(KD):
                S.dma(w_in[:, k, :], w_in_d[l][k * 128:(k + 1) * 128, :], q="pool")
            wq_f = R.alloc("wq_f", [2, 384], F32)
            wkv_f = R.alloc("wkv_f", [512], F32)
            gq = R.alloc("gq", [3], F32)
            S.dma(wq_f, w_uq_d[l].re("(j p) n -> p j n", p=128))
            S.dma(wkv_f, w_ukv_d[l])
            S.dma(gq[:, 0:2], g_cqT_d[l])
            S.dma(gq[:, 2:3], g_ckvT_d[l])
            wq = R.alloc("wq", [2, 384], BF16)
            wkv = R.alloc("wkv", [512], BF16)
            for j in range(2):
                S.ts(wq[:, j, :], wq_f[:, j, :], gq[:, j:j + 1], None, ALU.mult)
            S.ts(wkv, wkv_f, gq[:, 2:3], None, ALU.mult)
            xt = [R.alloc("xt%d" % i, [D], F32) for i in range(2)]
            rp = [R.alloc("rp%d" % i, [96], F32) for i in range(2)]
            hTs = [R.alloc("hTs%d" % i, [KD, 512], BF16) for i in range(2)]
            junk = R.alloc("junk", [D], BF16)
            qa = [R.alloc("qa%d" % i, [4, 96], BF16) for i in range(2)]
            ka = [R.alloc("ka%d" % i, [4, 96], BF16) for i in range(2)]
            va = [R.alloc("va%d" % i, [4, 65], BF16) for i in range(2)]
            sqk = [R.alloc("sqk%d" % i, [6, 64], BF16) for i in range(2)]
            sva = [R.alloc("sva%d" % i, [2, 65], BF16) for i in range(2)]
            qTt = [R.alloc("qTt%d" % i, [4, 128], BF16) for i in range(2)]
            kTt = [R.alloc("kTt%d" % i, [4, 128], BF16) for i in range(2)]
            sqkT = [R.alloc("sqkT%d" % i, [6, 128], BF16) for i in range(2)]
            zlt = [R.alloc("zlt%d" % i, [512], F32) for i in range(2)]
            for i in range(2):
                S.memset(va[i][:, :, 64:65], 1.0)
                S.memset(sva[i][:, :, 64:65], 1.0)
            TR = Region(arena, R.off, ARENA_BYTES, cached=True)
            tiles = list(range(NT))
            S.dma(xt[0], x_src(b, l, 0))
            groups = [[0, 1]] + [list(range(2 + 4 * g, 2 + 4 * g + 4)) for g in range(NLT // 4)]
            gi = 0
            for grp in groups:
                hT = hTs[gi % 2]
                for ti, t in enumerate(grp):
                    TR.reset()
                    is_ctx = t < 2
                    A = AB[:, rowc if is_ctx else rowl]
                    x_t = xt[t % 2]
                    if t + 1 < NT:
                        S.dma(xt[(t + 1) % 2], x_src(b, l, t + 1))
                    if not is_ctx:
                        S.dma(rp[t % 2], rope_d[t - 2])
                    rpt = rp[t % 2]
                    hTt = hT[:, :, ti * 128:(ti + 1) * 128]
                    norm_transpose(x_t, A[:, 0, :], A[:, 1, :], hTt, TR, junk, (PB[0], PB[1]))
                    for k in range(KD):
                        S.mm(PB[2][:, 0:416], hTt[:, k, :], w_in[:, k, 0:416], start=(k == 0), stop=(k == KD - 1))
                    for k in range(KD):
                        S.mm(PB[3][:, 0:512], hTt[:, k, :], w_in[:, k, 416:928], start=(k == 0), stop=(k == KD - 1))
                    z0, z1 = PB[2], PB[3]
                    ss2 = TR.alloc("ss2", [2], F32)
                    S.act(junk[:, 0:256], z0[:, 0:256], AF.Square, accum=ss2[:, 0:1])
                    S.act(junk[:, 256:384], z0[:, 256:384], AF.Square, accum=ss2[:, 1:2])
                    rs2 = TR.alloc("rs2", [2], F32)
                    S.act(rs2[:, 0:1], ss2[:, 0:1], AF.Sqrt, bias=EPS, scale=1.0 / 256)
                    S.act(rs2[:, 1:2], ss2[:, 1:2], AF.Sqrt, bias=EPS, scale=1.0 / 128)
                    S.recip(rs2, rs2)
                    cn = TR.alloc("cn", [384], BF16)
                    S.act(cn[:, 0:256], z0[:, 0:256], AF.Identity, scale=rs2[:, 0:1])
                    S.act(cn[:, 256:384], z0[:, 256:384], AF.Identity, scale=rs2[:, 1:2])
                    pT = pbf(PB[4])
                    for j in range(3):
                        S.transpose(pT[:, j * 128:(j + 1) * 128], cn[:, j * 128:(j + 1) * 128], identB)
                    cT = TR.alloc("cTt", [3, 128], BF16)
                    S.copy(cT, pT[:, 0:384].re("p (j t) -> p j t", j=3), eng="dve")
                    pq, pkv = PB[5], PB[6]
                    for j in range(2):
                        S.mm(pq[:, 0:384], cT[:, j, :], wq[:, j, :], start=(j == 0), stop=(j == 1))
                    S.mm(pkv[:, 0:512], cT[:, 2, :], wkv, start=True, stop=True)
                    kr = TR.alloc("kr", [32], F32)
                    q_a, k_a, v_a, sqk_a, sv_a = qa[t % 2], ka[t % 2], va[t % 2], sqk[t % 2], sva[t % 2]
                    pq3 = pq[:, 0:384].re("p (h d) -> p h d", h=4)
                    S.copy(q_a[:, :, 0:64], pq3[:, :, 0:64], eng="act")
                    S.copy(k_a[:, :, 0:64], pkv[:, 0:256].re("p (h d) -> p h d", h=4), eng="act")
                    S.copy(v_a[:, :, 0:64], pkv[:, 256:512].re("p (h d) -> p h d", h=4), eng="act")
                    S.copy(sv_a[:, :, 0:64], z1[:, 384:512].re("p (h d) -> p h d", h=2), eng="act")
                    z1h = z1[:, 0:384].re("p (h d) -> p h d", h=6)
                    if is_ctx:
                        S.copy(q_a[:, :, 64:96], pq3[:, :, 64:96], eng="dve")
                        S.copy(kr, z0[:, 384:416], eng="dve")
                        S.copy(sqk_a, z1h, eng="act")
                    else:
                        def rope(dst1, dst2, X1, X2, Ct, St, shape, tag):
                            t1 = TR.alloc("r1" + tag, shape, F32)
                            t2 = TR.alloc("r2" + tag, shape, F32)
                            S.tt(t1, X1, Ct, ALU.mult)
                            S.tt(t2, X2, St, ALU.mult)
                            S.tt(dst1, t1, t2, ALU.subtract)
                            t3 = TR.alloc("r3" + tag, shape, F32)
                            t4 = TR.alloc("r4" + tag, shape, F32)
                            S.tt(t3, X2, Ct, ALU.mult)
                            S.tt(t4, X1, St, ALU.mult)
                            S.tt(dst2, t3, t4, ALU.add)
                        Cm, Sm = rpt[:, 0:16], rpt[:, 16:32]
                        Cs, Ss = rpt[:, 32:64], rpt[:, 64:96]
                        rope(q_a[:, :, 64:80], q_a[:, :, 80:96], pq3[:, :, 64:80], pq3[:, :, 80:96],
                             Cm.bcast(1, [128, 4, 16]), Sm.bcast(1, [128, 4, 16]), [4, 16], "q")
                        rope(kr[:, 0:16], kr[:, 16:32], z0[:, 384:400], z0[:, 400:416], Cm, Sm, [16], "k")
                        rope(sqk_a[:, :, 0:32], sqk_a[:, :, 32:64], z1h[:, :, 0:32], z1h[:, :, 32:64],
                             Cs.bcast(1, [128, 6, 32]), Ss.bcast(1, [128, 6, 32]), [6, 32], "s")
                    S.copy(k_a[:, :, 64:96], kr.bcast(1, [128, 4, 32]), eng="dve")
                    pTq, pTk, pTs = pbf(PB[4]), pbf(PB[7]), pbf(PB[5])
                    for h in range(4):
                        S.transpose(pTq[0:96, h * 128:(h + 1) * 128], q_a[:, h, :], identB)
                    qT_t = qTt[t % 2]
                    S.copy(qT_t[0:96], pTq[0:96, 0:512].re("p (h t) -> p h t", h=4), eng="dve")
                    for h in range(4):
                        S.transpose(pTk[0:96, h * 128:(h + 1) * 128], k_a[:, h, :], identB)
                    kT_t = kTt[t % 2]
                    S.copy(kT_t[0:96], pTk[0:96, 0:512].re("p (h t) -> p h t", h=4), eng="act")
                    for h in range(6):
                        S.transpose(pTs[0:64, h * 128:(h + 1) * 128], sqk_a[:, h, :], identB)
                    sT_t = sqkT[t % 2]
                    S.copy(sT_t[0:64], pTs[0:64, 0:768].re("p (h t) -> p h t", h=6), eng="dve")
                    tok = slice(t * 128, (t + 1) * 128)
                    S.dma(QT[b, :, :, tok], qT_t[0:96])
                    S.dma(KT[b, :, :, tok], kT_t[0:96])
                    S.dma(SQKT[b, :, :, tok], sT_t[0:64])
                    S.dma(VA[b, tok, :], v_a.re("p h d -> p (h d)"))
                    S.dma(SVA[b, tok, :], sv_a.re("p h d -> p (h d)"))
                ntok = 128 * len(grp)
                tok0 = grp[0] * 128
                for m in range(8):
                    pz = PB[m % 2]
                    for k in range(KD):
                        S.mm(pz[:, 0:ntok], w_in[:, k, 928 + m * 128:928 + (m + 1) * 128], hT[:, k, 0:ntok],
                             start=(k == 0), stop=(k == KD - 1))
                    zt = zlt[m % 2]
                    S.copy(zt[:, 0:ntok], pz[:, 0:ntok], eng=("act" if m % 2 else "dve"))
                    S.dma(ZL[b, m * 128:(m + 1) * 128, tok0:tok0 + ntok], zt[:, 0:ntok])
                gi += 1
            S.barrier()

        def phase_B(b, l):
            R.reset()
            kT = R.alloc("kT", [4, T], BF16)
            vA = R.alloc("vA", [NT, 260], BF16)
            for h in range(4):
                S.dma(kT[0:96, h, :], KT[b, :, h, :])
            for c0 in range(0, NT, 8):
                c1 = min(NT, c0 + 8)
                S.dma(vA[:, c0:c1, :], VA[b, c0 * 128:c1 * 128, :].re("(c p) n -> p c n", p=128))
            qTb = [R.alloc("qTb%d" % i, [4, 512], BF16) for i in range(2)]
            pt = [R.alloc("pt%d" % i, [512], BF16) for i in range(4)]
            usb = [R.alloc("usb%d" % i, [512], F32) for i in range(2)]
            ot = [R.alloc("ot%d" % i, [4, 64], F32) for i in range(2)]
            rc = [R.alloc("rc%d" % i, [4], F32) for i in range(2)]
            chunks = []
            if l < depth - 1:
                chunks.append((0, 256, 2))
            for c in range(SEQ // 512):
                chunks.append((256 + c * 512, 512, NT))
            Sb = [PB[0], PB[1], PB[2], PB[3]]
            Ub = [PB[4], PB[5]]
            Tb = [PB[6], PB[7]]
            S.dma(qTb[0][0:96, :, 0:chunks[0][1]], QT[b, :, :, chunks[0][0]:chunks[0][0] + chunks[0][1]])
            cnt = 0
            ui = 0
            for ci, (q0, nq, nkc) in enumerate(chunks):
                qt = qTb[ci % 2]
                if ci + 1 < len(chunks):
                    nq0, nnq, _ = chunks[ci + 1]
                    S.dma(qTb[(ci + 1) % 2][0:96, :, 0:nnq], QT[b, :, :, nq0:nq0 + nnq])
                for h in range(4):
                    U = Ub[ui % 2]
                    def score(c):
                        sb = Sb[(cnt + c) % 4]
                        S.mm(sb[:, 0:nq], kT[0:96, h, c * 128:(c + 1) * 128], qt[0:96, h, 0:nq])
                        p = pt[(cnt + c) % 4]
                        S.act(p[:, 0:nq], sb[:, 0:nq], AF.Exp, scale=MLA_SCALE)
                        return p
                    ps = {0: score(0)}
                    if nkc > 1:
                        ps[1] = score(1)
                    for c in range(nkc):
                        if c + 2 < nkc:
                            ps[c + 2] = score(c + 2)
                        S.mm(U[0:65, 0:nq], vA[:, c, h * 65:(h + 1) * 65], ps[c][:, 0:nq],
                             start=(c == 0), stop=(c == nkc - 1))
                        del ps[c]
                    cnt += nkc
                    us = usb[ui % 2]
                    S.copy(us[0:65, 0:nq], U[0:65, 0:nq], eng="dve")
                    tb = Tb[ui % 2]
                    nsub = nq // 128
                    for s in range(nsub):
                        S.transpose(tb[:, s * 65:(s + 1) * 65], us[0:65, s * 128:(s + 1) * 128], identF[0:65, 0:65])
                    t3 = tb[:, 0:nsub * 65].re("p (s d) -> p s d", s=nsub)
                    r = rc[ui % 2]
                    S.recip(r[:, 0:nsub], t3[:, :, 64])
                    o = ot[ui % 2]
                    S.tt(o[:, 0:nsub, :], t3[:, :, 0:64], r[:, 0:nsub].bcast(2, [128, nsub, 64]), ALU.mult)
                    S.dma(OMIX[b, q0:q0 + nq, h * 64:(h + 1) * 64].re("(s p) d -> p s d", p=128), o[:, 0:nsub, :])
                    ui += 1
            S.barrier()

        def phase_B2(b, l):
            R.reset()
            sT_ = R.alloc("sqkT_all", [6, T], BF16)
            svA = R.alloc("svA", [NT, 130], BF16)
            for h in range(6):
                S.dma(sT_[0:64, h, :], SQKT[b, :, h, :])
            for c0 in range(0, NT, 8):
                c1 = min(NT, c0 + 8)
                S.dma(svA[:, c0:c1, :], SVA[b, c0 * 128:c1 * 128, :].re("(c p) n -> p c n", p=128))
            esink = R.alloc("esink", [4], F32)
            S.dma(esink, V(sink_d.ap[l].partition_broadcast(128), []))
            S.act(esink, esink, AF.Exp)
            pt = [R.alloc("spt%d" % i, [2, 128], BF16) for i in range(4)]
            usb = [R.alloc("susb%d" % i, [256], F32) for i in range(2)]
            ot = [R.alloc("sot%d" % i, [2, 64], F32) for i in range(2)]
            rc = [R.alloc("src%d" % i, [2], F32) for i in range(2)]
            Sb = [PB[0], PB[1], PB[2], PB[3]]
            Ub = [PB[4], PB[5]]
            Tb = [PB[6], PB[7]]
            qtiles = list(range(2, NT)) if l == depth - 1 else list(range(NT))
            cnt = 0
            ui = 0
            for tq in qtiles:
                if tq < 2:
                    keys = [(0, None), (1, None)]
                else:
                    keys = [(0, None), (1, None)]
                    if tq - 1 >= 2:
                        keys.append((tq - 1, 0))
                    keys.append((tq, None))
                    if tq + 1 < NT:
                        keys.append((tq + 1, 1))
                for g in range(2):
                    U = Ub[ui % 2]
                    q = sT_[0:64, 2 * g:2 * g + 2, tq * 128:(tq + 1) * 128]
                    plist = []
                    for (kc, mk) in keys:
                        sb = Sb[cnt % 4]
                        S.mm(sb[:, 0:256].re("p (h t) -> p h t", h=2), sT_[0:64, 4 + g, kc * 128:(kc + 1) * 128], q)
                        p = pt[cnt % 4]
                        S.act(p, sb[:, 0:256].re("p (h t) -> p h t", h=2), AF.Exp, scale=SWA_SCALE)
                        if mk is not None:
                            S.tt(p, p, maskB[:, mk, :].bcast(1, [128, 2, 128]), ALU.mult, eng="pool")
                        plist.append(p)
                        cnt += 1
                        if len(plist) >= 2:
                            idx = len(plist) - 2
                            kc2 = keys[idx][0]
                            S.mm(U[0:65, 0:256], svA[:, kc2, g * 65:(g + 1) * 65], plist[idx].re("p h t -> p (h t)"),
                                 start=(idx == 0), stop=False)
                    idx = len(plist) - 1
                    S.mm(U[0:65, 0:256], svA[:, keys[idx][0], g * 65:(g + 1) * 65], plist[idx].re("p h t -> p (h t)"),
                         start=(idx == 0), stop=True)
                    us = usb[ui % 2]
                    S.copy(us[0:65, :], U[0:65, 0:256], eng="dve")
                    tb = Tb[ui % 2]
                    for s in range(2):
                        S.transpose(tb[:, s * 65:(s + 1) * 65], us[0:65, s * 128:(s + 1) * 128], identF[0:65, 0:65])
                    t3 = tb[:, 0:130].re("p (s d) -> p s d", s=2)
                    r = rc[ui % 2]
                    S.tt(r, t3[:, :, 64], esink[:, 2 * g:2 * g + 2], ALU.add)
                    S.recip(r, r)
                    o = ot[ui % 2]
                    S.tt(o, t3[:, :, 0:64], r.bcast(2, [128, 2, 64]), ALU.mult)
                    S.dma(OMIX[b, tq * 128:(tq + 1) * 128, 256 + g * 128:256 + (g + 1) * 128], o.re("p s d -> p (s d)"))
                    ui += 1
            S.barrier()

        def phase_C(b, l):
            R.reset()
            ZW = T + 8
            CO, LO = 2, 261
            wst = R.alloc("wst", [4, 4, 128], F32)
            S.memset(wst, 0.0, eng="dve")
            for ty, wd_ in enumerate((lru_wa_d, lru_wx_d)):
                for d in range(2):
                    for half in range(2):
                        src = wd_[l, d].re("(m two) c e -> two c m e", two=2)[half]
                        S.dma(wst[half * 64:(half + 1) * 64, ty * 2 + d, :, half * 64:(half + 1) * 64], src)
            wbd = R.alloc("wbd", [4, 4, 128], BF16)
            S.copy(wbd, wst, eng="dve")
            vT = R.alloc("vT", [3, 2, 4], F32)
            S.dma(vT, lru_vT_d[l])
            cw = R.alloc("cw", [4, 4], F32)
            cb = R.alloc("cb", [4], F32)
            S.dma(cw, conv_wT_d[l])
            S.dma(cb, conv_bT_d[l])
            cneg = R.alloc("cneg", [2, 4], F32)
            S.act(cneg, vT[:, 2], AF.Exp, scale=-1.0)
            S.act(cneg, cneg, AF.Ln, bias=1.0, scale=1.0)
            S.ts(cneg, cneg, -8.0, None, ALU.mult)
            cnh = R.alloc("cnh", [2, 4], F32)
            S.ts(cnh, cneg, 0.5, None, ALU.mult)
            zb = R.alloc("zb", [ZW], F32)
            zbd = zb.sub(Buf("zbdata"))
            S.memset(V(zb.ap, zb.bufs + zbd.bufs), 0.0, eng="pool")
            u = R.alloc("u", [T], F32)
            ub = R.alloc("ub", [T], BF16)
            rr = R.alloc("rr", [T], F32)
            ii = R.alloc("ii", [T], F32)
            tq_ = R.alloc("tq", [T], F32)
            hf = R.alloc("hf", [T], F32)
            hb = R.alloc("hb", [T], F32)
            gz = R.alloc("gz", [T], F32)
            ob = R.alloc("ob", [T], BF16)
            tchunks = [(0, 256)] + [(256 + c * 512, 512) for c in range(SEQ // 512)]
            for m in range(4):
                S.dma(zbd[:, CO:CO + 256], ZL[b, m * 128:(m + 1) * 128, 0:256])
                S.dma(zbd[:, LO:LO + SEQ], ZL[b, m * 128:(m + 1) * 128, 256:T])
                S.dma(gz, ZL[b, 512 + m * 128:512 + (m + 1) * 128, :])
                zr = V(zb.ap, zb.bufs + zbd.bufs)
                for (o0, o1, n) in ((0, 0, 256), (256, 259, SEQ)):
                    S.ts(u[:, o0:o0 + n], zr[:, o1:o1 + n], cw[:, m, 0:1], cb[:, m:m + 1], ALU.mult, ALU.add)
                    for tap in range(1, 4):
                        S.stt(u[:, o0:o0 + n], zr[:, o1 + tap:o1 + tap + n], cw[:, m, tap:tap + 1], u[:, o0:o0 + n],
                              ALU.mult, ALU.add)
                S.copy(ub, u, eng="pool")
                S.act(gz, gz, AF.Gelu_apprx_tanh)
                for d in range(2):
                    for ci, (t0, n) in enumerate(tchunks):
                        pa, px = PB[(ci % 2) * 2], PB[(ci % 2) * 2 + 1]
                        S.mm(pa[:, 0:n], wbd[:, 0 * 2 + d, m, :], ub[:, t0:t0 + n])
                        S.mm(px[:, 0:n], wbd[:, 1 * 2 + d, m, :], ub[:, t0:t0 + n])
                        S.act(rr[:, t0:t0 + n], pa[:, 0:n], AF.Sigmoid, bias=vT[:, 0, d, m:m + 1], scale=1.0)
                        S.act(ii[:, t0:t0 + n], px[:, 0:n], AF.Sigmoid, bias=vT[:, 1, d, m:m + 1], scale=1.0)
                    S.act(tq_, rr, AF.Tanh, scale=cnh[:, d, m:m + 1])
                    S.act(rr, rr, AF.Exp, scale=cneg[:, d, m:m + 1])
                    S.act(tq_, tq_, AF.Sqrt, scale=-1.0)
                    S.stt(tq_, rr, 1.0, tq_, ALU.add, ALU.mult)
                    S.tt(ii, ii, u, ALU.mult, eng="pool")
                    S.tt(ii, ii, tq_, ALU.mult)
                    if d == 0:
                        S.scan(hf, rr, ii, 0.0)
                    else:
                        S.scan(hb[:, 0:256][:, ::-1], rr[:, 0:256][:, ::-1], ii[:, 0:256][:, ::-1], 0.0)
                        S.scan(hb[:, 256:T][:, ::-1], rr[:, 256:T][:, ::-1], ii[:, 256:T][:, ::-1], hb[:, 0:1])
                S.tt(hf, hf, hb, ALU.add)
                S.tt(ob, hf, gz, ALU.mult)
                S.dma(OLRU[b, m * 128:(m + 1) * 128, :], ob)
            S.barrier()

        def phase_DE(b, l, tiles):
            R.reset()
            last = l == depth - 1
            ntl = len(tiles)
            ntok = ntl * 128
            H2T = R.alloc("H2T", [KD, ntok], BF16)
            COMB = R.alloc("COMB", [ntl, 32], F32)
            mark = R.off
            wo_f = R.alloc("wo_f", [KD, D], F32)
            S.dma(wo_f, w_out_d[l].re("(k p) n -> p k n", p=128))
            gg = R.alloc("gg", [KD], F32)
            S.dma(gg, g_grpT_d[l])
            wo = R.alloc("wo", [KD, D], BF16)
            for k in range(KD):
                S.ts(wo[:, k, :], wo_f[:, k, :], gg[:, k:k + 1], None, ALU.mult, eng=("pool" if k % 2 else "dve"))
            wrt = R.alloc("wrt", [KD, 36], F32)
            S.dma(wrt, wr_d[l].re("(k p) n -> p k n", p=128))
            rbt = R.alloc("rbt", [36], F32)
            S.dma(rbt, V(rb_d.ap[l].partition_broadcast(128), []))
            G1 = [R.alloc("G1_%d" % i, [D], F32) for i in range(2)]
            S.dma(G1[0], V(GATES.ap[l, b, 0].partition_broadcast(128), []))
            S.dma(G1[1], V(GATES.ap[l, 2, 0].partition_broadcast(128), []))
            xt = [R.alloc("dxt%d" % i, [D], F32) for i in range(2)]
            om = [R.alloc("om%d" % i, [512], F32) for i in range(2)]
            ol = [R.alloc("ol%d" % i, [4, 128], BF16) for i in range(2)]
            junk = R.alloc("djunk", [D], BF16)
            tA = R.alloc("tA", [D], F32)
            h2f = R.alloc("h2f", [KD, 128], F32)
            TR = Region(arena, R.off, ARENA_BYTES, cached=True)
            rowl, rowc = l * 3 + b, l * 3 + 2

            def loads(i):
                t = tiles[i]
                tok = slice(t * 128, (t + 1) * 128)
                S.dma(xt[i % 2], x_src(b, l, t))
                S.dma(om[i % 2], OMIX[b, tok, :])
                S.dma(ol[i % 2], OLRU[b, :, tok].re("(m p) t -> p m t", p=128))
            loads(0)
            for i, t in enumerate(tiles):
                TR.reset()
                is_ctx = t < 2
                tok = slice(t * 128, (t + 1) * 128)
                if i + 1 < ntl:
                    loads(i + 1)
                x_t, o_m, o_l = xt[i % 2], om[i % 2], ol[i % 2]
                ss2 = TR.alloc("dss2", [2], F32)
                S.act(junk[:, 0:256], o_m[:, 0:256], AF.Square, accum=ss2[:, 0:1])
                S.act(junk[:, 256:512], o_m[:, 256:512], AF.Square, accum=ss2[:, 1:2])
                rs2 = rms_rstd(S, TR, ss2, 256, 2, "g")
                on = TR.alloc("on", [512], BF16)
                S.act(on[:, 0:256], o_m[:, 0:256], AF.Identity, scale=rs2[:, 0:1])
                S.act(on[:, 256:512], o_m[:, 256:512], AF.Identity, scale=rs2[:, 1:2])
                pT = pbf(PB[0])
                for j in range(4):
                    S.transpose(pT[:, j * 128:(j + 1) * 128], on[:, j * 128:(j + 1) * 128], identB)
                mT = TR.alloc("mT", [4, 128], BF16)
                S.copy(mT, pT[:, 0:512].re("p (j t) -> p j t", j=4), eng="dve")
                sq = TR.alloc("sq", [4, 128], F32)
                S.tt(sq, o_l, o_l, ALU.mult, eng="pool")
                pss = PB[1]
                for m in range(4):
                    S.mm(pss[:, 0:1], sq[:, m, :], ones1, start=(m == 0), stop=(m == 3))
                rsl = TR.alloc("rsl", [1], F32)
                S.act(rsl, pss[:, 0:1], AF.Sqrt, bias=EPS, scale=1.0 / 512)
                S.recip(rsl, rsl)
                PA, PL = psum[1], psum[2]
                for nh in range(2):
                    for j in range(4):
                        S.mm(PA[nh + 1], mT[:, j, :], wo[:, j, nh * 512:(nh + 1) * 512], start=(j == 0), stop=(j == 3))
                for nh in range(2):
                    for m in range(4):
                        S.mm(PL[nh + 1], o_l[:, m, :], wo[:, 4 + m, nh * 512:(nh + 1) * 512], start=(m == 0), stop=(m == 3))
                S.copy(tA, PA[0], eng="act")
                S.stt(tA, PL[0], rsl, tA, ALU.mult, ALU.add)
                S.tt(tA, tA, G1[1 if is_ctx else 0], ALU.mult, eng="pool")
                S.tt(x_t, x_t, tA, ALU.add)
                S.dma(X1S[b, tok, :], x_t)
                A = AB[:, rowc if is_ctx else rowl]
                norm_transpose(x_t, A[:, 2, :], A[:, 3, :], H2T[:, :, i * 128:(i + 1) * 128], TR, junk,
                               (PB[6], PB[7]), hTf_dst=h2f)
                pr = PB[0]
                for k in range(KD):
                    S.mm(pr[:, 0:36], h2f[:, k, :], wrt[:, k, :], start=(k == 0), stop=(k == KD - 1))
                lg = TR.alloc("lg", [36], F32)
                S.tt(lg, pr[:, 0:36], rbt, ALU.add)
                gmax = TR.alloc("gmax", [1], F32)
                S.reduce(gmax, lg[:, 0:4], ALU.max)
                mg = TR.alloc("mg", [4], F32)
                S.ts(mg, lg[:, 0:4], gmax, None, ALU.is_equal)
                ngm = TR.alloc("ngm", [1], F32)
                S.ts(ngm, gmax, -1.0, None, ALU.mult)
                eg = TR.alloc("eg", [4], F32)
                sg = TR.alloc("sg", [1], F32)
                S.act(eg, lg[:, 0:4], AF.Exp, bias=ngm, scale=1.0, accum=sg)
                pgt = TR.alloc("pgt", [1], F32)
                S.recip(pgt, sg)
                les = TR.alloc("les", [8], F32)
                S.ts(les, lg[:, 4:12], mg[:, 0:1], None, ALU.mult)
                for g in range(1, 4):
                    S.stt(les, lg[:, 4 + 8 * g:12 + 8 * g], mg[:, g:g + 1], les, ALU.mult, ALU.add)
                top = TR.alloc("top", [8], F32)
                S.max8(top, les)
                m1 = TR.alloc("m1", [8], F32)
                m2 = TR.alloc("m2", [8], F32)
                S.ts(m1, les, top[:, 0:1], None, ALU.is_equal)
                S.ts(m2, les, top[:, 1:2], None, ALU.is_equal)
                dd = TR.alloc("dd", [1], F32)
                S.tt(dd, top[:, 1:2], top[:, 0:1], ALU.subtract)
                e2 = TR.alloc("e2", [1], F32)
                S.act(e2, dd, AF.Exp)
                w1 = TR.alloc("w1", [1], F32)
                S.ts(w1, e2, 1.0, None, ALU.add)
                S.recip(w1, w1)
                S.tt(w1, w1, pgt, ALU.mult)
                w2 = TR.alloc("w2", [1], F32)
                S.tt(w2, w1, e2, ALU.mult)
                cwt = TR.alloc("cwt", [8], F32)
                S.ts(cwt, m1, w1, None, ALU.mult)
                S.stt(cwt, m2, w2, cwt, ALU.mult, ALU.add)
                for g in range(4):
                    S.ts(COMB[:, i, g * 8:(g + 1) * 8], cwt, mg[:, g:g + 1], None, ALU.mult)
            S.barrier()
            R.off = mark
            Y = R.alloc("Y", [ntl, D], F32)
            wg = [R.alloc("wg%d" % i, [KD, DEXP], BF16) for i in range(2)]
            wu = [R.alloc("wu%d" % i, [KD, DEXP], BF16) for i in range(2)]
            wd = [R.alloc("wd%d" % i, [2, D], BF16) for i in range(2)]
            sgl = [R.alloc("sgl%d" % i, [512], F32) for i in range(2)]
            aT = [R.alloc("aT%d" % i, [2, 512], BF16) for i in range(2)]
            Yb = [Y[:, i, :].sub(Buf("Y%d" % i)) for i in range(ntl)]

            def wload(e):
                S.dma(wg[e % 2], weg_d[l, e].re("(k p) n -> p k n", p=128), q="pool")
                S.dma(wu[e % 2], weu_d[l, e].re("(k p) n -> p k n", p=128), q="pool")
                S.dma(wd[e % 2], wed_d[l, e].re("(k p) n -> p k n", p=128), q="pool")
            wload(0)
            chunks = []
            c0 = 0
            while c0 < ntok:
                n = min(512, ntok - c0)
                chunks.append((c0, n))
                c0 += n
            it = 0
            for e in range(NEXP):
                if e + 1 < NEXP:
                    wload(e + 1)
                g_w, u_w, d_w = wg[e % 2], wu[e % 2], wd[e % 2]
                for (c0, n) in chunks:
                    a_t = aT[it % 2]
                    for half in range(2):
                        pg_, pu_ = PB[half * 2], PB[half * 2 + 1]
                        for k in range(KD):
                            S.mm(pg_[:, 0:n], g_w[:, k, half * 128:(half + 1) * 128], H2T[:, k, c0:c0 + n],
                                 start=(k == 0), stop=(k == KD - 1))
                        for k in range(KD):
                            S.mm(pu_[:, 0:n], u_w[:, k, half * 128:(half + 1) * 128], H2T[:, k, c0:c0 + n],
                                 start=(k == 0), stop=(k == KD - 1))
                        sg_ = sgl[half]
                        S.act(sg_[:, 0:n], pg_[:, 0:n], AF.Silu)
                        S.tt(a_t[:, half, 0:n], sg_[:, 0:n], pu_[:, 0:n], ALU.mult)
                    for s in range(n // 128):
                        ti = c0 // 128 + s
                        pd = psum[2 + (ti % 2)]
                        for nh in range(2):
                            for half in range(2):
                                S.mm(pd[nh + 1], a_t[:, half, s * 128:(s + 1) * 128], d_w[:, half, nh * 512:(nh + 1) * 512],
                                     start=(half == 0), stop=(half == 1))
                        if e == 0:
                            S.ts(Yb[ti], pd[0], COMB[:, ti, e:e + 1], None, ALU.mult)
                        else:
                            S.stt(Yb[ti], pd[0], COMB[:, ti, e:e + 1], Yb[ti], ALU.mult, ALU.add)
                    it += 1
            G2 = [R.alloc("G2_%d" % i, [D], F32) for i in range(2)]
            S.dma(G2[0], V(GATES.ap[l, b, 1].partition_broadcast(128), []))
            S.dma(G2[1], V(GATES.ap[l, 2, 1].partition_broadcast(128), []))
            x1 = [R.alloc("x1_%d" % i, [D], F32) for i in range(2)]
            ejunk = R.alloc("ejunk", [D], BF16)
            if last:
                GF = R.alloc("GF", [D], F32)
                S.dma(GF, V(gfin_d.ap.partition_broadcast(128), []))
            TR = Region(arena, R.off, ARENA_BYTES, cached=True)
            S.dma(x1[0], X1S[b, tiles[0] * 128:(tiles[0] + 1) * 128, :])
            for i, t in enumerate(tiles):
                TR.reset()
                tok = slice(t * 128, (t + 1) * 128)
                if i + 1 < ntl:
                    S.dma(x1[(i + 1) % 2], X1S[b, tiles[i + 1] * 128:(tiles[i + 1] + 1) * 128, :])
                x_t = x1[i % 2]
                S.tt(Yb[i], Yb[i], G2[1 if t < 2 else 0], ALU.mult, eng="pool")
                S.tt(x_t, x_t, Yb[i], ALU.add)
                if not last:
                    S.dma(XS[b, tok, :], x_t)
                else:
                    ss = TR.alloc("ess", [1], F32)
                    S.act(ejunk, x_t, AF.Square, accum=ss)
                    rstd = rms_rstd(S, TR, ss, D, 1, "f")
                    S.stt(x_t, x_t, rstd, GF, ALU.mult, ALU.mult)
                    S.dma(out_d[b, (t - 2) * 128:(t - 1) * 128, :], x_t, is_out=True)
            S.barrier()

        for b in range(NB):
            for l in range(depth if nlayers is None else nlayers):
                if "A" in plan:
                    phase_A(b, l)
                if "B" in plan:
                    phase_B(b, l)
                if "S" in plan:
                    phase_B2(b, l)
                if "C" in plan:
                    phase_C(b, l)
                if "D" not in plan:
                    continue
                if l == depth - 1:
                    tl = list(range(2, NT))
                else:
                    tl = list(range(NT))
                nblk = (len(tl) + 16) // 17
                per = (len(tl) + nblk - 1) // nblk
                for i in range(nblk):
                    blk = tl[i * per:(i + 1) * per]
                    if blk:
                        phase_DE(b, l, blk)
        if debug:
            print("total ops", S.nadd)
        S.emit()
    return nc


def _perm_rot(nheads, hd, base=0):
    q = hd // 4
    idx = []
    for h in range(nheads):
        o = base + h * hd
        idx += list(range(o, o + q)) + list(range(o + 2 * q, o + 3 * q)) + list(range(o + q, o + 2 * q)) + \
            list(range(o + 3 * q, o + 4 * q))
    return idx


def _chan_major(v, nch):
    return np.ascontiguousarray(np.swapaxes(v.reshape(v.shape[:-1] + (nch, 128)), -1, -2))


def host_shared(inp, SEQ):
    f = np.float32
    depth = inp["w_ada"].shape[0]
    sh = {}
    sh["w_ada"] = np.ascontiguousarray(inp["w_ada"], f)
    sh["b_ada"] = np.ascontiguousarray(inp["b_ada"], f)
    sh["b_adaT"] = _chan_major(inp["b_ada"].astype(f), 48)
    sh["g1T"] = _chan_major(inp["g_norm1"].astype(f), 8)
    sh["g2T"] = _chan_major(inp["g_norm2"].astype(f), 8)
    cols = list(range(0, 384)) + _perm_rot(1, 32, 384) + _perm_rot(4, 64, 416) + _perm_rot(2, 64, 672) + \
        list(range(800, 1952))
    sh["w_in_p"] = np.ascontiguousarray(inp["w_in"][:, :, cols], f)
    sh["g_cqT"] = _chan_major(inp["g_cq"].astype(f), 2)
    sh["g_ckvT"] = _chan_major(inp["g_ckv"].astype(f), 1)
    qcols = []
    for h in range(4):
        qcols += list(range(h * 96, h * 96 + 64)) + _perm_rot(1, 32, h * 96 + 64)
    sh["w_uq_p"] = np.ascontiguousarray(inp["w_uq"][:, :, qcols], f)
    kvcols = [h * 128 + i for h in range(4) for i in range(64)] + [h * 128 + 64 + i for h in range(4) for i in range(64)]
    sh["w_ukv_p"] = np.ascontiguousarray(inp["w_ukv"][:, :, kvcols], f)
    sh["sink"] = np.ascontiguousarray(inp["swa_sink"], f)
    cw = inp["conv_w"].astype(f)
    sh["conv_wT"] = np.ascontiguousarray(np.transpose(cw.reshape(depth, 4, 4, 128), (0, 3, 2, 1)))
    sh["conv_bT"] = _chan_major(inp["conv_b"].astype(f), 4)
    sh["lru_wa"] = np.ascontiguousarray(inp["lru_wa"], f)
    sh["lru_wx"] = np.ascontiguousarray(inp["lru_wx"], f)
    v = np.stack([inp["lru_ba"], inp["lru_bx"], inp["lru_lam"]], axis=1).astype(f)
    sh["lru_vT"] = np.ascontiguousarray(np.transpose(v.reshape(depth, 3, 2, 4, 128), (0, 4, 1, 2, 3)))
    sh["g_grpT"] = _chan_major(inp["g_grp"].astype(f), 8)
    sh["w_out"] = np.ascontiguousarray(inp["w_out"], f)
    wg2 = np.transpose(inp["w_g2"], (0, 2, 1, 3)).reshape(depth, D, 32)
    sh["wr"] = np.ascontiguousarray(np.concatenate([inp["w_g1"], wg2], axis=-1), f)
    sh["rb"] = np.ascontiguousarray(np.concatenate([inp["b_g1"], inp["b_g2"].reshape(depth, 32)], axis=-1), f)
    sh["w_e_gate"] = np.ascontiguousarray(inp["w_e_gate"], f)
    sh["w_e_up"] = np.ascontiguousarray(inp["w_e_up"], f)
    sh["w_e_down"] = np.ascontiguousarray(inp["w_e_down"], f)
    sh["g_final"] = np.ascontiguousarray(inp["g_final"], f)
    sh["ident"] = np.eye(128, dtype=f)
    j = np.arange(128)[:, None]
    i = np.arange(128)[None, :]
    sh["masks"] = np.ascontiguousarray(np.stack([(j >= i), (j <= i)], axis=1).astype(f))
    pos = np.arange(SEQ)
    rows = (pos // 64).astype(np.float32)
    colsp = (pos % 64).astype(np.float32)

    def tab(half):
        fr = (np.float32(10000.0) ** (-np.arange(half, dtype=np.float32) / np.float32(half))).astype(np.float32)
        ar = rows[:, None] * fr[None, :]
        ac = colsp[:, None] * fr[None, :]
        C = np.concatenate([np.cos(ar), np.cos(ac)], axis=1)
        Sn = np.concatenate([np.sin(ar), np.sin(ac)], axis=1)
        return C.astype(f), Sn.astype(f)
    Cm, Sm = tab(8)
    Cs, Ss = tab(16)
    sh["rope"] = np.ascontiguousarray(np.concatenate([Cm, Sm, Cs, Ss], axis=1).reshape(SEQ // 128, 128, 96))
    return sh


def host_core(inp, b0, NB):
    f = np.float32
    cv = np.stack([inp["c"][b0], inp["c"][min(b0 + 1, inp["c"].shape[0] - 1)], inp["c_ctx"]], axis=0).astype(f)
    cT = np.ascontiguousarray(np.transpose(cv.reshape(3, KD, 128), (2, 1, 0)))
    return {
        "x": np.ascontiguousarray(inp["x"][b0:b0 + NB], f),
        "ctx": np.ascontiguousarray(inp["ctx"][b0:b0 + NB], f),
        "cT": cT,
    }


_NC_CACHE = {}


def kernel(**inputs):
    inp = {k: np.asarray(v) for k, v in inputs.items()}
    B, SEQ, _ = inp["x"].shape
    ncores = 8
    NB = B // ncores
    key = (SEQ, NB)
    if key not in _NC_CACHE:
        _NC_CACHE[key] = build(SEQ, NB)
    nc = _NC_CACHE[key]
    sh = host_shared(inp, SEQ)
    in_maps = []
    for c in range(ncores):
        m = dict(sh)
        m.update(host_core(inp, c * NB, NB))
        in_maps.append(m)
    res = run_bass_kernel_spmd(nc, in_maps, core_ids=list(range(ncores)))
    out = np.concatenate([np.asarray(r["out"]) for r in res.results], axis=0)
    return out.astype(np.float32)
```

```python
import numpy as np
from contextlib import ExitStack
import concourse.bass as bass
import concourse.mybir as mybir
from concourse.bass_utils import run_bass_kernel_spmd

F32 = mybir.dt.float32
BF16 = mybir.dt.bfloat16
AF = mybir.ActivationFunctionType
ALU = mybir.AluOpType
AX = mybir.AxisListType

D = 1024
KD = 8
LCTX = 256
EPS = 1e-6
NEXP = 32
DEXP = 256
MLA_SCALE = 96 ** -0.5
SWA_SCALE = 0.125

COMPUTE = ("pe", "act", "dve", "pool")
DMAQ = ("sp", "pool")
NDMASEM = 12


class Buf:
    __slots__ = ("w", "r", "name", "excl")

    def __init__(self, name="", excl=False):
        self.w = None
        self.r = {}
        self.name = name
        self.excl = excl


class V:
    __slots__ = ("ap", "bufs")

    def __init__(self, ap, bufs):
        self.ap = ap
        self.bufs = bufs

    def __getitem__(self, idx):
        return V(self.ap[idx], self.bufs)

    def re(self, pat, **kw):
        return V(self.ap.rearrange(pat, **kw), self.bufs)

    def bcast(self, axis, shape):
        return V(self.ap.unsqueeze(axis).broadcast_to(list(shape)), self.bufs)

    def sub(self, buf):
        return V(self.ap, [buf])


class Op:
    __slots__ = ("eng", "fn", "deps", "sig", "cnt", "dma", "dsem", "dtgt", "idx")

    def __init__(self, eng, fn, dma):
        self.eng = eng
        self.fn = fn
        self.dma = dma
        self.deps = None
        self.sig = False
        self.cnt = 0
        self.dsem = None
        self.dtgt = 0


class Sched:
    def __init__(self, nc, es):
        self.nc = nc
        self.es = es
        self.ops = {e: [] for e in ("pe", "act", "dve", "pool", "sp")}
        self.dma_hist = {q: [None] * NDMASEM for q in DMAQ}
        self.dma_cnt = {q: [0] * NDMASEM for q in DMAQ}
        self.dma_rr = {q: 0 for q in DMAQ}
        self.sems = {}
        self.dsems = {}
        self.out_dmas = []
        self.bar = set()
        self.last = {e: None for e in COMPUTE}
        self.nadd = 0
        self.limit = None
        self.trace = None

    def dram(self, name, shape, dtype, kind="Internal"):
        t = self.nc.dram_tensor(name, list(shape), dtype, kind=kind)
        return V(t.ap(), [])

    def barrier(self):
        b = set()
        for e in COMPUTE:
            if self.last[e] is not None:
                b.add(self.last[e])
        for q in DMAQ:
            for o in self.dma_hist[q]:
                if o is not None:
                    b.add(o)
        for o in b:
            o.sig = True
        self.bar = b

    def add(self, eng, fn, reads=(), writes=(), dma=False):
        op = Op(eng, fn, dma)
        self.nadd += 1
        op.idx = self.nadd
        if self.trace is not None and self.trace[0] <= self.nadd <= self.trace[1]:
            import traceback
            fr = [f for f in traceback.extract_stack() if f.name not in ("add",)][-2:]
            print("OP", self.nadd, eng, "dma" if dma else "", [(f.lineno, f.line) for f in fr][-1])
        if self.limit is not None and self.nadd > self.limit:
            op.deps = set()
            return op
        deps = set(self.bar)
        for v in reads:
            for b in v.bufs:
                if b.w is not None:
                    deps.add(b.w)
                if b.excl:
                    for key, r in b.r.items():
                        if key != eng and not isinstance(r, list):
                            deps.add(r)
        for v in writes:
            for b in v.bufs:
                if b.w is not None:
                    deps.add(b.w)
                for r in b.r.values():
                    if isinstance(r, list):
                        deps.update(r)
                    elif r.eng != eng or r.dma or dma:
                        deps.add(r)
        if dma:
            q = eng
            slot = self.dma_rr[q] % NDMASEM
            self.dma_rr[q] += 1
            prev = self.dma_hist[q][slot]
            if prev is not None:
                deps.add(prev)
            self.dma_cnt[q][slot] += 1
            op.dsem = (q, slot)
            op.dtgt = 16 * self.dma_cnt[q][slot]
            self.dma_hist[q][slot] = op
        deps.discard(op)
        if eng == "pe":
            deps = {d for d in deps if d.dma or d.eng != "pe"}
        op.deps = deps
        for d in deps:
            d.sig = True
        for v in reads:
            for b in v.bufs:
                if dma:
                    b.r.setdefault(("dma", eng), []).append(op)
                else:
                    b.r[eng] = op
        for v in writes:
            for b in v.bufs:
                b.w = op
                b.r = {}
        self.ops[eng].append(op)
        if not dma:
            self.last[eng] = op
        return op

    def emit(self):
        nc = self.nc
        es = self.es
        import os as _os2
        for _i in range(int(_os2.environ.get("KDUMMYSEM", "0"))):
            es.enter_context(nc.semaphore("dummy%d" % _i))
        for e in COMPUTE:
            self.sems[e] = es.enter_context(nc.semaphore("s_" + e))
        for q in DMAQ:
            for i in range(NDMASEM):
                self.dsems[(q, i)] = es.enter_context(nc.semaphore("d_%s%d" % (q, i)))
        for e in COMPUTE:
            c = 0
            for op in self.ops[e]:
                if op.dma:
                    continue
                if op.sig:
                    c += 1
                    op.cnt = c
            assert c < 65000, (e, c)
        final_waits = list(self.out_dmas)
        block = es.enter_context(nc.Block())
        sched = self

        def run(ename, eng):
            waited = {e: 0 for e in COMPUTE}
            dwaited = {}
            for op in sched.ops[ename]:
                need = {}
                if sched.trace is not None and sched.trace[0] <= op.idx <= sched.trace[1]:
                    print("EMIT", op.idx, ename, "cnt", op.cnt, "sig", op.sig, "deps", sorted((d.eng, d.idx, d.cnt, d.dtgt) for d in op.deps))
                for d in op.deps:
                    if d.dma:
                        if dwaited.get(d.dsem, 0) < d.dtgt:
                            dwaited[d.dsem] = d.dtgt
                            eng.wait_ge(sched.dsems[d.dsem], d.dtgt)
                    else:
                        if d.cnt > need.get(d.eng, 0):
                            need[d.eng] = d.cnt
                for se, c in need.items():
                    if c > waited[se]:
                        waited[se] = c
                        eng.wait_ge(sched.sems[se], c)
                inst = op.fn(eng)
                if op.dma:
                    inst.then_inc(sched.dsems[op.dsem], 16)
                elif op.sig:
                    inst.then_inc(sched.sems[ename], 1)
            if ename == "sp":
                for d in final_waits:
                    eng.wait_ge(sched.dsems[d.dsem], d.dtgt)

        @block.tensor
        def _(eng):
            run("pe", eng)

        @block.scalar
        def _(eng):
            run("act", eng)

        @block.vector
        def _(eng):
            run("dve", eng)

        @block.gpsimd
        def _(eng):
            run("pool", eng)

        @block.sync
        def _(eng):
            run("sp", eng)

    def dma(self, out, in_, q="sp", is_out=False):
        op = self.add(q, lambda e: e.dma_start(out=out.ap, in_=in_.ap), reads=[in_], writes=[out], dma=True)
        if is_out:
            self.out_dmas.append(op)
        return op

    def mm(self, out, lhsT, rhs, start=True, stop=True):
        return self.add("pe", lambda e: e.matmul(out.ap, lhsT.ap, rhs.ap, start=start, stop=stop),
                        reads=[lhsT, rhs], writes=[out])

    def transpose(self, out, in_, ident):
        return self.add("pe", lambda e: e.transpose(out.ap, in_.ap, ident.ap), reads=[in_, ident], writes=[out])

    def act(self, out, in_, func, bias=None, scale=None, accum=None):
        reads = [in_]
        kw = {}
        if bias is not None:
            if isinstance(bias, V):
                reads.append(bias)
                kw["bias"] = bias.ap
            else:
                kw["bias"] = bias
        if scale is not None:
            if isinstance(scale, V):
                reads.append(scale)
                kw["scale"] = scale.ap
            else:
                kw["scale"] = scale
        writes = [out]
        if accum is not None:
            kw["accum_out"] = accum.ap
            writes.append(accum)
        return self.add("act", lambda e: e.activation(out.ap, in_.ap, func, **kw), reads=reads, writes=writes)

    def tt(self, out, a, b, op, eng="dve"):
        return self.add(eng, lambda e: e.tensor_tensor(out.ap, a.ap, b.ap, op), reads=[a, b], writes=[out])

    def ts(self, out, a, s1, s2, op0, op1=None, eng="dve"):
        reads = [a]
        a1 = s1.ap if isinstance(s1, V) else s1
        a2 = s2.ap if isinstance(s2, V) else s2
        if isinstance(s1, V):
            reads.append(s1)
        if isinstance(s2, V):
            reads.append(s2)
        if op1 is None:
            return self.add(eng, lambda e: e.tensor_scalar(out.ap, a.ap, a1, None, op0), reads=reads, writes=[out])
        return self.add(eng, lambda e: e.tensor_scalar(out.ap, a.ap, a1, a2, op0, op1), reads=reads, writes=[out])

    def stt(self, out, a, s, b, op0, op1):
        reads = [a, b]
        a1 = s.ap if isinstance(s, V) else s
        if isinstance(s, V):
            reads.append(s)
        return self.add("dve", lambda e: e.scalar_tensor_tensor(out.ap, a.ap, a1, b.ap, op0, op1),
                        reads=reads, writes=[out])

    def copy(self, out, in_, eng="dve"):
        if eng == "act":
            return self.add("act", lambda e: e.copy(out.ap, in_.ap), reads=[in_], writes=[out])
        return self.add(eng, lambda e: e.tensor_copy(out.ap, in_.ap), reads=[in_], writes=[out])

    def memset(self, out, val, eng="pool"):
        return self.add(eng, lambda e: e.memset(out.ap, val), reads=[], writes=[out])

    def scan(self, out, d0, d1, init):
        reads = [d0, d1]
        i = init.ap if isinstance(init, V) else init
        if isinstance(init, V):
            reads.append(init)
        return self.add("dve", lambda e: e.tensor_tensor_scan(out.ap, d0.ap, d1.ap, i, ALU.mult, ALU.add),
                        reads=reads, writes=[out])

    def recip(self, out, in_):
        return self.add("dve", lambda e: e.reciprocal(out.ap, in_.ap), reads=[in_], writes=[out])

    def reduce(self, out, in_, op):
        return self.add("dve", lambda e: e.tensor_reduce(out.ap, in_.ap, AX.X, op), reads=[in_], writes=[out])

    def max8(self, out, in_):
        return self.add("dve", lambda e: e.max(out.ap, in_.ap), reads=[in_], writes=[out])


class Region:
    def __init__(self, arena_ap, start, end, cached=False):
        self.ap = arena_ap
        self.cached = cached
        self.cache = {}
        self.start = start
        self.end = end
        self.off = start

    def reset(self):
        if not self.cached:
            self.off = self.start

    def alloc(self, name, free_shape, dtype):
        if self.cached and name in self.cache:
            return self.cache[name]
        v = self._alloc(name, free_shape, dtype)
        if self.cached:
            self.cache[name] = v
        return v

    def _alloc(self, name, free_shape, dtype):
        n = 1
        for s in free_shape:
            n *= s
        esz = 4 if dtype == F32 else 2
        nb = (n * esz + 63) // 64 * 64
        assert self.off + nb <= self.end, ("sbuf region overflow", name, self.off, nb, self.end)
        a = self.ap[:, self.off // 4:(self.off + nb) // 4]
        self.off += nb
        if dtype != F32:
            a = a.bitcast(dtype)
        a = a[:, 0:n]
        if len(free_shape) == 2:
            a = a.rearrange("p (a b) -> p a b", a=free_shape[0])
        elif len(free_shape) == 3:
            a = a.rearrange("p (a b c) -> p a b c", a=free_shape[0], b=free_shape[1])
        return V(a, [Buf(name)])


def rms_rstd(S, R, ss, n, width, tag):
    r = R.alloc("rstd_" + tag, [width], F32)
    S.act(r, ss, AF.Sqrt, bias=EPS, scale=1.0 / n)
    S.recip(r, r)
    return r


def build(SEQ, NB, depth=2, debug=False, plan="ABSCD", nlayers=None):
    T = LCTX + SEQ
    NT = T // 128
    NLT = SEQ // 128
    nc = bass.Bass("TRN2", target_bir_lowering=False)
    es = ExitStack()
    with es:
        S = Sched(nc, es)
        import os as _os
        if _os.environ.get("KTRACE"):
            S.trace = tuple(int(v) for v in _os.environ["KTRACE"].split(","))
        if _os.environ.get("KLIMIT"):
            S.limit = int(_os.environ["KLIMIT"])
        okind = "ExternalOutput" if debug else "Internal"
        x_d = S.dram("x", [NB, SEQ, D], F32, "ExternalInput")
        ctx_d = S.dram("ctx", [NB, LCTX, D], F32, "ExternalInput")
        cT_d = S.dram("cT", [128, KD, 3], F32, "ExternalInput")
        w_ada_d = S.dram("w_ada", [depth, D, 6 * D], F32, "ExternalInput")
        b_adaT_d = S.dram("b_adaT", [depth, 128, 48], F32, "ExternalInput")
        b_ada_d = S.dram("b_ada", [depth, 6 * D], F32, "ExternalInput")
        g1T_d = S.dram("g1T", [depth, 128, KD], F32, "ExternalInput")
        g2T_d = S.dram("g2T", [depth, 128, KD], F32, "ExternalInput")
        w_in_d = S.dram("w_in_p", [depth, D, 1952], F32, "ExternalInput")
        g_cqT_d = S.dram("g_cqT", [depth, 128, 2], F32, "ExternalInput")
        g_ckvT_d = S.dram("g_ckvT", [depth, 128, 1], F32, "ExternalInput")
        w_uq_d = S.dram("w_uq_p", [depth, 256, 384], F32, "ExternalInput")
        w_ukv_d = S.dram("w_ukv_p", [depth, 128, 512], F32, "ExternalInput")
        sink_d = S.dram("sink", [depth, 4], F32, "ExternalInput")
        conv_wT_d = S.dram("conv_wT", [depth, 128, 4, 4], F32, "ExternalInput")
        conv_bT_d = S.dram("conv_bT", [depth, 128, 4], F32, "ExternalInput")
        lru_wa_d = S.dram("lru_wa", [depth, 2, 8, 64, 64], F32, "ExternalInput")
        lru_wx_d = S.dram("lru_wx", [depth, 2, 8, 64, 64], F32, "ExternalInput")
        lru_vT_d = S.dram("lru_vT", [depth, 128, 3, 2, 4], F32, "ExternalInput")
        g_grpT_d = S.dram("g_grpT", [depth, 128, KD], F32, "ExternalInput")
        w_out_d = S.dram("w_out", [depth, D, D], F32, "ExternalInput")
        wr_d = S.dram("wr", [depth, D, 36], F32, "ExternalInput")
        rb_d = S.dram("rb", [depth, 36], F32, "ExternalInput")
        weg_d = S.dram("w_e_gate", [depth, NEXP, D, DEXP], F32, "ExternalInput")
        weu_d = S.dram("w_e_up", [depth, NEXP, D, DEXP], F32, "ExternalInput")
        wed_d = S.dram("w_e_down", [depth, NEXP, DEXP, D], F32, "ExternalInput")
        gfin_d = S.dram("g_final", [D], F32, "ExternalInput")
        ident_d = S.dram("ident", [128, 128], F32, "ExternalInput")
        masks_d = S.dram("masks", [128, 2, 128], F32, "ExternalInput")
        rope_d = S.dram("rope", [NLT, 128, 96], F32, "ExternalInput")
        out_d = S.dram("out", [NB, SEQ, D], F32, "ExternalOutput")
        GATES = S.dram("s_gates", [depth, 3, 2, D], F32, okind)
        XS = S.dram("s_xs", [NB, T, D], F32, okind)
        X1S = S.dram("s_x1s", [NB, T, D], F32, okind)
        ZL = S.dram("s_zl", [NB, 1024, T], F32, okind)
        QT = S.dram("s_qt", [NB, 96, 4, T], BF16, okind)
        KT = S.dram("s_kt", [NB, 96, 4, T], BF16, okind)
        VA = S.dram("s_va", [NB, T, 260], BF16, okind)
        SQKT = S.dram("s_sqkt", [NB, 64, 6, T], BF16, okind)
        SVA = S.dram("s_sva", [NB, T, 130], BF16, okind)
        OMIX = S.dram("s_omix", [NB, T, 512], F32, okind)
        OLRU = S.dram("s_olru", [NB, 512, T], BF16, okind)

        ARENA_BYTES = 190 * 1024
        arena_t = es.enter_context(nc.sbuf_tensor("arena", [128, ARENA_BYTES // 4], F32))
        arena = arena_t[:]
        PERS_BYTES = 12 * 1024
        P = Region(arena, 0, PERS_BYTES)
        R = Region(arena, PERS_BYTES, ARENA_BYTES)
        psum = []
        for i in range(4):
            t = es.enter_context(nc.psum_tensor("ps%d" % i, [128, 1024], F32))
            a = t[:]
            b0, b1 = Buf("ps%da" % i, excl=True), Buf("ps%db" % i, excl=True)
            psum.append((V(a, [b0, b1]), V(a[:, 0:512], [b0]), V(a[:, 512:1024], [b1])))
        PB = []
        for pr in psum:
            PB.append(pr[1])
            PB.append(pr[2])

        def pbf(bank):
            return V(bank.ap.bitcast(BF16), bank.bufs)

        identF = P.alloc("identF", [128], F32)
        identB = P.alloc("identB", [128], BF16)
        maskB = P.alloc("maskB", [2, 128], BF16)
        ones1 = P.alloc("ones1", [1], F32)
        modT = P.alloc("modT", [depth, 48, 3], F32)
        AB = P.alloc("AB", [depth * 3, 4, KD], F32)
        sT = P.alloc("sT", [KD, 3], F32)
        S.dma(identF, ident_d)
        S.copy(identB, identF, eng="dve")
        S.dma(sT, cT_d)
        S.memset(ones1, 1.0, eng="dve")
        R.reset()
        mtmp = R.alloc("mtmp", [2, 128], F32)
        S.dma(mtmp, masks_d)
        S.copy(maskB, mtmp, eng="dve")
        S.act(sT, sT, AF.Silu)
        wab = [R.alloc("wab%d" % i, [KD, 512], F32) for i in range(2)]
        brow = [R.alloc("brow%d" % i, [512], F32) for i in range(2)]
        grow = [R.alloc("grow%d" % i, [512], F32) for i in range(2)]
        badaT = R.alloc("badaT", [depth, 48], F32)
        gT = R.alloc("gT", [depth, 2, KD], F32)
        for l in range(depth):
            S.dma(badaT[:, l, :], b_adaT_d[l])
            S.dma(gT[:, l, 0, :], g1T_d[l])
            S.dma(gT[:, l, 1, :], g2T_d[l])
        it = 0
        for l in range(depth):
            for j in range(12):
                w = wab[it % 2]
                S.dma(w, w_ada_d[l][:, j * 512:(j + 1) * 512].re("(k p) n -> p k n", p=128))
                vec = j // 2
                if vec in (2, 5):
                    br = brow[it % 2]
                    S.dma(br[0:3, :], V(b_ada_d.ap[l, j * 512:(j + 1) * 512].partition_broadcast(3), []))
                    pm = PB[it % 2]
                    for k in range(KD):
                        S.mm(pm[0:3, :], sT[:, k, :], w[:, k, :], start=(k == 0), stop=(k == KD - 1))
                    gr = grow[it % 2]
                    S.tt(gr[0:3, :], pm[0:3, :], br[0:3, :], ALU.add)
                    S.dma(GATES[l, :, 0 if vec == 2 else 1, (j % 2) * 512:(j % 2 + 1) * 512], gr[0:3, :])
                else:
                    for m in range(4):
                        pm = PB[2 + (m % 2)]
                        for k in range(KD):
                            S.mm(pm[:, 0:3], w[:, k, m * 128:(m + 1) * 128], sT[:, k, :],
                                 start=(k == 0), stop=(k == KD - 1))
                        S.act(modT[:, l, j * 4 + m, :], pm[:, 0:3], AF.Identity,
                              bias=badaT[:, l, j * 4 + m:j * 4 + m + 1], scale=1.0)
                it += 1
            for r in range(3):
                ab = AB[:, l * 3 + r]
                S.ts(ab[:, 0, :], modT[:, l, 8:16, r], 1.0, None, ALU.add)
                S.tt(ab[:, 0, :], ab[:, 0, :], gT[:, l, 0, :], ALU.mult)
                S.copy(ab[:, 1, :], modT[:, l, 0:8, r])
                S.ts(ab[:, 2, :], modT[:, l, 32:40, r], 1.0, None, ALU.add)
                S.tt(ab[:, 2, :], ab[:, 2, :], gT[:, l, 1, :], ALU.mult)
                S.copy(ab[:, 3, :], modT[:, l, 24:32, r])
        S.barrier()
        if debug:
            print("ops after prologue", S.nadd)

        def x_src(b, l, t):
            if l == 0:
                if t < 2:
                    return ctx_d[b, t * 128:(t + 1) * 128, :]
                return x_d[b, (t - 2) * 128:(t - 1) * 128, :]
            return XS[b, t * 128:(t + 1) * 128, :]

        def norm_transpose(xt, A, Bc, hT_dst, tmpR, junk, pbanks, hTf_dst=None):
            ss = tmpR.alloc("ss", [1], F32)
            S.act(junk, xt, AF.Square, accum=ss)
            rstd = rms_rstd(S, tmpR, ss, D, 1, "x")
            xn = tmpR.alloc("xn", [D], F32)
            S.ts(xn, xt, rstd, None, ALU.mult, eng="pool")
            for half in range(2):
                pb = pbanks[half]
                for kk in range(4):
                    k = half * 4 + kk
                    S.transpose(pb[:, kk * 128:(kk + 1) * 128], xn[:, k * 128:(k + 1) * 128], identF)
                for kk in range(4):
                    k = half * 4 + kk
                    S.act(hT_dst[:, k, :], pb[:, kk * 128:(kk + 1) * 128], AF.Identity,
                          bias=Bc[:, k:k + 1], scale=A[:, k:k + 1])
                    if hTf_dst is not None:
                        S.ts(hTf_dst[:, k, :], pb[:, kk * 128:(kk + 1) * 128], A[:, k:k + 1], Bc[:, k:k + 1],
                             ALU.mult, ALU.add)

        def phase_A(b, l):
            R.reset()
            rowl, rowc = l * 3 + b, l * 3 + 2
            w_in = R.alloc("w_in", [KD, 1952], BF16)
            for k in range(KD):
                S.dma(w_in[:, k, :], w_in_d[l][k * 128:(k + 1) * 128, :], q="pool")
            wq_f = R.alloc("wq_f", [2, 384], F32)
            wkv_f = R.alloc("wkv_f", [512], F32)
            gq = R.alloc("gq", [3], F32)
            S.dma(wq_f, w_uq_d[l].re("(j p) n -> p j n", p=128))
            S.dma(wkv_f, w_ukv_d[l])
            S.dma(gq[:, 0:2], g_cqT_d[l])
            S.dma(gq[:, 2:3], g_ckvT_d[l])
            wq = R.alloc("wq", [2, 384], BF16)
            wkv = R.alloc("wkv", [512], BF16)
            for j in range(2):
                S.ts(wq[:, j, :], wq_f[:, j, :], gq[:, j:j + 1], None, ALU.mult)
            S.ts(wkv, wkv_f, gq[:, 2:3], None, ALU.mult)
            xt = [R.alloc("xt%d" % i, [D], F32) for i in range(2)]
            rp = [R.alloc("rp%d" % i, [96], F32) for i in range(2)]
            hTs = [R.alloc("hTs%d" % i, [KD, 512], BF16) for i in range(2)]
            junk = R.alloc("junk", [D], BF16)
            qa = [R.alloc("qa%d" % i, [4, 96], BF16) for i in range(2)]
            ka = [R.alloc("ka%d" % i, [4, 96], BF16) for i in range(2)]
            va = [R.alloc("va%d" % i, [4, 65], BF16) for i in range(2)]
            sqk = [R.alloc("sqk%d" % i, [6, 64], BF16) for i in range(2)]
            sva = [R.alloc("sva%d" % i, [2, 65], BF16) for i in range(2)]
            qTt = [R.alloc("qTt%d" % i, [4, 128], BF16) for i in range(2)]
            kTt = [R.alloc("kTt%d" % i, [4, 128], BF16) for i in range(2)]
            sqkT = [R.alloc("sqkT%d" % i, [6, 128], BF16) for i in range(2)]
            zlt = [R.alloc("zlt%d" % i, [512], F32) for i in range(2)]
            for i in range(2):
                S.memset(va[i][:, :, 64:65], 1.0)
                S.memset(sva[i][:, :, 64:65], 1.0)
            TR = Region(arena, R.off, ARENA_BYTES, cached=True)
            tiles = list(range(NT))
            S.dma(xt[0], x_src(b, l, 0))
            groups = [[0, 1]] + [list(range(2 + 4 * g, 2 + 4 * g + 4)) for g in range(NLT // 4)]
            gi = 0
            for grp in groups:
                hT = hTs[gi % 2]
                for ti, t in enumerate(grp):
                    TR.reset()
                    is_ctx = t < 2
                    A = AB[:, rowc if is_ctx else rowl]
                    x_t = xt[t % 2]
                    if t + 1 < NT:
                        S.dma(xt[(t + 1) % 2], x_src(b, l, t + 1))
                    if not is_ctx:
                        S.dma(rp[t % 2], rope_d[t - 2])
                    rpt = rp[t % 2]
                    hTt = hT[:, :, ti * 128:(ti + 1) * 128]
                    norm_transpose(x_t, A[:, 0, :], A[:, 1, :], hTt, TR, junk, (PB[0], PB[1]))
                    for k in range(KD):
                        S.mm(PB[2][:, 0:416], hTt[:, k, :], w_in[:, k, 0:416], start=(k == 0), stop=(k == KD - 1))
                    for k in range(KD):
                        S.mm(PB[3][:, 0:512], hTt[:, k, :], w_in[:, k, 416:928], start=(k == 0), stop=(k == KD - 1))
                    z0, z1 = PB[2], PB[3]
                    ss2 = TR.alloc("ss2", [2], F32)
                    S.act(junk[:, 0:256], z0[:, 0:256], AF.Square, accum=ss2[:, 0:1])
                    S.act(junk[:, 256:384], z0[:, 256:384], AF.Square, accum=ss2[:, 1:2])
                    rs2 = TR.alloc("rs2", [2], F32)
                    S.act(rs2[:, 0:1], ss2[:, 0:1], AF.Sqrt, bias=EPS, scale=1.0 / 256)
                    S.act(rs2[:, 1:2], ss2[:, 1:2], AF.Sqrt, bias=EPS, scale=1.0 / 128)
                    S.recip(rs2, rs2)
                    cn = TR.alloc("cn", [384], BF16)
                    S.act(cn[:, 0:256], z0[:, 0:256], AF.Identity, scale=rs2[:, 0:1])
                    S.act(cn[:, 256:384], z0[:, 256:384], AF.Identity, scale=rs2[:, 1:2])
                    pT = pbf(PB[4])
                    for j in range(3):
                        S.transpose(pT[:, j * 128:(j + 1) * 128], cn[:, j * 128:(j + 1) * 128], identB)
                    cT = TR.alloc("cTt", [3, 128], BF16)
                    S.copy(cT, pT[:, 0:384].re("p (j t) -> p j t", j=3), eng="dve")
                    pq, pkv = PB[5], PB[6]
                    for j in range(2):
                        S.mm(pq[:, 0:384], cT[:, j, :], wq[:, j, :], start=(j == 0), stop=(j == 1))
                    S.mm(pkv[:, 0:512], cT[:, 2, :], wkv, start=True, stop=True)
                    kr = TR.alloc("kr", [32], F32)
                    q_a, k_a, v_a, sqk_a, sv_a = qa[t % 2], ka[t % 2], va[t % 2], sqk[t % 2], sva[t % 2]
                    pq3 = pq[:, 0:384].re("p (h d) -> p h d", h=4)
                    S.copy(q_a[:, :, 0:64], pq3[:, :, 0:64], eng="act")
                    S.copy(k_a[:, :, 0:64], pkv[:, 0:256].re("p (h d) -> p h d", h=4), eng="act")
                    S.copy(v_a[:, :, 0:64], pkv[:, 256:512].re("p (h d) -> p h d", h=4), eng="act")
                    S.copy(sv_a[:, :, 0:64], z1[:, 384:512].re("p (h d) -> p h d", h=2), eng="act")
                    z1h = z1[:, 0:384].re("p (h d) -> p h d", h=6)
                    if is_ctx:
                        S.copy(q_a[:, :, 64:96], pq3[:, :, 64:96], eng="dve")
                        S.copy(kr, z0[:, 384:416], eng="dve")
                        S.copy(sqk_a, z1h, eng="act")
                    else:
                        def rope(dst1, dst2, X1, X2, Ct, St, shape, tag):
                            t1 = TR.alloc("r1" + tag, shape, F32)
                            t2 = TR.alloc("r2" + tag, shape, F32)
                            S.tt(t1, X1, Ct, ALU.mult)
                            S.tt(t2, X2, St, ALU.mult)
                            S.tt(dst1, t1, t2, ALU.subtract)
                            t3 = TR.alloc("r3" + tag, shape, F32)
                            t4 = TR.alloc("r4" + tag, shape, F32)
                            S.tt(t3, X2, Ct, ALU.mult)
                            S.tt(t4, X1, St, ALU.mult)
                            S.tt(dst2, t3, t4, ALU.add)
                        Cm, Sm = rpt[:, 0:16], rpt[:, 16:32]
                        Cs, Ss = rpt[:, 32:64], rpt[:, 64:96]
                        rope(q_a[:, :, 64:80], q_a[:, :, 80:96], pq3[:, :, 64:80], pq3[:, :, 80:96],
                             Cm.bcast(1, [128, 4, 16]), Sm.bcast(1, [128, 4, 16]), [4, 16], "q")
                        rope(kr[:, 0:16], kr[:, 16:32], z0[:, 384:400], z0[:, 400:416], Cm, Sm, [16], "k")
                        rope(sqk_a[:, :, 0:32], sqk_a[:, :, 32:64], z1h[:, :, 0:32], z1h[:, :, 32:64],
                             Cs.bcast(1, [128, 6, 32]), Ss.bcast(1, [128, 6, 32]), [6, 32], "s")
                    S.copy(k_a[:, :, 64:96], kr.bcast(1, [128, 4, 32]), eng="dve")
                    pTq, pTk, pTs = pbf(PB[4]), pbf(PB[7]), pbf(PB[5])
                    for h in range(4):
                        S.transpose(pTq[0:96, h * 128:(h + 1) * 128], q_a[:, h, :], identB)
                    qT_t = qTt[t % 2]
                    S.copy(qT_t[0:96], pTq[0:96, 0:512].re("p (h t) -> p h t", h=4), eng="dve")
                    for h in range(4):
                        S.transpose(pTk[0:96, h * 128:(h + 1) * 128], k_a[:, h, :], identB)
                    kT_t = kTt[t % 2]
                    S.copy(kT_t[0:96], pTk[0:96, 0:512].re("p (h t) -> p h t", h=4), eng="act")
                    for h in range(6):
                        S.transpose(pTs[0:64, h * 128:(h + 1) * 128], sqk_a[:, h, :], identB)
                    sT_t = sqkT[t % 2]
                    S.copy(sT_t[0:64], pTs[0:64, 0:768].re("p (h t) -> p h t", h=6), eng="dve")
                    tok = slice(t * 128, (t + 1) * 128)
                    S.dma(QT[b, :, :, tok], qT_t[0:96])
                    S.dma(KT[b, :, :, tok], kT_t[0:96])
                    S.dma(SQKT[b, :, :, tok], sT_t[0:64])
                    S.dma(VA[b, tok, :], v_a.re("p h d -> p (h d)"))
                    S.dma(SVA[b, tok, :], sv_a.re("p h d -> p (h d)"))
                ntok = 128 * len(grp)
                tok0 = grp[0] * 128
                for m in range(8):
                    pz = PB[m % 2]
                    for k in range(KD):
                        S.mm(pz[:, 0:ntok], w_in[:, k, 928 + m * 128:928 + (m + 1) * 128], hT[:, k, 0:ntok],
                             start=(k == 0), stop=(k == KD - 1))
                    zt = zlt[m % 2]
                    S.copy(zt[:, 0:ntok], pz[:, 0:ntok], eng=("act" if m % 2 else "dve"))
                    S.dma(ZL[b, m * 128:(m + 1) * 128, tok0:tok0 + ntok], zt[:, 0:ntok])
                gi += 1
            S.barrier()

        def phase_B(b, l):
            R.reset()
            kT = R.alloc("kT", [4, T], BF16)
            vA = R.alloc("vA", [NT, 260], BF16)
            for h in range(4):
                S.dma(kT[0:96, h, :], KT[b, :, h, :])
            for c0 in range(0, NT, 8):
                c1 = min(NT, c0 + 8)
                S.dma(vA[:, c0:c1, :], VA[b, c0 * 128:c1 * 128, :].re("(c p) n -> p c n", p=128))
            qTb = [R.alloc("qTb%d" % i, [4, 512], BF16) for i in range(2)]
            pt = [R.alloc("pt%d" % i, [512], BF16) for i in range(4)]
            usb = [R.alloc("usb%d" % i, [512], F32) for i in range(2)]
            ot = [R.alloc("ot%d" % i, [4, 64], F32) for i in range(2)]
            rc = [R.alloc("rc%d" % i, [4], F32) for i in range(2)]
            chunks = []
            if l < depth - 1:
                chunks.append((0, 256, 2))
            for c in range(SEQ // 512):
                chunks.append((256 + c * 512, 512, NT))
            Sb = [PB[0], PB[1], PB[2], PB[3]]
            Ub = [PB[4], PB[5]]
            Tb = [PB[6], PB[7]]
            S.dma(qTb[0][0:96, :, 0:chunks[0][1]], QT[b, :, :, chunks[0][0]:chunks[0][0] + chunks[0][1]])
            cnt = 0
            ui = 0
            for ci, (q0, nq, nkc) in enumerate(chunks):
                qt = qTb[ci % 2]
                if ci + 1 < len(chunks):
                    nq0, nnq, _ = chunks[ci + 1]
                    S.dma(qTb[(ci + 1) % 2][0:96, :, 0:nnq], QT[b, :, :, nq0:nq0 + nnq])
                for h in range(4):
                    U = Ub[ui % 2]
                    def score(c):
                        sb = Sb[(cnt + c) % 4]
                        S.mm(sb[:, 0:nq], kT[0:96, h, c * 128:(c + 1) * 128], qt[0:96, h, 0:nq])
                        p = pt[(cnt + c) % 4]
                        S.act(p[:, 0:nq], sb[:, 0:nq], AF.Exp, scale=MLA_SCALE)
                        return p
                    ps = {0: score(0)}
                    if nkc > 1:
                        ps[1] = score(1)
                    for c in range(nkc):
                        if c + 2 < nkc:
                            ps[c + 2] = score(c + 2)
                        S.mm(U[0:65, 0:nq], vA[:, c, h * 65:(h + 1) * 65], ps[c][:, 0:nq],
                             start=(c == 0), stop=(c == nkc - 1))
                        del ps[c]
                    cnt += nkc
                    us = usb[ui % 2]
                    S.copy(us[0:65, 0:nq], U[0:65, 0:nq], eng="dve")
                    tb = Tb[ui % 2]
                    nsub = nq // 128
                    for s in range(nsub):
                        S.transpose(tb[:, s * 65:(s + 1) * 65], us[0:65, s * 128:(s + 1) * 128], identF[0:65, 0:65])
                    t3 = tb[:, 0:nsub * 65].re("p (s d) -> p s d", s=nsub)
                    r = rc[ui % 2]
                    S.recip(r[:, 0:nsub], t3[:, :, 64])
                    o = ot[ui % 2]
                    S.tt(o[:, 0:nsub, :], t3[:, :, 0:64], r[:, 0:nsub].bcast(2, [128, nsub, 64]), ALU.mult)
                    S.dma(OMIX[b, q0:q0 + nq, h * 64:(h + 1) * 64].re("(s p) d -> p s d", p=128), o[:, 0:nsub, :])
                    ui += 1
            S.barrier()

        def phase_B2(b, l):
            R.reset()
            sT_ = R.alloc("sqkT_all", [6, T], BF16)
            svA = R.alloc("svA", [NT, 130], BF16)
            for h in range(6):
                S.dma(sT_[0:64, h, :], SQKT[b, :, h, :])
            for c0 in range(0, NT, 8):
                c1 = min(NT, c0 + 8)
                S.dma(svA[:, c0:c1, :], SVA[b, c0 * 128:c1 * 128, :].re("(c p) n -> p c n", p=128))
            esink = R.alloc("esink", [4], F32)
            S.dma(esink, V(sink_d.ap[l].partition_broadcast(128), []))
            S.act(esink, esink, AF.Exp)
            pt = [R.alloc("spt%d" % i, [2, 128], BF16) for i in range(4)]
            usb = [R.alloc("susb%d" % i, [256], F32) for i in range(2)]
            ot = [R.alloc("sot%d" % i, [2, 64], F32) for i in range(2)]
            rc = [R.alloc("src%d" % i, [2], F32) for i in range(2)]
            Sb = [PB[0], PB[1], PB[2], PB[3]]
            Ub = [PB[4], PB[5]]
            Tb = [PB[6], PB[7]]
            qtiles = list(range(2, NT)) if l == depth - 1 else list(range(NT))
            cnt = 0
            ui = 0
            for tq in qtiles:
                if tq < 2:
                    keys = [(0, None), (1, None)]
                else:
                    keys = [(0, None), (1, None)]
                    if tq - 1 >= 2:
                        keys.append((tq - 1, 0))
                    keys.append((tq, None))
                    if tq + 1 < NT:
                        keys.append((tq + 1, 1))
                for g in range(2):
                    U = Ub[ui % 2]
                    q = sT_[0:64, 2 * g:2 * g + 2, tq * 128:(tq + 1) * 128]
                    plist = []
                    for (kc, mk) in keys:
                        sb = Sb[cnt % 4]
                        S.mm(sb[:, 0:256].re("p (h t) -> p h t", h=2), sT_[0:64, 4 + g, kc * 128:(kc + 1) * 128], q)
                        p = pt[cnt % 4]
                        S.act(p, sb[:, 0:256].re("p (h t) -> p h t", h=2), AF.Exp, scale=SWA_SCALE)
                        if mk is not None:
                            S.tt(p, p, maskB[:, mk, :].bcast(1, [128, 2, 128]), ALU.mult, eng="pool")
                        plist.append(p)
                        cnt += 1
                        if len(plist) >= 2:
                            idx = len(plist) - 2
                            kc2 = keys[idx][0]
                            S.mm(U[0:65, 0:256], svA[:, kc2, g * 65:(g + 1) * 65], plist[idx].re("p h t -> p (h t)"),
                                 start=(idx == 0), stop=False)
                    idx = len(plist) - 1
                    S.mm(U[0:65, 0:256], svA[:, keys[idx][0], g * 65:(g + 1) * 65], plist[idx].re("p h t -> p (h t)"),
                         start=(idx == 0), stop=True)
                    us = usb[ui % 2]
                    S.copy(us[0:65, :], U[0:65, 0:256], eng="dve")
                    tb = Tb[ui % 2]
                    for s in range(2):
                        S.transpose(tb[:, s * 65:(s + 1) * 65], us[0:65, s * 128:(s + 1) * 128], identF[0:65, 0:65])
                    t3 = tb[:, 0:130].re("p (s d) -> p s d", s=2)
                    r = rc[ui % 2]
                    S.tt(r, t3[:, :, 64], esink[:, 2 * g:2 * g + 2], ALU.add)
                    S.recip(r, r)
                    o = ot[ui % 2]
                    S.tt(o, t3[:, :, 0:64], r.bcast(2, [128, 2, 64]), ALU.mult)
                    S.dma(OMIX[b, tq * 128:(tq + 1) * 128, 256 + g * 128:256 + (g + 1) * 128], o.re("p s d -> p (s d)"))
                    ui += 1
            S.barrier()

        def phase_C(b, l):
            R.reset()
            ZW = T + 8
            CO, LO = 2, 261
            wst = R.alloc("wst", [4, 4, 128], F32)
            S.memset(wst, 0.0, eng="dve")
            for ty, wd_ in enumerate((lru_wa_d, lru_wx_d)):
                for d in range(2):
                    for half in range(2):
                        src = wd_[l, d].re("(m two) c e -> two c m e", two=2)[half]
                        S.dma(wst[half * 64:(half + 1) * 64, ty * 2 + d, :, half * 64:(half + 1) * 64], src)
            wbd = R.alloc("wbd", [4, 4, 128], BF16)
            S.copy(wbd, wst, eng="dve")
            vT = R.alloc("vT", [3, 2, 4], F32)
            S.dma(vT, lru_vT_d[l])
            cw = R.alloc("cw", [4, 4], F32)
            cb = R.alloc("cb", [4], F32)
            S.dma(cw, conv_wT_d[l])
            S.dma(cb, conv_bT_d[l])
            cneg = R.alloc("cneg", [2, 4], F32)
            S.act(cneg, vT[:, 2], AF.Exp, scale=-1.0)
            S.act(cneg, cneg, AF.Ln, bias=1.0, scale=1.0)
            S.ts(cneg, cneg, -8.0, None, ALU.mult)
            cnh = R.alloc("cnh", [2, 4], F32)
            S.ts(cnh, cneg, 0.5, None, ALU.mult)
            zb = R.alloc("zb", [ZW], F32)
            zbd = zb.sub(Buf("zbdata"))
            S.memset(V(zb.ap, zb.bufs + zbd.bufs), 0.0, eng="pool")
            u = R.alloc("u", [T], F32)
            ub = R.alloc("ub", [T], BF16)
            rr = R.alloc("rr", [T], F32)
            ii = R.alloc("ii", [T], F32)
            tq_ = R.alloc("tq", [T], F32)
            hf = R.alloc("hf", [T], F32)
            hb = R.alloc("hb", [T], F32)
            gz = R.alloc("gz", [T], F32)
            ob = R.alloc("ob", [T], BF16)
            tchunks = [(0, 256)] + [(256 + c * 512, 512) for c in range(SEQ // 512)]
            for m in range(4):
                S.dma(zbd[:, CO:CO + 256], ZL[b, m * 128:(m + 1) * 128, 0:256])
                S.dma(zbd[:, LO:LO + SEQ], ZL[b, m * 128:(m + 1) * 128, 256:T])
                S.dma(gz, ZL[b, 512 + m * 128:512 + (m + 1) * 128, :])
                zr = V(zb.ap, zb.bufs + zbd.bufs)
                for (o0, o1, n) in ((0, 0, 256), (256, 259, SEQ)):
                    S.ts(u[:, o0:o0 + n], zr[:, o1:o1 + n], cw[:, m, 0:1], cb[:, m:m + 1], ALU.mult, ALU.add)
                    for tap in range(1, 4):
                        S.stt(u[:, o0:o0 + n], zr[:, o1 + tap:o1 + tap + n], cw[:, m, tap:tap + 1], u[:, o0:o0 + n],
                              ALU.mult, ALU.add)
                S.copy(ub, u, eng="pool")
                S.act(gz, gz, AF.Gelu_apprx_tanh)
                for d in range(2):
                    for ci, (t0, n) in enumerate(tchunks):
                        pa, px = PB[(ci % 2) * 2], PB[(ci % 2) * 2 + 1]
                        S.mm(pa[:, 0:n], wbd[:, 0 * 2 + d, m, :], ub[:, t0:t0 + n])
                        S.mm(px[:, 0:n], wbd[:, 1 * 2 + d, m, :], ub[:, t0:t0 + n])
                        S.act(rr[:, t0:t0 + n], pa[:, 0:n], AF.Sigmoid, bias=vT[:, 0, d, m:m + 1], scale=1.0)
                        S.act(ii[:, t0:t0 + n], px[:, 0:n], AF.Sigmoid, bias=vT[:, 1, d, m:m + 1], scale=1.0)
                    S.act(tq_, rr, AF.Tanh, scale=cnh[:, d, m:m + 1])
                    S.act(rr, rr, AF.Exp, scale=cneg[:, d, m:m + 1])
                    S.act(tq_, tq_, AF.Sqrt, scale=-1.0)
                    S.stt(tq_, rr, 1.0, tq_, ALU.add, ALU.mult)
                    S.tt(ii, ii, u, ALU.mult, eng="pool")
                    S.tt(ii, ii, tq_, ALU.mult)
                    if d == 0:
                        S.scan(hf, rr, ii, 0.0)
                    else:
                        S.scan(hb[:, 0:256][:, ::-1], rr[:, 0:256][:, ::-1], ii[:, 0:256][:, ::-1], 0.0)
                        S.scan(hb[:, 256:T][:, ::-1], rr[:, 256:T][:, ::-1], ii[:, 256:T][:, ::-1], hb[:, 0:1])
                S.tt(hf, hf, hb, ALU.add)
                S.tt(ob, hf, gz, ALU.mult)
                S.dma(OLRU[b, m * 128:(m + 1) * 128, :], ob)
            S.barrier()

        def phase_DE(b, l, tiles):
            R.reset()
            last = l == depth - 1
            ntl = len(tiles)
            ntok = ntl * 128
            H2T = R.alloc("H2T", [KD, ntok], BF16)
            COMB = R.alloc("COMB", [ntl, 32], F32)
            mark = R.off
            wo_f = R.alloc("wo_f", [KD, D], F32)
            S.dma(wo_f, w_out_d[l].re("(k p) n -> p k n", p=128))
            gg = R.alloc("gg", [KD], F32)
            S.dma(gg, g_grpT_d[l])
            wo = R.alloc("wo", [KD, D], BF16)
            for k in range(KD):
                S.ts(wo[:, k, :], wo_f[:, k, :], gg[:, k:k + 1], None, ALU.mult, eng=("pool" if k % 2 else "dve"))
            wrt = R.alloc("wrt", [KD, 36], F32)
            S.dma(wrt, wr_d[l].re("(k p) n -> p k n", p=128))
            rbt = R.alloc("rbt", [36], F32)
            S.dma(rbt, V(rb_d.ap[l].partition_broadcast(128), []))
            G1 = [R.alloc("G1_%d" % i, [D], F32) for i in range(2)]
            S.dma(G1[0], V(GATES.ap[l, b, 0].partition_broadcast(128), []))
            S.dma(G1[1], V(GATES.ap[l, 2, 0].partition_broadcast(128), []))
            NBUF = 4
            xt = [R.alloc("dxt%d" % i, [D], F32) for i in range(NBUF)]
            om = [R.alloc("om%d" % i, [512], F32) for i in range(NBUF)]
            ol = [R.alloc("ol%d" % i, [4, 128], BF16) for i in range(NBUF)]
            LG = R.alloc("LG", [ntl, 36], F32)
            rowl, rowc = l * 3 + b, l * 3 + 2
            TRs = [Region(arena, R.off + pp * 24576, R.off + (pp + 1) * 24576, cached=True) for pp in range(2)]
            RB = Region(arena, R.off + 2 * 24576, ARENA_BYTES, cached=True)

            def loads(i):
                t = tiles[i]
                tok = slice(t * 128, (t + 1) * 128)
                S.dma(xt[i % NBUF], x_src(b, l, t))
                S.dma(om[i % NBUF], OMIX[b, tok, :])
                S.dma(ol[i % NBUF], OLRU[b, :, tok].re("(m p) t -> p m t", p=128))

            def d_tile(i, t):
                pp = i % 2
                TR = TRs[pp]
                Q = PB[4 * pp:4 * pp + 4]
                PA, PL = psum[2 * pp], psum[2 * pp + 1]
                is_ctx = t < 2
                tok = slice(t * 128, (t + 1) * 128)
                if i + 2 < ntl:
                    loads(i + 2)
                x_t, o_m, o_l = xt[i % NBUF], om[i % NBUF], ol[i % NBUF]
                junk = TR.alloc("djunk", [D], BF16)
                ss2 = TR.alloc("dss2", [2], F32)
                S.act(junk[:, 0:256], o_m[:, 0:256], AF.Square, accum=ss2[:, 0:1])
                S.act(junk[:, 256:512], o_m[:, 256:512], AF.Square, accum=ss2[:, 1:2])
                sq = TR.alloc("sq", [4, 128], F32)
                S.tt(sq, o_l, o_l, ALU.mult, eng="pool")
                yield
                rs2 = TR.alloc("rstd_g", [2], F32)
                S.act(rs2, ss2, AF.Sqrt, bias=EPS, scale=1.0 / 256)
                pss = Q[1]
                for m in range(4):
                    S.mm(pss[:, 0:1], sq[:, m, :], ones1, start=(m == 0), stop=(m == 3))
                yield
                S.recip(rs2, rs2)
                rsl = TR.alloc("rsl", [1], F32)
                S.act(rsl, pss[:, 0:1], AF.Sqrt, bias=EPS, scale=1.0 / 512)
                yield
                on = TR.alloc("on", [512], BF16)
                S.act(on[:, 0:256], o_m[:, 0:256], AF.Identity, scale=rs2[:, 0:1])
                S.act(on[:, 256:512], o_m[:, 256:512], AF.Identity, scale=rs2[:, 1:2])
                S.recip(rsl, rsl)
                yield
                pT = pbf(Q[0])
                for j in range(4):
                    S.transpose(pT[:, j * 128:(j + 1) * 128], on[:, j * 128:(j + 1) * 128], identB)
                yield
                mT = TR.alloc("mT", [4, 128], BF16)
                S.copy(mT, pT[:, 0:512].re("p (j t) -> p j t", j=4), eng="dve")
                yield
                for nh in range(2):
                    for m in range(4):
                        S.mm(PL[nh + 1], o_l[:, m, :], wo[:, 4 + m, nh * 512:(nh + 1) * 512], start=(m == 0), stop=(m == 3))
                for nh in range(2):
                    for j in range(4):
                        S.mm(PA[nh + 1], mT[:, j, :], wo[:, j, nh * 512:(nh + 1) * 512], start=(j == 0), stop=(j == 3))
                yield
                tA = TR.alloc("tA", [D], F32)
                S.copy(tA, PA[0], eng="act")
                yield
                S.stt(tA, PL[0], rsl, tA, ALU.mult, ALU.add)
                yield
                S.tt(tA, tA, G1[1 if is_ctx else 0], ALU.mult, eng="pool")
                yield
                S.tt(x_t, x_t, tA, ALU.add)
                yield
                S.dma(X1S[b, tok, :], x_t)
                ss = TR.alloc("ss", [1], F32)
                S.act(junk, x_t, AF.Square, accum=ss)
                yield
                rstd = TR.alloc("rstd_x", [1], F32)
                S.act(rstd, ss, AF.Sqrt, bias=EPS, scale=1.0 / D)
                yield
                S.recip(rstd, rstd)
                yield
                xn = TR.alloc("xn", [D], F32)
                S.ts(xn, x_t, rstd, None, ALU.mult, eng="pool")
                yield
                A = AB[:, rowc if is_ctx else rowl]
                h2f = TR.alloc("h2f", [KD, 128], F32)
                for half in range(2):
                    pb = Q[half]
                    for kk in range(4):
                        k = half * 4 + kk
                        S.transpose(pb[:, kk * 128:(kk + 1) * 128], xn[:, k * 128:(k + 1) * 128], identF)
                yield
                for half in range(2):
                    pb = Q[half]
                    for kk in range(4):
                        k = half * 4 + kk
                        S.act(h2f[:, k, :], pb[:, kk * 128:(kk + 1) * 128], AF.Identity,
                              bias=A[:, 3, k:k + 1], scale=A[:, 2, k:k + 1])
                yield
                S.copy(H2T[:, :, i * 128:(i + 1) * 128], h2f, eng="pool")
                pr = Q[2]
                for k in range(KD):
                    S.mm(pr[:, 0:36], h2f[:, k, :], wrt[:, k, :], start=(k == 0), stop=(k == KD - 1))
                yield
                S.tt(LG[:, i, :], pr[:, 0:36], rbt, ALU.add)

            for i in range(min(2, ntl)):
                loads(i)
            active = []
            nxt = 0
            while nxt < ntl or active:
                while len(active) < 2 and nxt < ntl:
                    active.append(d_tile(nxt, tiles[nxt]))
                    nxt += 1
                for g_ in list(active):
                    try:
                        next(g_)
                    except StopIteration:
                        active.remove(g_)
            lgG = LG[:, :, 0:4]
            lgE = LG[:, :, 4:36].re("p t (g e) -> p t g e", g=4)
            gmax = RB.alloc("gmax", [ntl], F32)
            S.reduce(gmax, lgG, ALU.max)
            mg = RB.alloc("mg", [ntl, 4], F32)
            S.tt(mg, lgG, gmax.bcast(2, [128, ntl, 4]), ALU.is_equal)
            eg = RB.alloc("eg", [ntl, 4], F32)
            S.tt(eg, lgG, gmax.bcast(2, [128, ntl, 4]), ALU.subtract)
            S.act(eg, eg, AF.Exp)
            pgt = RB.alloc("pgt", [ntl], F32)
            S.reduce(pgt, eg, ALU.add)
            S.recip(pgt, pgt)
            les = RB.alloc("les", [ntl, 8], F32)
            tmp8 = RB.alloc("tmp8", [ntl, 8], F32)
            S.tt(les, lgE[:, :, 0, :], mg[:, :, 0].bcast(2, [128, ntl, 8]), ALU.mult)
            for g in range(1, 4):
                S.tt(tmp8, lgE[:, :, g, :], mg[:, :, g].bcast(2, [128, ntl, 8]), ALU.mult)
                S.tt(les, les, tmp8, ALU.add)
            m1v = RB.alloc("m1v", [ntl], F32)
            S.reduce(m1v, les, ALU.max)
            k1 = RB.alloc("k1", [ntl, 8], F32)
            S.tt(k1, les, m1v.bcast(2, [128, ntl, 8]), ALU.is_equal)
            les2 = RB.alloc("les2", [ntl, 8], F32)
            S.stt(les2, k1, -1e30, les, ALU.mult, ALU.add)
            m2v = RB.alloc("m2v", [ntl], F32)
            S.reduce(m2v, les2, ALU.max)
            k2 = RB.alloc("k2", [ntl, 8], F32)
            S.tt(k2, les2, m2v.bcast(2, [128, ntl, 8]), ALU.is_equal)
            e2 = RB.alloc("e2", [ntl], F32)
            S.tt(e2, m2v, m1v, ALU.subtract)
            S.act(e2, e2, AF.Exp)
            w1 = RB.alloc("w1", [ntl], F32)
            S.ts(w1, e2, 1.0, None, ALU.add)
            S.recip(w1, w1)
            S.tt(w1, w1, pgt, ALU.mult)
            w2 = RB.alloc("w2", [ntl], F32)
            S.tt(w2, w1, e2, ALU.mult)
            cwt = RB.alloc("cwt", [ntl, 8], F32)
            S.tt(cwt, k1, w1.bcast(2, [128, ntl, 8]), ALU.mult)
            S.tt(tmp8, k2, w2.bcast(2, [128, ntl, 8]), ALU.mult)
            S.tt(cwt, cwt, tmp8, ALU.add)
            for g in range(4):
                S.tt(COMB[:, :, g * 8:(g + 1) * 8], cwt, mg[:, :, g].bcast(2, [128, ntl, 8]), ALU.mult)
            S.barrier()
            R.off = mark
            Y = R.alloc("Y", [ntl, D], F32)
            wg = [R.alloc("wg%d" % i, [KD, DEXP], BF16) for i in range(2)]
            wu = [R.alloc("wu%d" % i, [KD, DEXP], BF16) for i in range(2)]
            wd = [R.alloc("wd%d" % i, [2, D], BF16) for i in range(2)]
            sgl = [R.alloc("sgl%d" % i, [512], F32) for i in range(2)]
            aT = [R.alloc("aT%d" % i, [2, 512], BF16) for i in range(2)]
            Yb = [Y[:, i, :].sub(Buf("Y%d" % i)) for i in range(ntl)]

            def wload(e):
                S.dma(wg[e % 2], weg_d[l, e].re("(k p) n -> p k n", p=128), q="pool")
                S.dma(wu[e % 2], weu_d[l, e].re("(k p) n -> p k n", p=128), q="pool")
                S.dma(wd[e % 2], wed_d[l, e].re("(k p) n -> p k n", p=128), q="pool")
            wload(0)
            chunks = []
            c0 = 0
            while c0 < ntok:
                n = min(512, ntok - c0)
                chunks.append((c0, n))
                c0 += n
            it = 0
            for e in range(NEXP):
                if e + 1 < NEXP:
                    wload(e + 1)
                g_w, u_w, d_w = wg[e % 2], wu[e % 2], wd[e % 2]
                for (c0, n) in chunks:
                    a_t = aT[it % 2]
                    for half in range(2):
                        pg_, pu_ = PB[half * 2], PB[half * 2 + 1]
                        for k in range(KD):
                            S.mm(pg_[:, 0:n], g_w[:, k, half * 128:(half + 1) * 128], H2T[:, k, c0:c0 + n],
                                 start=(k == 0), stop=(k == KD - 1))
                        for k in range(KD):
                            S.mm(pu_[:, 0:n], u_w[:, k, half * 128:(half + 1) * 128], H2T[:, k, c0:c0 + n],
                                 start=(k == 0), stop=(k == KD - 1))
                        sg_ = sgl[half]
                        S.act(sg_[:, 0:n], pg_[:, 0:n], AF.Silu)
                        S.tt(a_t[:, half, 0:n], sg_[:, 0:n], pu_[:, 0:n], ALU.mult)
                    for s in range(n // 128):
                        ti = c0 // 128 + s
                        pd = psum[2 + (ti % 2)]
                        for nh in range(2):
                            for half in range(2):
                                S.mm(pd[nh + 1], a_t[:, half, s * 128:(s + 1) * 128], d_w[:, half, nh * 512:(nh + 1) * 512],
                                     start=(half == 0), stop=(half == 1))
                        if e == 0:
                            S.ts(Yb[ti], pd[0], COMB[:, ti, e:e + 1], None, ALU.mult)
                        else:
                            S.stt(Yb[ti], pd[0], COMB[:, ti, e:e + 1], Yb[ti], ALU.mult, ALU.add)
                    it += 1
            G2 = [R.alloc("G2_%d" % i, [D], F32) for i in range(2)]
            S.dma(G2[0], V(GATES.ap[l, b, 1].partition_broadcast(128), []))
            S.dma(G2[1], V(GATES.ap[l, 2, 1].partition_broadcast(128), []))
            x1 = [R.alloc("x1_%d" % i, [D], F32) for i in range(2)]
            ejunk = R.alloc("ejunk", [D], BF16)
            if last:
                GF = R.alloc("GF", [D], F32)
                S.dma(GF, V(gfin_d.ap.partition_broadcast(128), []))
            TR = Region(arena, R.off, ARENA_BYTES, cached=True)
            S.dma(x1[0], X1S[b, tiles[0] * 128:(tiles[0] + 1) * 128, :])
            for i, t in enumerate(tiles):
                TR.reset()
                tok = slice(t * 128, (t + 1) * 128)
                if i + 1 < ntl:
                    S.dma(x1[(i + 1) % 2], X1S[b, tiles[i + 1] * 128:(tiles[i + 1] + 1) * 128, :])
                x_t = x1[i % 2]
                S.tt(Yb[i], Yb[i], G2[1 if t < 2 else 0], ALU.mult, eng="pool")
                S.tt(x_t, x_t, Yb[i], ALU.add)
                if not last:
                    S.dma(XS[b, tok, :], x_t)
                else:
                    ss = TR.alloc("ess", [1], F32)
                    S.act(ejunk, x_t, AF.Square, accum=ss)
                    rstd = rms_rstd(S, TR, ss, D, 1, "f")
                    S.stt(x_t, x_t, rstd, GF, ALU.mult, ALU.mult)
                    S.dma(out_d[b, (t - 2) * 128:(t - 1) * 128, :], x_t, is_out=True)
            S.barrier()

        for b in range(NB):
            for l in range(depth if nlayers is None else nlayers):
                if "A" in plan:
                    phase_A(b, l)
                if "B" in plan:
                    phase_B(b, l)
                if "S" in plan:
                    phase_B2(b, l)
                if "C" in plan:
                    phase_C(b, l)
                if "D" not in plan:
                    continue
                if l == depth - 1:
                    tl = list(range(2, NT))
                else:
                    tl = list(range(NT))
                nblk = (len(tl) + 16) // 17
                per = (len(tl) + nblk - 1) // nblk
                for i in range(nblk):
                    blk = tl[i * per:(i + 1) * per]
                    if blk:
                        phase_DE(b, l, blk)
        if debug:
            print("total ops", S.nadd)
        S.emit()
    return nc


def _perm_rot(nheads, hd, base=0):
    q = hd // 4
    idx = []
    for h in range(nheads):
        o = base + h * hd
        idx += list(range(o, o + q)) + list(range(o + 2 * q, o + 3 * q)) + list(range(o + q, o + 2 * q)) + \
            list(range(o + 3 * q, o + 4 * q))
    return idx


def _chan_major(v, nch):
    return np.ascontiguousarray(np.swapaxes(v.reshape(v.shape[:-1] + (nch, 128)), -1, -2))


def host_shared(inp, SEQ):
    f = np.float32
    depth = inp["w_ada"].shape[0]
    sh = {}
    sh["w_ada"] = np.ascontiguousarray(inp["w_ada"], f)
    sh["b_ada"] = np.ascontiguousarray(inp["b_ada"], f)
    sh["b_adaT"] = _chan_major(inp["b_ada"].astype(f), 48)
    sh["g1T"] = _chan_major(inp["g_norm1"].astype(f), 8)
    sh["g2T"] = _chan_major(inp["g_norm2"].astype(f), 8)
    cols = list(range(0, 384)) + _perm_rot(1, 32, 384) + _perm_rot(4, 64, 416) + _perm_rot(2, 64, 672) + \
        list(range(800, 1952))
    sh["w_in_p"] = np.ascontiguousarray(inp["w_in"][:, :, cols], f)
    sh["g_cqT"] = _chan_major(inp["g_cq"].astype(f), 2)
    sh["g_ckvT"] = _chan_major(inp["g_ckv"].astype(f), 1)
    qcols = []
    for h in range(4):
        qcols += list(range(h * 96, h * 96 + 64)) + _perm_rot(1, 32, h * 96 + 64)
    sh["w_uq_p"] = np.ascontiguousarray(inp["w_uq"][:, :, qcols], f)
    kvcols = [h * 128 + i for h in range(4) for i in range(64)] + [h * 128 + 64 + i for h in range(4) for i in range(64)]
    sh["w_ukv_p"] = np.ascontiguousarray(inp["w_ukv"][:, :, kvcols], f)
    sh["sink"] = np.ascontiguousarray(inp["swa_sink"], f)
    cw = inp["conv_w"].astype(f)
    sh["conv_wT"] = np.ascontiguousarray(np.transpose(cw.reshape(depth, 4, 4, 128), (0, 3, 2, 1)))
    sh["conv_bT"] = _chan_major(inp["conv_b"].astype(f), 4)
    sh["lru_wa"] = np.ascontiguousarray(inp["lru_wa"], f)
    sh["lru_wx"] = np.ascontiguousarray(inp["lru_wx"], f)
    v = np.stack([inp["lru_ba"], inp["lru_bx"], inp["lru_lam"]], axis=1).astype(f)
    sh["lru_vT"] = np.ascontiguousarray(np.transpose(v.reshape(depth, 3, 2, 4, 128), (0, 4, 1, 2, 3)))
    sh["g_grpT"] = _chan_major(inp["g_grp"].astype(f), 8)
    sh["w_out"] = np.ascontiguousarray(inp["w_out"], f)
    wg2 = np.transpose(inp["w_g2"], (0, 2, 1, 3)).reshape(depth, D, 32)
    sh["wr"] = np.ascontiguousarray(np.concatenate([inp["w_g1"], wg2], axis=-1), f)
    sh["rb"] = np.ascontiguousarray(np.concatenate([inp["b_g1"], inp["b_g2"].reshape(depth, 32)], axis=-1), f)
    sh["w_e_gate"] = np.ascontiguousarray(inp["w_e_gate"], f)
    sh["w_e_up"] = np.ascontiguousarray(inp["w_e_up"], f)
    sh["w_e_down"] = np.ascontiguousarray(inp["w_e_down"], f)
    sh["g_final"] = np.ascontiguousarray(inp["g_final"], f)
    sh["ident"] = np.eye(128, dtype=f)
    j = np.arange(128)[:, None]
    i = np.arange(128)[None, :]
    sh["masks"] = np.ascontiguousarray(np.stack([(j >= i), (j <= i)], axis=1).astype(f))
    pos = np.arange(SEQ)
    rows = (pos // 64).astype(np.float32)
    colsp = (pos % 64).astype(np.float32)

    def tab(half):
        fr = (np.float32(10000.0) ** (-np.arange(half, dtype=np.float32) / np.float32(half))).astype(np.float32)
        ar = rows[:, None] * fr[None, :]
        ac = colsp[:, None] * fr[None, :]
        C = np.concatenate([np.cos(ar), np.cos(ac)], axis=1)
        Sn = np.concatenate([np.sin(ar), np.sin(ac)], axis=1)
        return C.astype(f), Sn.astype(f)
    Cm, Sm = tab(8)
    Cs, Ss = tab(16)
    sh["rope"] = np.ascontiguousarray(np.concatenate([Cm, Sm, Cs, Ss], axis=1).reshape(SEQ // 128, 128, 96))
    return sh


def host_core(inp, b0, NB):
    f = np.float32
    cv = np.stack([inp["c"][b0], inp["c"][min(b0 + 1, inp["c"].shape[0] - 1)], inp["c_ctx"]], axis=0).astype(f)
    cT = np.ascontiguousarray(np.transpose(cv.reshape(3, KD, 128), (2, 1, 0)))
    return {
        "x": np.ascontiguousarray(inp["x"][b0:b0 + NB], f),
        "ctx": np.ascontiguousarray(inp["ctx"][b0:b0 + NB], f),
        "cT": cT,
    }


_NC_CACHE = {}


def kernel(**inputs):
    inp = {k: np.asarray(v) for k, v in inputs.items()}
    B, SEQ, _ = inp["x"].shape
    ncores = 8
    NB = B // ncores
    key = (SEQ, NB)
    if key not in _NC_CACHE:
        _NC_CACHE[key] = build(SEQ, NB)
    nc = _NC_CACHE[key]
    sh = host_shared(inp, SEQ)
    in_maps = []
    for c in range(ncores):
        m = dict(sh)
        m.update(host_core(inp, c * NB, NB))
        in_maps.append(m)
    res = run_bass_kernel_spmd(nc, in_maps, core_ids=list(range(ncores)))
    out = np.concatenate([np.asarray(r["out"]) for r in res.results], axis=0)
    return out.astype(np.float32)
```

```python
import numpy as np
from contextlib import ExitStack
import concourse.bass as bass
import concourse.mybir as mybir
from concourse.bass_utils import run_bass_kernel_spmd

F32 = mybir.dt.float32
BF16 = mybir.dt.bfloat16
AF = mybir.ActivationFunctionType
ALU = mybir.AluOpType
AX = mybir.AxisListType

D = 1024
KD = 8
LCTX = 256
EPS = 1e-6
NEXP = 32
DEXP = 256
MLA_SCALE = 96 ** -0.5
SWA_SCALE = 0.125

COMPUTE = ("pe", "act", "dve", "pool")
DMAQ = ("sp", "pool")
NDMASEM = 12


class Buf:
    __slots__ = ("w", "r", "name", "excl")

    def __init__(self, name="", excl=False):
        self.w = None
        self.r = {}
        self.name = name
        self.excl = excl


class V:
    __slots__ = ("ap", "bufs")

    def __init__(self, ap, bufs):
        self.ap = ap
        self.bufs = bufs

    def __getitem__(self, idx):
        return V(self.ap[idx], self.bufs)

    def re(self, pat, **kw):
        return V(self.ap.rearrange(pat, **kw), self.bufs)

    def bcast(self, axis, shape):
        return V(self.ap.unsqueeze(axis).broadcast_to(list(shape)), self.bufs)

    def sub(self, buf):
        return V(self.ap, [buf])


class Op:
    __slots__ = ("eng", "fn", "deps", "sig", "cnt", "dma", "dsem", "dtgt", "idx")

    def __init__(self, eng, fn, dma):
        self.eng = eng
        self.fn = fn
        self.dma = dma
        self.deps = None
        self.sig = False
        self.cnt = 0
        self.dsem = None
        self.dtgt = 0


class Sched:
    def __init__(self, nc, es):
        self.nc = nc
        self.es = es
        self.ops = {e: [] for e in ("pe", "act", "dve", "pool", "sp")}
        self.dma_hist = {q: [None] * NDMASEM for q in DMAQ}
        self.dma_cnt = {q: [0] * NDMASEM for q in DMAQ}
        self.dma_rr = {q: 0 for q in DMAQ}
        self.sems = {}
        self.dsems = {}
        self.out_dmas = []
        self.bar = set()
        self.last = {e: None for e in COMPUTE}
        self.nadd = 0
        self.limit = None
        self.trace = None

    def dram(self, name, shape, dtype, kind="Internal"):
        t = self.nc.dram_tensor(name, list(shape), dtype, kind=kind)
        return V(t.ap(), [])

    def barrier(self):
        b = set()
        for e in COMPUTE:
            if self.last[e] is not None:
                b.add(self.last[e])
        for q in DMAQ:
            for o in self.dma_hist[q]:
                if o is not None:
                    b.add(o)
        for o in b:
            o.sig = True
        self.bar = b

    def add(self, eng, fn, reads=(), writes=(), dma=False):
        op = Op(eng, fn, dma)
        self.nadd += 1
        op.idx = self.nadd
        if self.trace is not None and self.trace[0] <= self.nadd <= self.trace[1]:
            import traceback
            fr = [f for f in traceback.extract_stack() if f.name not in ("add",)][-2:]
            print("OP", self.nadd, eng, "dma" if dma else "", [(f.lineno, f.line) for f in fr][-1])
        if self.limit is not None and self.nadd > self.limit:
            op.deps = set()
            return op
        deps = set(self.bar)
        for v in reads:
            for b in v.bufs:
                if b.w is not None:
                    deps.add(b.w)
                if b.excl:
                    for key, r in b.r.items():
                        if key != eng and not isinstance(r, list):
                            deps.add(r)
        for v in writes:
            for b in v.bufs:
                if b.w is not None:
                    deps.add(b.w)
                for r in b.r.values():
                    if isinstance(r, list):
                        deps.update(r)
                    elif r.eng != eng or r.dma or dma:
                        deps.add(r)
        if dma:
            q = eng
            slot = self.dma_rr[q] % NDMASEM
            self.dma_rr[q] += 1
            prev = self.dma_hist[q][slot]
            if prev is not None:
                deps.add(prev)
            self.dma_cnt[q][slot] += 1
            op.dsem = (q, slot)
            op.dtgt = 16 * self.dma_cnt[q][slot]
            self.dma_hist[q][slot] = op
        deps.discard(op)
        if eng == "pe":
            deps = {d for d in deps if d.dma or d.eng != "pe"}
        op.deps = deps
        for d in deps:
            d.sig = True
        for v in reads:
            for b in v.bufs:
                if dma:
                    b.r.setdefault(("dma", eng), []).append(op)
                else:
                    b.r[eng] = op
        for v in writes:
            for b in v.bufs:
                b.w = op
                b.r = {}
        self.ops[eng].append(op)
        if not dma:
            self.last[eng] = op
        return op

    def emit(self):
        nc = self.nc
        es = self.es
        import os as _os2
        for _i in range(int(_os2.environ.get("KDUMMYSEM", "0"))):
            es.enter_context(nc.semaphore("dummy%d" % _i))
        for e in COMPUTE:
            self.sems[e] = es.enter_context(nc.semaphore("s_" + e))
        for q in DMAQ:
            for i in range(NDMASEM):
                self.dsems[(q, i)] = es.enter_context(nc.semaphore("d_%s%d" % (q, i)))
        for e in COMPUTE:
            c = 0
            for op in self.ops[e]:
                if op.dma:
                    continue
                if op.sig:
                    c += 1
                    op.cnt = c
            assert c < 65000, (e, c)
        final_waits = list(self.out_dmas)
        block = es.enter_context(nc.Block())
        sched = self

        def run(ename, eng):
            waited = {e: 0 for e in COMPUTE}
            dwaited = {}
            for op in sched.ops[ename]:
                need = {}
                if sched.trace is not None and sched.trace[0] <= op.idx <= sched.trace[1]:
                    print("EMIT", op.idx, ename, "cnt", op.cnt, "sig", op.sig, "deps", sorted((d.eng, d.idx, d.cnt, d.dtgt) for d in op.deps))
                for d in op.deps:
                    if d.dma:
                        if dwaited.get(d.dsem, 0) < d.dtgt:
                            dwaited[d.dsem] = d.dtgt
                            eng.wait_ge(sched.dsems[d.dsem], d.dtgt)
                    else:
                        if d.cnt > need.get(d.eng, 0):
                            need[d.eng] = d.cnt
                for se, c in need.items():
                    if c > waited[se]:
                        waited[se] = c
                        eng.wait_ge(sched.sems[se], c)
                inst = op.fn(eng)
                if op.dma:
                    inst.then_inc(sched.dsems[op.dsem], 16)
                elif op.sig:
                    inst.then_inc(sched.sems[ename], 1)
            if ename == "sp":
                for d in final_waits:
                    eng.wait_ge(sched.dsems[d.dsem], d.dtgt)

        @block.tensor
        def _(eng):
            run("pe", eng)

        @block.scalar
        def _(eng):
            run("act", eng)

        @block.vector
        def _(eng):
            run("dve", eng)

        @block.gpsimd
        def _(eng):
            run("pool", eng)

        @block.sync
        def _(eng):
            run("sp", eng)

    def dma(self, out, in_, q="sp", is_out=False):
        op = self.add(q, lambda e: e.dma_start(out=out.ap, in_=in_.ap), reads=[in_], writes=[out], dma=True)
        if is_out:
            self.out_dmas.append(op)
        return op

    def mm(self, out, lhsT, rhs, start=True, stop=True):
        return self.add("pe", lambda e: e.matmul(out.ap, lhsT.ap, rhs.ap, start=start, stop=stop),
                        reads=[lhsT, rhs], writes=[out])

    def transpose(self, out, in_, ident):
        return self.add("pe", lambda e: e.transpose(out.ap, in_.ap, ident.ap), reads=[in_, ident], writes=[out])

    def act(self, out, in_, func, bias=None, scale=None, accum=None):
        reads = [in_]
        kw = {}
        if bias is not None:
            if isinstance(bias, V):
                reads.append(bias)
                kw["bias"] = bias.ap
            else:
                kw["bias"] = bias
        if scale is not None:
            if isinstance(scale, V):
                reads.append(scale)
                kw["scale"] = scale.ap
            else:
                kw["scale"] = scale
        writes = [out]
        if accum is not None:
            kw["accum_out"] = accum.ap
            writes.append(accum)
        return self.add("act", lambda e: e.activation(out.ap, in_.ap, func, **kw), reads=reads, writes=writes)

    def tt(self, out, a, b, op, eng="dve"):
        return self.add(eng, lambda e: e.tensor_tensor(out.ap, a.ap, b.ap, op), reads=[a, b], writes=[out])

    def ts(self, out, a, s1, s2, op0, op1=None, eng="dve"):
        reads = [a]
        a1 = s1.ap if isinstance(s1, V) else s1
        a2 = s2.ap if isinstance(s2, V) else s2
        if isinstance(s1, V):
            reads.append(s1)
        if isinstance(s2, V):
            reads.append(s2)
        if op1 is None:
            return self.add(eng, lambda e: e.tensor_scalar(out.ap, a.ap, a1, None, op0), reads=reads, writes=[out])
        return self.add(eng, lambda e: e.tensor_scalar(out.ap, a.ap, a1, a2, op0, op1), reads=reads, writes=[out])

    def stt(self, out, a, s, b, op0, op1):
        reads = [a, b]
        a1 = s.ap if isinstance(s, V) else s
        if isinstance(s, V):
            reads.append(s)
        return self.add("dve", lambda e: e.scalar_tensor_tensor(out.ap, a.ap, a1, b.ap, op0, op1),
                        reads=reads, writes=[out])

    def copy(self, out, in_, eng="dve"):
        if eng == "act":
            return self.add("act", lambda e: e.copy(out.ap, in_.ap), reads=[in_], writes=[out])
        return self.add(eng, lambda e: e.tensor_copy(out.ap, in_.ap), reads=[in_], writes=[out])

    def memset(self, out, val, eng="pool"):
        return self.add(eng, lambda e: e.memset(out.ap, val), reads=[], writes=[out])

    def scan(self, out, d0, d1, init):
        reads = [d0, d1]
        i = init.ap if isinstance(init, V) else init
        if isinstance(init, V):
            reads.append(init)
        return self.add("dve", lambda e: e.tensor_tensor_scan(out.ap, d0.ap, d1.ap, i, ALU.mult, ALU.add),
                        reads=reads, writes=[out])

    def recip(self, out, in_):
        return self.add("dve", lambda e: e.reciprocal(out.ap, in_.ap), reads=[in_], writes=[out])

    def reduce(self, out, in_, op):
        return self.add("dve", lambda e: e.tensor_reduce(out.ap, in_.ap, AX.X, op), reads=[in_], writes=[out])

    def max8(self, out, in_):
        return self.add("dve", lambda e: e.max(out.ap, in_.ap), reads=[in_], writes=[out])


class Region:
    def __init__(self, arena_ap, start, end, cached=False):
        self.ap = arena_ap
        self.cached = cached
        self.cache = {}
        self.start = start
        self.end = end
        self.off = start

    def reset(self):
        if not self.cached:
            self.off = self.start

    def alloc(self, name, free_shape, dtype):
        if self.cached and name in self.cache:
            return self.cache[name]
        v = self._alloc(name, free_shape, dtype)
        if self.cached:
            self.cache[name] = v
        return v

    def _alloc(self, name, free_shape, dtype):
        n = 1
        for s in free_shape:
            n *= s
        esz = 4 if dtype == F32 else 2
        nb = (n * esz + 63) // 64 * 64
        assert self.off + nb <= self.end, ("sbuf region overflow", name, self.off, nb, self.end)
        a = self.ap[:, self.off // 4:(self.off + nb) // 4]
        self.off += nb
        if dtype != F32:
            a = a.bitcast(dtype)
        a = a[:, 0:n]
        if len(free_shape) == 2:
            a = a.rearrange("p (a b) -> p a b", a=free_shape[0])
        elif len(free_shape) == 3:
            a = a.rearrange("p (a b c) -> p a b c", a=free_shape[0], b=free_shape[1])
        return V(a, [Buf(name)])


def rms_rstd(S, R, ss, n, width, tag):
    r = R.alloc("rstd_" + tag, [width], F32)
    S.act(r, ss, AF.Sqrt, bias=EPS, scale=1.0 / n)
    S.recip(r, r)
    return r


def build(SEQ, NB, depth=2, debug=False, plan="ABSCD", nlayers=None):
    T = LCTX + SEQ
    NT = T // 128
    NLT = SEQ // 128
    nc = bass.Bass("TRN2", target_bir_lowering=False)
    es = ExitStack()
    with es:
        S = Sched(nc, es)
        import os as _os
        if _os.environ.get("KTRACE"):
            S.trace = tuple(int(v) for v in _os.environ["KTRACE"].split(","))
        if _os.environ.get("KLIMIT"):
            S.limit = int(_os.environ["KLIMIT"])
        okind = "ExternalOutput" if debug else "Internal"
        x_d = S.dram("x", [NB, SEQ, D], F32, "ExternalInput")
        ctx_d = S.dram("ctx", [NB, LCTX, D], F32, "ExternalInput")
        cT_d = S.dram("cT", [128, KD, 3], F32, "ExternalInput")
        w_ada_d = S.dram("w_ada", [depth, D, 6 * D], F32, "ExternalInput")
        b_adaT_d = S.dram("b_adaT", [depth, 128, 48], F32, "ExternalInput")
        b_ada_d = S.dram("b_ada", [depth, 6 * D], F32, "ExternalInput")
        g1T_d = S.dram("g1T", [depth, 128, KD], F32, "ExternalInput")
        g2T_d = S.dram("g2T", [depth, 128, KD], F32, "ExternalInput")
        w_in_d = S.dram("w_in_p", [depth, D, 1952], F32, "ExternalInput")
        g_cqT_d = S.dram("g_cqT", [depth, 128, 2], F32, "ExternalInput")
        g_ckvT_d = S.dram("g_ckvT", [depth, 128, 1], F32, "ExternalInput")
        w_uq_d = S.dram("w_uq_p", [depth, 256, 384], F32, "ExternalInput")
        w_ukv_d = S.dram("w_ukv_p", [depth, 128, 512], F32, "ExternalInput")
        sink_d = S.dram("sink", [depth, 4], F32, "ExternalInput")
        conv_wT_d = S.dram("conv_wT", [depth, 128, 4, 4], F32, "ExternalInput")
        conv_bT_d = S.dram("conv_bT", [depth, 128, 4], F32, "ExternalInput")
        lru_wa_d = S.dram("lru_wa", [depth, 2, 8, 64, 64], F32, "ExternalInput")
        lru_wx_d = S.dram("lru_wx", [depth, 2, 8, 64, 64], F32, "ExternalInput")
        lru_vT_d = S.dram("lru_vT", [depth, 128, 3, 2, 4], F32, "ExternalInput")
        g_grpT_d = S.dram("g_grpT", [depth, 128, KD], F32, "ExternalInput")
        w_out_d = S.dram("w_out", [depth, D, D], F32, "ExternalInput")
        wr_d = S.dram("wr", [depth, D, 36], F32, "ExternalInput")
        rb_d = S.dram("rb", [depth, 36], F32, "ExternalInput")
        weg_d = S.dram("w_e_gate", [depth, NEXP, D, DEXP], F32, "ExternalInput")
        weu_d = S.dram("w_e_up", [depth, NEXP, D, DEXP], F32, "ExternalInput")
        wed_d = S.dram("w_e_down", [depth, NEXP, DEXP, D], F32, "ExternalInput")
        gfin_d = S.dram("g_final", [D], F32, "ExternalInput")
        ident_d = S.dram("ident", [128, 128], F32, "ExternalInput")
        masks_d = S.dram("masks", [128, 2, 128], F32, "ExternalInput")
        rope_d = S.dram("rope", [NLT + 1, 128, 96], F32, "ExternalInput")
        out_d = S.dram("out", [NB, SEQ, D], F32, "ExternalOutput")
        GATES = S.dram("s_gates", [depth, 3, 2, D], F32, okind)
        XS = S.dram("s_xs", [NB, T, D], F32, okind)
        X1S = S.dram("s_x1s", [NB, T, D], F32, okind)
        ZL = S.dram("s_zl", [NB, 1024, T], F32, okind)
        QT = S.dram("s_qt", [NB, 96, 4, T], BF16, okind)
        KT = S.dram("s_kt", [NB, 96, 4, T], BF16, okind)
        VA = S.dram("s_va", [NB, T, 260], BF16, okind)
        SQKT = S.dram("s_sqkt", [NB, 64, 6, T], BF16, okind)
        SVA = S.dram("s_sva", [NB, T, 130], BF16, okind)
        OMIX = S.dram("s_omix", [NB, T, 512], F32, okind)
        OLRU = S.dram("s_olru", [NB, 512, T], BF16, okind)

        ARENA_BYTES = 190 * 1024
        arena_t = es.enter_context(nc.sbuf_tensor("arena", [128, ARENA_BYTES // 4], F32))
        arena = arena_t[:]
        PERS_BYTES = 12 * 1024
        P = Region(arena, 0, PERS_BYTES)
        R = Region(arena, PERS_BYTES, ARENA_BYTES)
        psum = []
        for i in range(4):
            t = es.enter_context(nc.psum_tensor("ps%d" % i, [128, 1024], F32))
            a = t[:]
            b0, b1 = Buf("ps%da" % i, excl=True), Buf("ps%db" % i, excl=True)
            psum.append((V(a, [b0, b1]), V(a[:, 0:512], [b0]), V(a[:, 512:1024], [b1])))
        PB = []
        for pr in psum:
            PB.append(pr[1])
            PB.append(pr[2])

        def pbf(bank):
            return V(bank.ap.bitcast(BF16), bank.bufs)

        identF = P.alloc("identF", [128], F32)
        identB = P.alloc("identB", [128], BF16)
        maskB = P.alloc("maskB", [2, 128], BF16)
        ones1 = P.alloc("ones1", [1], F32)
        modT = P.alloc("modT", [depth, 48, 3], F32)
        AB = P.alloc("AB", [depth * 3, 4, KD], F32)
        sT = P.alloc("sT", [KD, 3], F32)
        S.dma(identF, ident_d)
        S.copy(identB, identF, eng="dve")
        S.dma(sT, cT_d)
        S.memset(ones1, 1.0, eng="dve")
        R.reset()
        mtmp = R.alloc("mtmp", [2, 128], F32)
        S.dma(mtmp, masks_d)
        S.copy(maskB, mtmp, eng="dve")
        S.act(sT, sT, AF.Silu)
        wab = [R.alloc("wab%d" % i, [KD, 512], F32) for i in range(2)]
        brow = [R.alloc("brow%d" % i, [512], F32) for i in range(2)]
        grow = [R.alloc("grow%d" % i, [512], F32) for i in range(2)]
        badaT = R.alloc("badaT", [depth, 48], F32)
        gT = R.alloc("gT", [depth, 2, KD], F32)
        for l in range(depth):
            S.dma(badaT[:, l, :], b_adaT_d[l])
            S.dma(gT[:, l, 0, :], g1T_d[l])
            S.dma(gT[:, l, 1, :], g2T_d[l])
        it = 0
        for l in range(depth):
            for j in range(12):
                w = wab[it % 2]
                S.dma(w, w_ada_d[l][:, j * 512:(j + 1) * 512].re("(k p) n -> p k n", p=128))
                vec = j // 2
                if vec in (2, 5):
                    br = brow[it % 2]
                    S.dma(br[0:3, :], V(b_ada_d.ap[l, j * 512:(j + 1) * 512].partition_broadcast(3), []))
                    pm = PB[it % 2]
                    for k in range(KD):
                        S.mm(pm[0:3, :], sT[:, k, :], w[:, k, :], start=(k == 0), stop=(k == KD - 1))
                    gr = grow[it % 2]
                    S.tt(gr[0:3, :], pm[0:3, :], br[0:3, :], ALU.add)
                    S.dma(GATES[l, :, 0 if vec == 2 else 1, (j % 2) * 512:(j % 2 + 1) * 512], gr[0:3, :])
                else:
                    for m in range(4):
                        pm = PB[2 + (m % 2)]
                        for k in range(KD):
                            S.mm(pm[:, 0:3], w[:, k, m * 128:(m + 1) * 128], sT[:, k, :],
                                 start=(k == 0), stop=(k == KD - 1))
                        S.act(modT[:, l, j * 4 + m, :], pm[:, 0:3], AF.Identity,
                              bias=badaT[:, l, j * 4 + m:j * 4 + m + 1], scale=1.0)
                it += 1
            for r in range(3):
                ab = AB[:, l * 3 + r]
                S.ts(ab[:, 0, :], modT[:, l, 8:16, r], 1.0, None, ALU.add)
                S.tt(ab[:, 0, :], ab[:, 0, :], gT[:, l, 0, :], ALU.mult)
                S.copy(ab[:, 1, :], modT[:, l, 0:8, r])
                S.ts(ab[:, 2, :], modT[:, l, 32:40, r], 1.0, None, ALU.add)
                S.tt(ab[:, 2, :], ab[:, 2, :], gT[:, l, 1, :], ALU.mult)
                S.copy(ab[:, 3, :], modT[:, l, 24:32, r])
        S.barrier()
        if debug:
            print("ops after prologue", S.nadd)

        def x_src(b, l, t):
            if l == 0:
                if t < 2:
                    return ctx_d[b, t * 128:(t + 1) * 128, :]
                return x_d[b, (t - 2) * 128:(t - 1) * 128, :]
            return XS[b, t * 128:(t + 1) * 128, :]

        def norm_transpose(xt, A, Bc, hT_dst, tmpR, junk, pbanks, hTf_dst=None):
            ss = tmpR.alloc("ss", [1], F32)
            S.act(junk, xt, AF.Square, accum=ss)
            rstd = rms_rstd(S, tmpR, ss, D, 1, "x")
            xn = tmpR.alloc("xn", [D], F32)
            S.ts(xn, xt, rstd, None, ALU.mult, eng="pool")
            for half in range(2):
                pb = pbanks[half]
                for kk in range(4):
                    k = half * 4 + kk
                    S.transpose(pb[:, kk * 128:(kk + 1) * 128], xn[:, k * 128:(k + 1) * 128], identF)
                for kk in range(4):
                    k = half * 4 + kk
                    S.act(hT_dst[:, k, :], pb[:, kk * 128:(kk + 1) * 128], AF.Identity,
                          bias=Bc[:, k:k + 1], scale=A[:, k:k + 1])
                    if hTf_dst is not None:
                        S.ts(hTf_dst[:, k, :], pb[:, kk * 128:(kk + 1) * 128], A[:, k:k + 1], Bc[:, k:k + 1],
                             ALU.mult, ALU.add)

        def phase_A(b, l):
            R.reset()
            rowl, rowc = l * 3 + b, l * 3 + 2
            w_in = R.alloc("w_in", [KD, 1952], BF16)
            for k in range(KD):
                S.dma(w_in[:, k, :], w_in_d[l][k * 128:(k + 1) * 128, :], q="pool")
            wq_f = R.alloc("wq_f", [2, 384], F32)
            wkv_f = R.alloc("wkv_f", [512], F32)
            gq = R.alloc("gq", [3], F32)
            S.dma(wq_f, w_uq_d[l].re("(j p) n -> p j n", p=128))
            S.dma(wkv_f, w_ukv_d[l])
            S.dma(gq[:, 0:2], g_cqT_d[l])
            S.dma(gq[:, 2:3], g_ckvT_d[l])
            wq = R.alloc("wq", [2, 384], BF16)
            wkv = R.alloc("wkv", [512], BF16)
            for j in range(2):
                S.ts(wq[:, j, :], wq_f[:, j, :], gq[:, j:j + 1], None, ALU.mult)
            S.ts(wkv, wkv_f, gq[:, 2:3], None, ALU.mult)
            NS = 4
            xt = [R.alloc("xt%d" % i, [D], F32) for i in range(NS)]
            rp = [R.alloc("rp%d" % i, [96], F32) for i in range(NS)]
            hTs = [R.alloc("hTs%d" % i, [KD, 512], BF16) for i in range(2)]
            qa = [R.alloc("qa%d" % i, [4, 96], BF16) for i in range(NS)]
            ka = [R.alloc("ka%d" % i, [4, 96], BF16) for i in range(NS)]
            va = [R.alloc("va%d" % i, [4, 65], BF16) for i in range(NS)]
            sqk = [R.alloc("sqk%d" % i, [6, 64], BF16) for i in range(NS)]
            sva = [R.alloc("sva%d" % i, [2, 65], BF16) for i in range(NS)]
            qTt = [R.alloc("qTt%d" % i, [4, 128], BF16) for i in range(NS)]
            kTt = [R.alloc("kTt%d" % i, [4, 128], BF16) for i in range(NS)]
            sqkT = [R.alloc("sqkT%d" % i, [6, 128], BF16) for i in range(NS)]
            zlt = [R.alloc("zlt%d" % i, [512], F32) for i in range(2)]
            for i in range(NS):
                S.memset(va[i][:, :, 64:65], 1.0)
                S.memset(sva[i][:, :, 64:65], 1.0)
            TSZ = 15 * 1024
            TRs = [Region(arena, R.off + i * TSZ, R.off + (i + 1) * TSZ, cached=True) for i in range(NS)]
            assert R.off + NS * TSZ <= ARENA_BYTES, ("phase A sbuf", R.off + NS * TSZ)
            groups = [[0, 1]] + [list(range(2 + 4 * g, 2 + 4 * g + 4)) for g in range(NLT // 4)]
            loaded = set()

            def load_tile(t, slot, what="xr"):
                for w_ in what:
                    if (t, w_) in loaded:
                        continue
                    loaded.add((t, w_))
                    if w_ == "x":
                        S.dma(xt[slot], x_src(b, l, t))
                    else:
                        S.dma(rp[slot], rope_d[NLT if t < 2 else t - 2])

            for ti, t in enumerate(groups[0]):
                load_tile(t, ti)
            if len(groups) > 1:
                for ti, t in enumerate(groups[1]):
                    if ti >= len(groups[0]):
                        load_tile(t, ti)

            def a_tile(gi, ti, t, hT):
                TR = TRs[ti]
                bA, bB = PB[2 * ti], PB[2 * ti + 1]
                is_ctx = t < 2
                A = AB[:, rowc if is_ctx else rowl]
                x_t, rpt = xt[ti], rp[ti]
                hTt = hT[:, :, ti * 128:(ti + 1) * 128]
                junk = TR.alloc("junk", [D], BF16)
                ss = TR.alloc("ss", [1], F32)
                S.act(junk, x_t, AF.Square, accum=ss)
                yield
                rstd = TR.alloc("rstd_x", [1], F32)
                S.act(rstd, ss, AF.Sqrt, bias=EPS, scale=1.0 / D)
                yield
                S.recip(rstd, rstd)
                yield
                xn = TR.alloc("xn", [D], F32)
                S.ts(xn, x_t, rstd, None, ALU.mult, eng="pool")
                if gi + 1 < len(groups) and ti < len(groups[gi + 1]):
                    load_tile(groups[gi + 1][ti], ti, "x")
                yield
                for half, pb in enumerate((bA, bB)):
                    for kk in range(4):
                        k = half * 4 + kk
                        S.transpose(pb[:, kk * 128:(kk + 1) * 128], xn[:, k * 128:(k + 1) * 128], identF)
                yield
                for half, pb in enumerate((bA, bB)):
                    for kk in range(4):
                        k = half * 4 + kk
                        S.act(hTt[:, k, :], pb[:, kk * 128:(kk + 1) * 128], AF.Identity,
                              bias=A[:, 1, k:k + 1], scale=A[:, 0, k:k + 1])
                yield
                for k in range(KD):
                    S.mm(bA[:, 0:416], hTt[:, k, :], w_in[:, k, 0:416], start=(k == 0), stop=(k == KD - 1))
                for k in range(KD):
                    S.mm(bB[:, 0:512], hTt[:, k, :], w_in[:, k, 416:928], start=(k == 0), stop=(k == KD - 1))
                yield
                ss2 = TR.alloc("ss2", [2], F32)
                S.act(junk[:, 0:256], bA[:, 0:256], AF.Square, accum=ss2[:, 0:1])
                S.act(junk[:, 256:384], bA[:, 256:384], AF.Square, accum=ss2[:, 1:2])
                krr = TR.alloc("krr", [32], F32)
                S.act(krr, bA[:, 384:416], AF.Identity)
                z1s = TR.alloc("z1s", [512], F32)
                S.copy(z1s, bB[:, 0:512], eng="dve")
                yield
                rs2 = TR.alloc("rs2", [2], F32)
                S.act(rs2[:, 0:1], ss2[:, 0:1], AF.Sqrt, bias=EPS, scale=1.0 / 256)
                S.act(rs2[:, 1:2], ss2[:, 1:2], AF.Sqrt, bias=EPS, scale=1.0 / 128)
                yield
                S.recip(rs2, rs2)
                yield
                cn = TR.alloc("cn", [384], BF16)
                S.act(cn[:, 0:256], bA[:, 0:256], AF.Identity, scale=rs2[:, 0:1])
                S.act(cn[:, 256:384], bA[:, 256:384], AF.Identity, scale=rs2[:, 1:2])
                q_a, k_a, v_a, sqk_a, sv_a = qa[ti], ka[ti], va[ti], sqk[ti], sva[ti]
                Cm, Sm = rpt[:, 0:16], rpt[:, 16:32]
                Cs, Ss = rpt[:, 32:64], rpt[:, 64:96]
                z1h = z1s[:, 0:384].re("p (h d) -> p h d", h=6)
                S.copy(sv_a[:, :, 0:64], z1s[:, 384:512].re("p (h d) -> p h d", h=2), eng="pool")
                Cs6, Ss6 = Cs.bcast(1, [128, 6, 32]), Ss.bcast(1, [128, 6, 32])
                s1 = TR.alloc("s1", [6, 32], F32)
                s2 = TR.alloc("s2", [6, 32], F32)
                s3 = TR.alloc("s3", [6, 32], F32)
                s4 = TR.alloc("s4", [6, 32], F32)
                S.tt(s1, z1h[:, :, 0:32], Cs6, ALU.mult)
                S.tt(s2, z1h[:, :, 32:64], Ss6, ALU.mult)
                S.tt(s3, z1h[:, :, 32:64], Cs6, ALU.mult)
                S.tt(s4, z1h[:, :, 0:32], Ss6, ALU.mult)
                k1 = TR.alloc("k1", [16], F32)
                k2 = TR.alloc("k2", [16], F32)
                k3 = TR.alloc("k3", [16], F32)
                k4 = TR.alloc("k4", [16], F32)
                S.tt(k1, krr[:, 0:16], Cm, ALU.mult, eng="pool")
                S.tt(k2, krr[:, 16:32], Sm, ALU.mult, eng="pool")
                S.tt(k3, krr[:, 16:32], Cm, ALU.mult)
                S.tt(k4, krr[:, 0:16], Sm, ALU.mult)
                yield
                pT = pbf(bA)
                for j in range(3):
                    S.transpose(pT[:, j * 128:(j + 1) * 128], cn[:, j * 128:(j + 1) * 128], identB)
                S.tt(sqk_a[:, :, 0:32], s1, s2, ALU.subtract, eng="pool")
                S.tt(sqk_a[:, :, 32:64], s3, s4, ALU.add)
                kr = TR.alloc("kr", [32], F32)
                S.tt(kr[:, 0:16], k1, k2, ALU.subtract, eng="pool")
                S.tt(kr[:, 16:32], k3, k4, ALU.add)
                yield
                cT = TR.alloc("cTt", [3, 128], BF16)
                S.copy(cT, pT[:, 0:384].re("p (j t) -> p j t", j=3), eng="dve")
                S.copy(k_a[:, :, 64:96], kr.bcast(1, [128, 4, 32]), eng="dve")
                yield
                pq, pkv = bA, bB
                for j in range(2):
                    S.mm(pq[:, 0:384], cT[:, j, :], wq[:, j, :], start=(j == 0), stop=(j == 1))
                S.mm(pkv[:, 0:512], cT[:, 2, :], wkv, start=True, stop=True)
                yield
                pq3 = pq[:, 0:384].re("p (h d) -> p h d", h=4)
                S.copy(q_a[:, :, 0:64], pq3[:, :, 0:64], eng="act")
                qr = TR.alloc("qr", [4, 32], F32)
                S.copy(qr, pq3[:, :, 64:96], eng="act")
                S.copy(k_a[:, :, 0:64], pkv[:, 0:256].re("p (h d) -> p h d", h=4), eng="dve")
                S.copy(v_a[:, :, 0:64], pkv[:, 256:512].re("p (h d) -> p h d", h=4), eng="dve")
                yield
                Cm4, Sm4 = Cm.bcast(1, [128, 4, 16]), Sm.bcast(1, [128, 4, 16])
                q1 = TR.alloc("q1", [4, 16], F32)
                q2 = TR.alloc("q2", [4, 16], F32)
                q3 = TR.alloc("q3", [4, 16], F32)
                q4 = TR.alloc("q4", [4, 16], F32)
                S.tt(q1, qr[:, :, 0:16], Cm4, ALU.mult)
                S.tt(q2, qr[:, :, 16:32], Sm4, ALU.mult)
                S.tt(q3, qr[:, :, 16:32], Cm4, ALU.mult)
                S.tt(q4, qr[:, :, 0:16], Sm4, ALU.mult)
                pTk = pbf(bB)
                for h in range(4):
                    S.transpose(pTk[0:96, h * 128:(h + 1) * 128], k_a[:, h, :], identB)
                yield
                S.tt(q_a[:, :, 64:80], q1, q2, ALU.subtract, eng="pool")
                S.tt(q_a[:, :, 80:96], q3, q4, ALU.add)
                kT_t = kTt[ti]
                S.copy(kT_t[0:96], pTk[0:96, 0:512].re("p (h t) -> p h t", h=4), eng="act")
                yield
                pTq = pbf(bA)
                for h in range(4):
                    S.transpose(pTq[0:96, h * 128:(h + 1) * 128], q_a[:, h, :], identB)
                pTs = pbf(bB)
                for h in range(6):
                    S.transpose(pTs[0:64, h * 128:(h + 1) * 128], sqk_a[:, h, :], identB)
                yield
                qT_t = qTt[ti]
                S.copy(qT_t[0:96], pTq[0:96, 0:512].re("p (h t) -> p h t", h=4), eng="dve")
                sT_t = sqkT[ti]
                S.copy(sT_t[0:64], pTs[0:64, 0:768].re("p (h t) -> p h t", h=6), eng="act")
                tok = slice(t * 128, (t + 1) * 128)
                S.dma(KT[b, :, :, tok], kT_t[0:96])
                S.dma(VA[b, tok, :], v_a.re("p h d -> p (h d)"))
                S.dma(SVA[b, tok, :], sv_a.re("p h d -> p (h d)"))
                yield
                S.dma(QT[b, :, :, tok], qT_t[0:96])
                S.dma(SQKT[b, :, :, tok], sT_t[0:64])
                if gi + 1 < len(groups) and ti < len(groups[gi + 1]):
                    load_tile(groups[gi + 1][ti], ti, "r")

            for gi, grp in enumerate(groups):
                hT = hTs[gi % 2]
                active = [a_tile(gi, ti, t, hT) for ti, t in enumerate(grp)]
                while active:
                    for g_ in list(active):
                        try:
                            next(g_)
                        except StopIteration:
                            active.remove(g_)
                ntok = 128 * len(grp)
                tok0 = grp[0] * 128
                for m in range(8):
                    pz = PB[m]
                    for k in range(KD):
                        S.mm(pz[:, 0:ntok], w_in[:, k, 928 + m * 128:928 + (m + 1) * 128], hT[:, k, 0:ntok],
                             start=(k == 0), stop=(k == KD - 1))
                    zt = zlt[m % 2]
                    S.copy(zt[:, 0:ntok], pz[:, 0:ntok], eng=("act" if m % 2 else "dve"))
                    S.dma(ZL[b, m * 128:(m + 1) * 128, tok0:tok0 + ntok], zt[:, 0:ntok])
            S.barrier()

        def phase_B(b, l):
            R.reset()
            kT = R.alloc("kT", [4, T], BF16)
            vA = R.alloc("vA", [NT, 260], BF16)
            for h in range(4):
                S.dma(kT[0:96, h, :], KT[b, :, h, :])
            for c0 in range(0, NT, 8):
                c1 = min(NT, c0 + 8)
                S.dma(vA[:, c0:c1, :], VA[b, c0 * 128:c1 * 128, :].re("(c p) n -> p c n", p=128))
            qTb = [R.alloc("qTb%d" % i, [4, 512], BF16) for i in range(2)]
            pt = [R.alloc("pt%d" % i, [512], BF16) for i in range(4)]
            usb = [R.alloc("usb%d" % i, [512], F32) for i in range(2)]
            ot = [R.alloc("ot%d" % i, [4, 64], F32) for i in range(2)]
            rc = [R.alloc("rc%d" % i, [4], F32) for i in range(2)]
            chunks = []
            if l < depth - 1:
                chunks.append((0, 256, 2))
            for c in range(SEQ // 512):
                chunks.append((256 + c * 512, 512, NT))
            Sb = [PB[0], PB[1], PB[2], PB[3]]
            Ub = [PB[4], PB[5]]
            Tb = [PB[6], PB[7]]
            S.dma(qTb[0][0:96, :, 0:chunks[0][1]], QT[b, :, :, chunks[0][0]:chunks[0][0] + chunks[0][1]])
            cnt = 0
            ui = 0
            for ci, (q0, nq, nkc) in enumerate(chunks):
                qt = qTb[ci % 2]
                if ci + 1 < len(chunks):
                    nq0, nnq, _ = chunks[ci + 1]
                    S.dma(qTb[(ci + 1) % 2][0:96, :, 0:nnq], QT[b, :, :, nq0:nq0 + nnq])
                for h in range(4):
                    U = Ub[ui % 2]
                    def score(c):
                        sb = Sb[(cnt + c) % 4]
                        S.mm(sb[:, 0:nq], kT[0:96, h, c * 128:(c + 1) * 128], qt[0:96, h, 0:nq])
                        p = pt[(cnt + c) % 4]
                        S.act(p[:, 0:nq], sb[:, 0:nq], AF.Exp, scale=MLA_SCALE)
                        return p
                    ps = {0: score(0)}
                    if nkc > 1:
                        ps[1] = score(1)
                    for c in range(nkc):
                        if c + 2 < nkc:
                            ps[c + 2] = score(c + 2)
                        S.mm(U[0:65, 0:nq], vA[:, c, h * 65:(h + 1) * 65], ps[c][:, 0:nq],
                             start=(c == 0), stop=(c == nkc - 1))
                        del ps[c]
                    cnt += nkc
                    us = usb[ui % 2]
                    S.copy(us[0:65, 0:nq], U[0:65, 0:nq], eng="dve")
                    tb = Tb[ui % 2]
                    nsub = nq // 128
                    for s in range(nsub):
                        S.transpose(tb[:, s * 65:(s + 1) * 65], us[0:65, s * 128:(s + 1) * 128], identF[0:65, 0:65])
                    t3 = tb[:, 0:nsub * 65].re("p (s d) -> p s d", s=nsub)
                    r = rc[ui % 2]
                    S.recip(r[:, 0:nsub], t3[:, :, 64])
                    o = ot[ui % 2]
                    S.tt(o[:, 0:nsub, :], t3[:, :, 0:64], r[:, 0:nsub].bcast(2, [128, nsub, 64]), ALU.mult)
                    S.dma(OMIX[b, q0:q0 + nq, h * 64:(h + 1) * 64].re("(s p) d -> p s d", p=128), o[:, 0:nsub, :])
                    ui += 1
            S.barrier()

        def phase_B2(b, l):
            R.reset()
            sT_ = R.alloc("sqkT_all", [6, T], BF16)
            svA = R.alloc("svA", [NT, 130], BF16)
            for h in range(6):
                S.dma(sT_[0:64, h, :], SQKT[b, :, h, :])
            for c0 in range(0, NT, 8):
                c1 = min(NT, c0 + 8)
                S.dma(svA[:, c0:c1, :], SVA[b, c0 * 128:c1 * 128, :].re("(c p) n -> p c n", p=128))
            esink = R.alloc("esink", [4], F32)
            S.dma(esink, V(sink_d.ap[l].partition_broadcast(128), []))
            S.act(esink, esink, AF.Exp)
            pt = [R.alloc("spt%d" % i, [2, 128], BF16) for i in range(4)]
            usb = [R.alloc("susb%d" % i, [256], F32) for i in range(2)]
            ot = [R.alloc("sot%d" % i, [2, 64], F32) for i in range(2)]
            rc = [R.alloc("src%d" % i, [2], F32) for i in range(2)]
            Sb = [PB[0], PB[1], PB[2], PB[3]]
            Ub = [PB[4], PB[5]]
            Tb = [PB[6], PB[7]]
            qtiles = list(range(2, NT)) if l == depth - 1 else list(range(NT))
            cnt = 0
            ui = 0
            for tq in qtiles:
                if tq < 2:
                    keys = [(0, None), (1, None)]
                else:
                    keys = [(0, None), (1, None)]
                    if tq - 1 >= 2:
                        keys.append((tq - 1, 0))
                    keys.append((tq, None))
                    if tq + 1 < NT:
                        keys.append((tq + 1, 1))
                for g in range(2):
                    U = Ub[ui % 2]
                    q = sT_[0:64, 2 * g:2 * g + 2, tq * 128:(tq + 1) * 128]
                    plist = []
                    for (kc, mk) in keys:
                        sb = Sb[cnt % 4]
                        S.mm(sb[:, 0:256].re("p (h t) -> p h t", h=2), sT_[0:64, 4 + g, kc * 128:(kc + 1) * 128], q)
                        p = pt[cnt % 4]
                        S.act(p, sb[:, 0:256].re("p (h t) -> p h t", h=2), AF.Exp, scale=SWA_SCALE)
                        if mk is not None:
                            S.tt(p, p, maskB[:, mk, :].bcast(1, [128, 2, 128]), ALU.mult, eng="pool")
                        plist.append(p)
                        cnt += 1
                        if len(plist) >= 2:
                            idx = len(plist) - 2
                            kc2 = keys[idx][0]
                            S.mm(U[0:65, 0:256], svA[:, kc2, g * 65:(g + 1) * 65], plist[idx].re("p h t -> p (h t)"),
                                 start=(idx == 0), stop=False)
                    idx = len(plist) - 1
                    S.mm(U[0:65, 0:256], svA[:, keys[idx][0], g * 65:(g + 1) * 65], plist[idx].re("p h t -> p (h t)"),
                         start=(idx == 0), stop=True)
                    us = usb[ui % 2]
                    S.copy(us[0:65, :], U[0:65, 0:256], eng="dve")
                    tb = Tb[ui % 2]
                    for s in range(2):
                        S.transpose(tb[:, s * 65:(s + 1) * 65], us[0:65, s * 128:(s + 1) * 128], identF[0:65, 0:65])
                    t3 = tb[:, 0:130].re("p (s d) -> p s d", s=2)
                    r = rc[ui % 2]
                    S.tt(r, t3[:, :, 64], esink[:, 2 * g:2 * g + 2], ALU.add)
                    S.recip(r, r)
                    o = ot[ui % 2]
                    S.tt(o, t3[:, :, 0:64], r.bcast(2, [128, 2, 64]), ALU.mult)
                    S.dma(OMIX[b, tq * 128:(tq + 1) * 128, 256 + g * 128:256 + (g + 1) * 128], o.re("p s d -> p (s d)"))
                    ui += 1
            S.barrier()

        def phase_C(b, l):
            R.reset()
            ZW = T + 8
            CO, LO = 2, 261
            wst = R.alloc("wst", [4, 4, 128], F32)
            S.memset(wst, 0.0, eng="dve")
            for ty, wd_ in enumerate((lru_wa_d, lru_wx_d)):
                for d in range(2):
                    for half in range(2):
                        src = wd_[l, d].re("(m two) c e -> two c m e", two=2)[half]
                        S.dma(wst[half * 64:(half + 1) * 64, ty * 2 + d, :, half * 64:(half + 1) * 64], src)
            wbd = R.alloc("wbd", [4, 4, 128], BF16)
            S.copy(wbd, wst, eng="dve")
            vT = R.alloc("vT", [3, 2, 4], F32)
            S.dma(vT, lru_vT_d[l])
            cw = R.alloc("cw", [4, 4], F32)
            cb = R.alloc("cb", [4], F32)
            S.dma(cw, conv_wT_d[l])
            S.dma(cb, conv_bT_d[l])
            cneg = R.alloc("cneg", [2, 4], F32)
            S.act(cneg, vT[:, 2], AF.Exp, scale=-1.0)
            S.act(cneg, cneg, AF.Ln, bias=1.0, scale=1.0)
            S.ts(cneg, cneg, -8.0, None, ALU.mult)
            cnh = R.alloc("cnh", [2, 4], F32)
            S.ts(cnh, cneg, 0.5, None, ALU.mult)
            zb = R.alloc("zb", [ZW], F32)
            zbd = zb.sub(Buf("zbdata"))
            S.memset(V(zb.ap, zb.bufs + zbd.bufs), 0.0, eng="pool")
            u = R.alloc("u", [T], F32)
            ub = R.alloc("ub", [T], BF16)
            rr = R.alloc("rr", [T], F32)
            ii = R.alloc("ii", [T], F32)
            tq_ = R.alloc("tq", [T], F32)
            hf = R.alloc("hf", [T], F32)
            hb = R.alloc("hb", [T], F32)
            gz = R.alloc("gz", [T], F32)
            ob = R.alloc("ob", [T], BF16)
            tchunks = [(0, 256)] + [(256 + c * 512, 512) for c in range(SEQ // 512)]
            for m in range(4):
                S.dma(zbd[:, CO:CO + 256], ZL[b, m * 128:(m + 1) * 128, 0:256])
                S.dma(zbd[:, LO:LO + SEQ], ZL[b, m * 128:(m + 1) * 128, 256:T])
                S.dma(gz, ZL[b, 512 + m * 128:512 + (m + 1) * 128, :])
                zr = V(zb.ap, zb.bufs + zbd.bufs)
                for (o0, o1, n) in ((0, 0, 256), (256, 259, SEQ)):
                    S.ts(u[:, o0:o0 + n], zr[:, o1:o1 + n], cw[:, m, 0:1], cb[:, m:m + 1], ALU.mult, ALU.add)
                    for tap in range(1, 4):
                        S.stt(u[:, o0:o0 + n], zr[:, o1 + tap:o1 + tap + n], cw[:, m, tap:tap + 1], u[:, o0:o0 + n],
                              ALU.mult, ALU.add)
                S.copy(ub, u, eng="pool")
                S.act(gz, gz, AF.Gelu_apprx_tanh)
                for d in range(2):
                    for ci, (t0, n) in enumerate(tchunks):
                        pa, px = PB[(ci % 2) * 2], PB[(ci % 2) * 2 + 1]
                        S.mm(pa[:, 0:n], wbd[:, 0 * 2 + d, m, :], ub[:, t0:t0 + n])
                        S.mm(px[:, 0:n], wbd[:, 1 * 2 + d, m, :], ub[:, t0:t0 + n])
                        S.act(rr[:, t0:t0 + n], pa[:, 0:n], AF.Sigmoid, bias=vT[:, 0, d, m:m + 1], scale=1.0)
                        S.act(ii[:, t0:t0 + n], px[:, 0:n], AF.Sigmoid, bias=vT[:, 1, d, m:m + 1], scale=1.0)
                    S.act(tq_, rr, AF.Tanh, scale=cnh[:, d, m:m + 1])
                    S.act(rr, rr, AF.Exp, scale=cneg[:, d, m:m + 1])
                    S.act(tq_, tq_, AF.Sqrt, scale=-1.0)
                    S.stt(tq_, rr, 1.0, tq_, ALU.add, ALU.mult)
                    S.tt(ii, ii, u, ALU.mult, eng="pool")
                    S.tt(ii, ii, tq_, ALU.mult)
                    if d == 0:
                        S.scan(hf, rr, ii, 0.0)
                    else:
                        S.scan(hb[:, 0:256][:, ::-1], rr[:, 0:256][:, ::-1], ii[:, 0:256][:, ::-1], 0.0)
                        S.scan(hb[:, 256:T][:, ::-1], rr[:, 256:T][:, ::-1], ii[:, 256:T][:, ::-1], hb[:, 0:1])
                S.tt(hf, hf, hb, ALU.add)
                S.tt(ob, hf, gz, ALU.mult)
                S.dma(OLRU[b, m * 128:(m + 1) * 128, :], ob)
            S.barrier()

        def phase_DE(b, l, tiles):
            R.reset()
            last = l == depth - 1
            ntl = len(tiles)
            ntok = ntl * 128
            H2T = R.alloc("H2T", [KD, ntok], BF16)
            COMB = R.alloc("COMB", [ntl, 32], F32)
            mark = R.off
            wo_f = R.alloc("wo_f", [KD, D], F32)
            S.dma(wo_f, w_out_d[l].re("(k p) n -> p k n", p=128))
            gg = R.alloc("gg", [KD], F32)
            S.dma(gg, g_grpT_d[l])
            wo = R.alloc("wo", [KD, D], BF16)
            for k in range(KD):
                S.ts(wo[:, k, :], wo_f[:, k, :], gg[:, k:k + 1], None, ALU.mult, eng=("pool" if k % 2 else "dve"))
            wrt = R.alloc("wrt", [KD, 36], F32)
            S.dma(wrt, wr_d[l].re("(k p) n -> p k n", p=128))
            rbt = R.alloc("rbt", [36], F32)
            S.dma(rbt, V(rb_d.ap[l].partition_broadcast(128), []))
            G1 = [R.alloc("G1_%d" % i, [D], F32) for i in range(2)]
            S.dma(G1[0], V(GATES.ap[l, b, 0].partition_broadcast(128), []))
            S.dma(G1[1], V(GATES.ap[l, 2, 0].partition_broadcast(128), []))
            NBUF = 4
            xt = [R.alloc("dxt%d" % i, [D], F32) for i in range(NBUF)]
            om = [R.alloc("om%d" % i, [512], F32) for i in range(NBUF)]
            ol = [R.alloc("ol%d" % i, [4, 128], BF16) for i in range(NBUF)]
            LG = R.alloc("LG", [ntl, 36], F32)
            rowl, rowc = l * 3 + b, l * 3 + 2
            TRs = [Region(arena, R.off + pp * 24576, R.off + (pp + 1) * 24576, cached=True) for pp in range(2)]
            RB = Region(arena, R.off + 2 * 24576, ARENA_BYTES, cached=True)

            def loads(i):
                t = tiles[i]
                tok = slice(t * 128, (t + 1) * 128)
                S.dma(xt[i % NBUF], x_src(b, l, t))
                S.dma(om[i % NBUF], OMIX[b, tok, :])
                S.dma(ol[i % NBUF], OLRU[b, :, tok].re("(m p) t -> p m t", p=128))

            def d_tile(i, t):
                pp = i % 2
                TR = TRs[pp]
                Q = PB[4 * pp:4 * pp + 4]
                PA, PL = psum[2 * pp], psum[2 * pp + 1]
                is_ctx = t < 2
                tok = slice(t * 128, (t + 1) * 128)
                if i + 2 < ntl:
                    loads(i + 2)
                x_t, o_m, o_l = xt[i % NBUF], om[i % NBUF], ol[i % NBUF]
                junk = TR.alloc("djunk", [D], BF16)
                ss2 = TR.alloc("dss2", [2], F32)
                S.act(junk[:, 0:256], o_m[:, 0:256], AF.Square, accum=ss2[:, 0:1])
                S.act(junk[:, 256:512], o_m[:, 256:512], AF.Square, accum=ss2[:, 1:2])
                sq = TR.alloc("sq", [4, 128], F32)
                S.tt(sq, o_l, o_l, ALU.mult, eng="pool")
                yield
                rs2 = TR.alloc("rstd_g", [2], F32)
                S.act(rs2, ss2, AF.Sqrt, bias=EPS, scale=1.0 / 256)
                pss = Q[1]
                for m in range(4):
                    S.mm(pss[:, 0:1], sq[:, m, :], ones1, start=(m == 0), stop=(m == 3))
                yield
                S.recip(rs2, rs2)
                rsl = TR.alloc("rsl", [1], F32)
                S.act(rsl, pss[:, 0:1], AF.Sqrt, bias=EPS, scale=1.0 / 512)
                yield
                on = TR.alloc("on", [512], BF16)
                S.act(on[:, 0:256], o_m[:, 0:256], AF.Identity, scale=rs2[:, 0:1])
                S.act(on[:, 256:512], o_m[:, 256:512], AF.Identity, scale=rs2[:, 1:2])
                S.recip(rsl, rsl)
                yield
                pT = pbf(Q[0])
                for j in range(4):
                    S.transpose(pT[:, j * 128:(j + 1) * 128], on[:, j * 128:(j + 1) * 128], identB)
                yield
                mT = TR.alloc("mT", [4, 128], BF16)
                S.copy(mT, pT[:, 0:512].re("p (j t) -> p j t", j=4), eng="dve")
                yield
                for nh in range(2):
                    for m in range(4):
                        S.mm(PL[nh + 1], o_l[:, m, :], wo[:, 4 + m, nh * 512:(nh + 1) * 512], start=(m == 0), stop=(m == 3))
                for nh in range(2):
                    for j in range(4):
                        S.mm(PA[nh + 1], mT[:, j, :], wo[:, j, nh * 512:(nh + 1) * 512], start=(j == 0), stop=(j == 3))
                yield
                tA = TR.alloc("tA", [D], F32)
                S.copy(tA, PA[0], eng="act")
                yield
                S.stt(tA, PL[0], rsl, tA, ALU.mult, ALU.add)
                yield
                S.tt(tA, tA, G1[1 if is_ctx else 0], ALU.mult, eng="pool")
                yield
                S.tt(x_t, x_t, tA, ALU.add)
                yield
                S.dma(X1S[b, tok, :], x_t)
                ss = TR.alloc("ss", [1], F32)
                S.act(junk, x_t, AF.Square, accum=ss)
                yield
                rstd = TR.alloc("rstd_x", [1], F32)
                S.act(rstd, ss, AF.Sqrt, bias=EPS, scale=1.0 / D)
                yield
                S.recip(rstd, rstd)
                yield
                xn = TR.alloc("xn", [D], F32)
                S.ts(xn, x_t, rstd, None, ALU.mult, eng="pool")
                yield
                A = AB[:, rowc if is_ctx else rowl]
                h2f = TR.alloc("h2f", [KD, 128], F32)
                for half in range(2):
                    pb = Q[half]
                    for kk in range(4):
                        k = half * 4 + kk
                        S.transpose(pb[:, kk * 128:(kk + 1) * 128], xn[:, k * 128:(k + 1) * 128], identF)
                yield
                for half in range(2):
                    pb = Q[half]
                    for kk in range(4):
                        k = half * 4 + kk
                        S.act(h2f[:, k, :], pb[:, kk * 128:(kk + 1) * 128], AF.Identity,
                              bias=A[:, 3, k:k + 1], scale=A[:, 2, k:k + 1])
                yield
                S.copy(H2T[:, :, i * 128:(i + 1) * 128], h2f, eng="pool")
                pr = Q[2]
                for k in range(KD):
                    S.mm(pr[:, 0:36], h2f[:, k, :], wrt[:, k, :], start=(k == 0), stop=(k == KD - 1))
                yield
                S.tt(LG[:, i, :], pr[:, 0:36], rbt, ALU.add)

            for i in range(min(2, ntl)):
                loads(i)
            active = []
            nxt = 0
            while nxt < ntl or active:
                while len(active) < 2 and nxt < ntl:
                    active.append(d_tile(nxt, tiles[nxt]))
                    nxt += 1
                for g_ in list(active):
                    try:
                        next(g_)
                    except StopIteration:
                        active.remove(g_)
            lgG = LG[:, :, 0:4]
            lgE = LG[:, :, 4:36].re("p t (g e) -> p t g e", g=4)
            gmax = RB.alloc("gmax", [ntl], F32)
            S.reduce(gmax, lgG, ALU.max)
            mg = RB.alloc("mg", [ntl, 4], F32)
            S.tt(mg, lgG, gmax.bcast(2, [128, ntl, 4]), ALU.is_equal)
            eg = RB.alloc("eg", [ntl, 4], F32)
            S.tt(eg, lgG, gmax.bcast(2, [128, ntl, 4]), ALU.subtract)
            S.act(eg, eg, AF.Exp)
            pgt = RB.alloc("pgt", [ntl], F32)
            S.reduce(pgt, eg, ALU.add)
            S.recip(pgt, pgt)
            les = RB.alloc("les", [ntl, 8], F32)
            tmp8 = RB.alloc("tmp8", [ntl, 8], F32)
            S.tt(les, lgE[:, :, 0, :], mg[:, :, 0].bcast(2, [128, ntl, 8]), ALU.mult)
            for g in range(1, 4):
                S.tt(tmp8, lgE[:, :, g, :], mg[:, :, g].bcast(2, [128, ntl, 8]), ALU.mult)
                S.tt(les, les, tmp8, ALU.add)
            m1v = RB.alloc("m1v", [ntl], F32)
            S.reduce(m1v, les, ALU.max)
            k1 = RB.alloc("k1", [ntl, 8], F32)
            S.tt(k1, les, m1v.bcast(2, [128, ntl, 8]), ALU.is_equal)
            les2 = RB.alloc("les2", [ntl, 8], F32)
            S.stt(les2, k1, -1e30, les, ALU.mult, ALU.add)
            m2v = RB.alloc("m2v", [ntl], F32)
            S.reduce(m2v, les2, ALU.max)
            k2 = RB.alloc("k2", [ntl, 8], F32)
            S.tt(k2, les2, m2v.bcast(2, [128, ntl, 8]), ALU.is_equal)
            e2 = RB.alloc("e2", [ntl], F32)
            S.tt(e2, m2v, m1v, ALU.subtract)
            S.act(e2, e2, AF.Exp)
            w1 = RB.alloc("w1", [ntl], F32)
            S.ts(w1, e2, 1.0, None, ALU.add)
            S.recip(w1, w1)
            S.tt(w1, w1, pgt, ALU.mult)
            w2 = RB.alloc("w2", [ntl], F32)
            S.tt(w2, w1, e2, ALU.mult)
            cwt = RB.alloc("cwt", [ntl, 8], F32)
            S.tt(cwt, k1, w1.bcast(2, [128, ntl, 8]), ALU.mult)
            S.tt(tmp8, k2, w2.bcast(2, [128, ntl, 8]), ALU.mult)
            S.tt(cwt, cwt, tmp8, ALU.add)
            for g in range(4):
                S.tt(COMB[:, :, g * 8:(g + 1) * 8], cwt, mg[:, :, g].bcast(2, [128, ntl, 8]), ALU.mult)
            S.barrier()
            R.off = mark
            Y = R.alloc("Y", [ntl, D], F32)
            wg = [R.alloc("wg%d" % i, [KD, DEXP], BF16) for i in range(2)]
            wu = [R.alloc("wu%d" % i, [KD, DEXP], BF16) for i in range(2)]
            wd = [R.alloc("wd%d" % i, [2, D], BF16) for i in range(2)]
            sgl = [R.alloc("sgl%d" % i, [512], F32) for i in range(2)]
            aT = [R.alloc("aT%d" % i, [2, 512], BF16) for i in range(2)]
            Yb = [Y[:, i, :].sub(Buf("Y%d" % i)) for i in range(ntl)]

            def wload(e):
                S.dma(wg[e % 2], weg_d[l, e].re("(k p) n -> p k n", p=128), q="pool")
                S.dma(wu[e % 2], weu_d[l, e].re("(k p) n -> p k n", p=128), q="pool")
                S.dma(wd[e % 2], wed_d[l, e].re("(k p) n -> p k n", p=128), q="pool")
            wload(0)
            chunks = []
            c0 = 0
            while c0 < ntok:
                n = min(512, ntok - c0)
                chunks.append((c0, n))
                c0 += n
            it = 0
            for e in range(NEXP):
                if e + 1 < NEXP:
                    wload(e + 1)
                g_w, u_w, d_w = wg[e % 2], wu[e % 2], wd[e % 2]
                for (c0, n) in chunks:
                    a_t = aT[it % 2]
                    for half in range(2):
                        pg_, pu_ = PB[half * 2], PB[half * 2 + 1]
                        for k in range(KD):
                            S.mm(pg_[:, 0:n], g_w[:, k, half * 128:(half + 1) * 128], H2T[:, k, c0:c0 + n],
                                 start=(k == 0), stop=(k == KD - 1))
                        for k in range(KD):
                            S.mm(pu_[:, 0:n], u_w[:, k, half * 128:(half + 1) * 128], H2T[:, k, c0:c0 + n],
                                 start=(k == 0), stop=(k == KD - 1))
                        sg_ = sgl[half]
                        S.act(sg_[:, 0:n], pg_[:, 0:n], AF.Silu)
                        S.tt(a_t[:, half, 0:n], sg_[:, 0:n], pu_[:, 0:n], ALU.mult)
                    for s in range(n // 128):
                        ti = c0 // 128 + s
                        pd = psum[2 + (ti % 2)]
                        for nh in range(2):
                            for half in range(2):
                                S.mm(pd[nh + 1], a_t[:, half, s * 128:(s + 1) * 128], d_w[:, half, nh * 512:(nh + 1) * 512],
                                     start=(half == 0), stop=(half == 1))
                        if e == 0:
                            S.ts(Yb[ti], pd[0], COMB[:, ti, e:e + 1], None, ALU.mult)
                        else:
                            S.stt(Yb[ti], pd[0], COMB[:, ti, e:e + 1], Yb[ti], ALU.mult, ALU.add)
                    it += 1
            G2 = [R.alloc("G2_%d" % i, [D], F32) for i in range(2)]
            S.dma(G2[0], V(GATES.ap[l, b, 1].partition_broadcast(128), []))
            S.dma(G2[1], V(GATES.ap[l, 2, 1].partition_broadcast(128), []))
            x1 = [R.alloc("x1_%d" % i, [D], F32) for i in range(2)]
            ejunk = R.alloc("ejunk", [D], BF16)
            if last:
                GF = R.alloc("GF", [D], F32)
                S.dma(GF, V(gfin_d.ap.partition_broadcast(128), []))
            TR = Region(arena, R.off, ARENA_BYTES, cached=True)
            S.dma(x1[0], X1S[b, tiles[0] * 128:(tiles[0] + 1) * 128, :])
            for i, t in enumerate(tiles):
                TR.reset()
                tok = slice(t * 128, (t + 1) * 128)
                if i + 1 < ntl:
                    S.dma(x1[(i + 1) % 2], X1S[b, tiles[i + 1] * 128:(tiles[i + 1] + 1) * 128, :])
                x_t = x1[i % 2]
                S.tt(Yb[i], Yb[i], G2[1 if t < 2 else 0], ALU.mult, eng="pool")
                S.tt(x_t, x_t, Yb[i], ALU.add)
                if not last:
                    S.dma(XS[b, tok, :], x_t)
                else:
                    ss = TR.alloc("ess", [1], F32)
                    S.act(ejunk, x_t, AF.Square, accum=ss)
                    rstd = rms_rstd(S, TR, ss, D, 1, "f")
                    S.stt(x_t, x_t, rstd, GF, ALU.mult, ALU.mult)
                    S.dma(out_d[b, (t - 2) * 128:(t - 1) * 128, :], x_t, is_out=True)
            S.barrier()

        for b in range(NB):
            for l in range(depth if nlayers is None else nlayers):
                if "A" in plan:
                    phase_A(b, l)
                if "B" in plan:
                    phase_B(b, l)
                if "S" in plan:
                    phase_B2(b, l)
                if "C" in plan:
                    phase_C(b, l)
                if "D" not in plan:
                    continue
                if l == depth - 1:
                    tl = list(range(2, NT))
                else:
                    tl = list(range(NT))
                nblk = (len(tl) + 16) // 17
                per = (len(tl) + nblk - 1) // nblk
                for i in range(nblk):
                    blk = tl[i * per:(i + 1) * per]
                    if blk:
                        phase_DE(b, l, blk)
        if debug:
            print("total ops", S.nadd)
        S.emit()
    return nc


def _perm_rot(nheads, hd, base=0):
    q = hd // 4
    idx = []
    for h in range(nheads):
        o = base + h * hd
        idx += list(range(o, o + q)) + list(range(o + 2 * q, o + 3 * q)) + list(range(o + q, o + 2 * q)) + \
            list(range(o + 3 * q, o + 4 * q))
    return idx


def _chan_major(v, nch):
    return np.ascontiguousarray(np.swapaxes(v.reshape(v.shape[:-1] + (nch, 128)), -1, -2))


def host_shared(inp, SEQ):
    f = np.float32
    depth = inp["w_ada"].shape[0]
    sh = {}
    sh["w_ada"] = np.ascontiguousarray(inp["w_ada"], f)
    sh["b_ada"] = np.ascontiguousarray(inp["b_ada"], f)
    sh["b_adaT"] = _chan_major(inp["b_ada"].astype(f), 48)
    sh["g1T"] = _chan_major(inp["g_norm1"].astype(f), 8)
    sh["g2T"] = _chan_major(inp["g_norm2"].astype(f), 8)
    cols = list(range(0, 384)) + _perm_rot(1, 32, 384) + _perm_rot(4, 64, 416) + _perm_rot(2, 64, 672) + \
        list(range(800, 1952))
    sh["w_in_p"] = np.ascontiguousarray(inp["w_in"][:, :, cols], f)
    sh["g_cqT"] = _chan_major(inp["g_cq"].astype(f), 2)
    sh["g_ckvT"] = _chan_major(inp["g_ckv"].astype(f), 1)
    qcols = []
    for h in range(4):
        qcols += list(range(h * 96, h * 96 + 64)) + _perm_rot(1, 32, h * 96 + 64)
    sh["w_uq_p"] = np.ascontiguousarray(inp["w_uq"][:, :, qcols], f)
    kvcols = [h * 128 + i for h in range(4) for i in range(64)] + [h * 128 + 64 + i for h in range(4) for i in range(64)]
    sh["w_ukv_p"] = np.ascontiguousarray(inp["w_ukv"][:, :, kvcols], f)
    sh["sink"] = np.ascontiguousarray(inp["swa_sink"], f)
    cw = inp["conv_w"].astype(f)
    sh["conv_wT"] = np.ascontiguousarray(np.transpose(cw.reshape(depth, 4, 4, 128), (0, 3, 2, 1)))
    sh["conv_bT"] = _chan_major(inp["conv_b"].astype(f), 4)
    sh["lru_wa"] = np.ascontiguousarray(inp["lru_wa"], f)
    sh["lru_wx"] = np.ascontiguousarray(inp["lru_wx"], f)
    v = np.stack([inp["lru_ba"], inp["lru_bx"], inp["lru_lam"]], axis=1).astype(f)
    sh["lru_vT"] = np.ascontiguousarray(np.transpose(v.reshape(depth, 3, 2, 4, 128), (0, 4, 1, 2, 3)))
    sh["g_grpT"] = _chan_major(inp["g_grp"].astype(f), 8)
    sh["w_out"] = np.ascontiguousarray(inp["w_out"], f)
    wg2 = np.transpose(inp["w_g2"], (0, 2, 1, 3)).reshape(depth, D, 32)
    sh["wr"] = np.ascontiguousarray(np.concatenate([inp["w_g1"], wg2], axis=-1), f)
    sh["rb"] = np.ascontiguousarray(np.concatenate([inp["b_g1"], inp["b_g2"].reshape(depth, 32)], axis=-1), f)
    sh["w_e_gate"] = np.ascontiguousarray(inp["w_e_gate"], f)
    sh["w_e_up"] = np.ascontiguousarray(inp["w_e_up"], f)
    sh["w_e_down"] = np.ascontiguousarray(inp["w_e_down"], f)
    sh["g_final"] = np.ascontiguousarray(inp["g_final"], f)
    sh["ident"] = np.eye(128, dtype=f)
    j = np.arange(128)[:, None]
    i = np.arange(128)[None, :]
    sh["masks"] = np.ascontiguousarray(np.stack([(j >= i), (j <= i)], axis=1).astype(f))
    pos = np.arange(SEQ)
    rows = (pos // 64).astype(np.float32)
    colsp = (pos % 64).astype(np.float32)

    def tab(half):
        fr = (np.float32(10000.0) ** (-np.arange(half, dtype=np.float32) / np.float32(half))).astype(np.float32)
        ar = rows[:, None] * fr[None, :]
        ac = colsp[:, None] * fr[None, :]
        C = np.concatenate([np.cos(ar), np.cos(ac)], axis=1)
        Sn = np.concatenate([np.sin(ar), np.sin(ac)], axis=1)
        return C.astype(f), Sn.astype(f)
    Cm, Sm = tab(8)
    Cs, Ss = tab(16)
    tabs = np.concatenate([Cm, Sm, Cs, Ss], axis=1).reshape(SEQ // 128, 128, 96)
    ident_tab = np.concatenate([np.ones((128, 16), f), np.zeros((128, 16), f), np.ones((128, 32), f), np.zeros((128, 32), f)], axis=1)
    sh["rope"] = np.ascontiguousarray(np.concatenate([tabs, ident_tab[None]], axis=0))
    return sh


def host_core(inp, b0, NB):
    f = np.float32
    cv = np.stack([inp["c"][b0], inp["c"][min(b0 + 1, inp["c"].shape[0] - 1)], inp["c_ctx"]], axis=0).astype(f)
    cT = np.ascontiguousarray(np.transpose(cv.reshape(3, KD, 128), (2, 1, 0)))
    return {
        "x": np.ascontiguousarray(inp["x"][b0:b0 + NB], f),
        "ctx": np.ascontiguousarray(inp["ctx"][b0:b0 + NB], f),
        "cT": cT,
    }


_NC_CACHE = {}


def kernel(**inputs):
    inp = {k: np.asarray(v) for k, v in inputs.items()}
    B, SEQ, _ = inp["x"].shape
    ncores = 8
    NB = B // ncores
    key = (SEQ, NB)
    if key not in _NC_CACHE:
        _NC_CACHE[key] = build(SEQ, NB)
    nc = _NC_CACHE[key]
    sh = host_shared(inp, SEQ)
    in_maps = []
    for c in range(ncores):
        m = dict(sh)
        m.update(host_core(inp, c * NB, NB))
        in_maps.append(m)
    res = run_bass_kernel_spmd(nc, in_maps, core_ids=list(range(ncores)))
    out = np.concatenate([np.asarray(r["out"]) for r in res.results], axis=0)
    return out.astype(np.float32)
```

```python
import numpy as np
from contextlib import ExitStack
import concourse.bass as bass
import concourse.mybir as mybir
from concourse.bass_utils import run_bass_kernel_spmd

F32 = mybir.dt.float32
BF16 = mybir.dt.bfloat16
AF = mybir.ActivationFunctionType
ALU = mybir.AluOpType
AX = mybir.AxisListType

D = 1024
KD = 8
LCTX = 256
EPS = 1e-6
NEXP = 32
DEXP = 256
MLA_SCALE = 96 ** -0.5
SWA_SCALE = 0.125

COMPUTE = ("pe", "act", "dve", "pool")
DMAQ = ("sp", "pool")
NDMASEM = 12


class Buf:
    __slots__ = ("w", "r", "name", "excl")

    def __init__(self, name="", excl=False):
        self.w = None
        self.r = {}
        self.name = name
        self.excl = excl


class V:
    __slots__ = ("ap", "bufs")

    def __init__(self, ap, bufs):
        self.ap = ap
        self.bufs = bufs

    def __getitem__(self, idx):
        return V(self.ap[idx], self.bufs)

    def re(self, pat, **kw):
        return V(self.ap.rearrange(pat, **kw), self.bufs)

    def bcast(self, axis, shape):
        return V(self.ap.unsqueeze(axis).broadcast_to(list(shape)), self.bufs)

    def sub(self, buf):
        return V(self.ap, [buf])


class Op:
    __slots__ = ("eng", "fn", "deps", "sig", "cnt", "dma", "dsem", "dtgt", "idx")

    def __init__(self, eng, fn, dma):
        self.eng = eng
        self.fn = fn
        self.dma = dma
        self.deps = None
        self.sig = False
        self.cnt = 0
        self.dsem = None
        self.dtgt = 0


class Sched:
    def __init__(self, nc, es):
        self.nc = nc
        self.es = es
        self.ops = {e: [] for e in ("pe", "act", "dve", "pool", "sp")}
        self.dma_hist = {q: [None] * NDMASEM for q in DMAQ}
        self.dma_cnt = {q: [0] * NDMASEM for q in DMAQ}
        self.dma_rr = {q: 0 for q in DMAQ}
        self.sems = {}
        self.dsems = {}
        self.out_dmas = []
        self.bar = set()
        self.last = {e: None for e in COMPUTE}
        self.nadd = 0
        self.limit = None
        self.trace = None

    def dram(self, name, shape, dtype, kind="Internal"):
        t = self.nc.dram_tensor(name, list(shape), dtype, kind=kind)
        return V(t.ap(), [])

    def barrier(self):
        b = set()
        for e in COMPUTE:
            if self.last[e] is not None:
                b.add(self.last[e])
        for q in DMAQ:
            for o in self.dma_hist[q]:
                if o is not None:
                    b.add(o)
        for o in b:
            o.sig = True
        self.bar = b

    def add(self, eng, fn, reads=(), writes=(), dma=False):
        op = Op(eng, fn, dma)
        self.nadd += 1
        op.idx = self.nadd
        if self.trace is not None and self.trace[0] <= self.nadd <= self.trace[1]:
            import traceback
            fr = [f for f in traceback.extract_stack() if f.name not in ("add",)][-2:]
            print("OP", self.nadd, eng, "dma" if dma else "", [(f.lineno, f.line) for f in fr][-1])
        if self.limit is not None and self.nadd > self.limit:
            op.deps = set()
            return op
        deps = set(self.bar)
        for v in reads:
            for b in v.bufs:
                if b.w is not None:
                    deps.add(b.w)
                if b.excl:
                    for key, r in b.r.items():
                        if key != eng and not isinstance(r, list):
                            deps.add(r)
        for v in writes:
            for b in v.bufs:
                if b.w is not None:
                    deps.add(b.w)
                for r in b.r.values():
                    if isinstance(r, list):
                        deps.update(r)
                    elif r.eng != eng or r.dma or dma:
                        deps.add(r)
        if dma:
            q = eng
            slot = self.dma_rr[q] % NDMASEM
            self.dma_rr[q] += 1
            prev = self.dma_hist[q][slot]
            if prev is not None:
                deps.add(prev)
            self.dma_cnt[q][slot] += 1
            op.dsem = (q, slot)
            op.dtgt = 16 * self.dma_cnt[q][slot]
            self.dma_hist[q][slot] = op
        deps.discard(op)
        if eng == "pe":
            deps = {d for d in deps if d.dma or d.eng != "pe"}
        op.deps = deps
        for d in deps:
            d.sig = True
        for v in reads:
            for b in v.bufs:
                if dma:
                    b.r.setdefault(("dma", eng), []).append(op)
                else:
                    b.r[eng] = op
        for v in writes:
            for b in v.bufs:
                b.w = op
                b.r = {}
        self.ops[eng].append(op)
        if not dma:
            self.last[eng] = op
        return op

    def emit(self):
        nc = self.nc
        es = self.es
        import os as _os2
        for _i in range(int(_os2.environ.get("KDUMMYSEM", "0"))):
            es.enter_context(nc.semaphore("dummy%d" % _i))
        for e in COMPUTE:
            self.sems[e] = es.enter_context(nc.semaphore("s_" + e))
        for q in DMAQ:
            for i in range(NDMASEM):
                self.dsems[(q, i)] = es.enter_context(nc.semaphore("d_%s%d" % (q, i)))
        for e in COMPUTE:
            c = 0
            for op in self.ops[e]:
                if op.dma:
                    continue
                if op.sig:
                    c += 1
                    op.cnt = c
            assert c < 65000, (e, c)
        final_waits = list(self.out_dmas)
        block = es.enter_context(nc.Block())
        sched = self

        def run(ename, eng):
            waited = {e: 0 for e in COMPUTE}
            dwaited = {}
            for op in sched.ops[ename]:
                need = {}
                if sched.trace is not None and sched.trace[0] <= op.idx <= sched.trace[1]:
                    print("EMIT", op.idx, ename, "cnt", op.cnt, "sig", op.sig, "deps", sorted((d.eng, d.idx, d.cnt, d.dtgt) for d in op.deps))
                for d in op.deps:
                    if d.dma:
                        if dwaited.get(d.dsem, 0) < d.dtgt:
                            dwaited[d.dsem] = d.dtgt
                            eng.wait_ge(sched.dsems[d.dsem], d.dtgt)
                    else:
                        if d.cnt > need.get(d.eng, 0):
                            need[d.eng] = d.cnt
                for se, c in need.items():
                    if c > waited[se]:
                        waited[se] = c
                        eng.wait_ge(sched.sems[se], c)
                inst = op.fn(eng)
                if op.dma:
                    inst.then_inc(sched.dsems[op.dsem], 16)
                elif op.sig:
                    inst.then_inc(sched.sems[ename], 1)
            if ename == "sp":
                for d in final_waits:
                    eng.wait_ge(sched.dsems[d.dsem], d.dtgt)

        @block.tensor
        def _(eng):
            run("pe", eng)

        @block.scalar
        def _(eng):
            run("act", eng)

        @block.vector
        def _(eng):
            run("dve", eng)

        @block.gpsimd
        def _(eng):
            run("pool", eng)

        @block.sync
        def _(eng):
            run("sp", eng)

    def dma(self, out, in_, q="sp", is_out=False):
        op = self.add(q, lambda e: e.dma_start(out=out.ap, in_=in_.ap), reads=[in_], writes=[out], dma=True)
        if is_out:
            self.out_dmas.append(op)
        return op

    def mm(self, out, lhsT, rhs, start=True, stop=True):
        return self.add("pe", lambda e: e.matmul(out.ap, lhsT.ap, rhs.ap, start=start, stop=stop),
                        reads=[lhsT, rhs], writes=[out])

    def transpose(self, out, in_, ident):
        return self.add("pe", lambda e: e.transpose(out.ap, in_.ap, ident.ap), reads=[in_, ident], writes=[out])

    def act(self, out, in_, func, bias=None, scale=None, accum=None):
        reads = [in_]
        kw = {}
        if bias is not None:
            if isinstance(bias, V):
                reads.append(bias)
                kw["bias"] = bias.ap
            else:
                kw["bias"] = bias
        if scale is not None:
            if isinstance(scale, V):
                reads.append(scale)
                kw["scale"] = scale.ap
            else:
                kw["scale"] = scale
        writes = [out]
        if accum is not None:
            kw["accum_out"] = accum.ap
            writes.append(accum)
        return self.add("act", lambda e: e.activation(out.ap, in_.ap, func, **kw), reads=reads, writes=writes)

    def tt(self, out, a, b, op, eng="dve"):
        return self.add(eng, lambda e: e.tensor_tensor(out.ap, a.ap, b.ap, op), reads=[a, b], writes=[out])

    def ts(self, out, a, s1, s2, op0, op1=None, eng="dve"):
        reads = [a]
        a1 = s1.ap if isinstance(s1, V) else s1
        a2 = s2.ap if isinstance(s2, V) else s2
        if isinstance(s1, V):
            reads.append(s1)
        if isinstance(s2, V):
            reads.append(s2)
        if op1 is None:
            return self.add(eng, lambda e: e.tensor_scalar(out.ap, a.ap, a1, None, op0), reads=reads, writes=[out])
        return self.add(eng, lambda e: e.tensor_scalar(out.ap, a.ap, a1, a2, op0, op1), reads=reads, writes=[out])

    def stt(self, out, a, s, b, op0, op1):
        reads = [a, b]
        a1 = s.ap if isinstance(s, V) else s
        if isinstance(s, V):
            reads.append(s)
        return self.add("dve", lambda e: e.scalar_tensor_tensor(out.ap, a.ap, a1, b.ap, op0, op1),
                        reads=reads, writes=[out])

    def copy(self, out, in_, eng="dve"):
        if eng == "act":
            return self.add("act", lambda e: e.copy(out.ap, in_.ap), reads=[in_], writes=[out])
        return self.add(eng, lambda e: e.tensor_copy(out.ap, in_.ap), reads=[in_], writes=[out])

    def memset(self, out, val, eng="pool"):
        return self.add(eng, lambda e: e.memset(out.ap, val), reads=[], writes=[out])

    def scan(self, out, d0, d1, init):
        reads = [d0, d1]
        i = init.ap if isinstance(init, V) else init
        if isinstance(init, V):
            reads.append(init)
        return self.add("dve", lambda e: e.tensor_tensor_scan(out.ap, d0.ap, d1.ap, i, ALU.mult, ALU.add),
                        reads=reads, writes=[out])

    def recip(self, out, in_):
        return self.add("dve", lambda e: e.reciprocal(out.ap, in_.ap), reads=[in_], writes=[out])

    def reduce(self, out, in_, op):
        return self.add("dve", lambda e: e.tensor_reduce(out.ap, in_.ap, AX.X, op), reads=[in_], writes=[out])

    def max8(self, out, in_):
        return self.add("dve", lambda e: e.max(out.ap, in_.ap), reads=[in_], writes=[out])


class Region:
    def __init__(self, arena_ap, start, end, cached=False):
        self.ap = arena_ap
        self.cached = cached
        self.cache = {}
        self.start = start
        self.end = end
        self.off = start

    def reset(self):
        if not self.cached:
            self.off = self.start

    def alloc(self, name, free_shape, dtype):
        if self.cached and name in self.cache:
            return self.cache[name]
        v = self._alloc(name, free_shape, dtype)
        if self.cached:
            self.cache[name] = v
        return v

    def _alloc(self, name, free_shape, dtype):
        n = 1
        for s in free_shape:
            n *= s
        esz = 4 if dtype == F32 else 2
        nb = (n * esz + 63) // 64 * 64
        assert self.off + nb <= self.end, ("sbuf region overflow", name, self.off, nb, self.end)
        a = self.ap[:, self.off // 4:(self.off + nb) // 4]
        self.off += nb
        if dtype != F32:
            a = a.bitcast(dtype)
        a = a[:, 0:n]
        if len(free_shape) == 2:
            a = a.rearrange("p (a b) -> p a b", a=free_shape[0])
        elif len(free_shape) == 3:
            a = a.rearrange("p (a b c) -> p a b c", a=free_shape[0], b=free_shape[1])
        return V(a, [Buf(name)])


def rms_rstd(S, R, ss, n, width, tag):
    r = R.alloc("rstd_" + tag, [width], F32)
    S.act(r, ss, AF.Sqrt, bias=EPS, scale=1.0 / n)
    S.recip(r, r)
    return r


def build(SEQ, NB, depth=2, debug=False, plan="ABSCD", nlayers=None):
    T = LCTX + SEQ
    NT = T // 128
    NLT = SEQ // 128
    nc = bass.Bass("TRN2", target_bir_lowering=False)
    es = ExitStack()
    with es:
        S = Sched(nc, es)
        import os as _os
        if _os.environ.get("KTRACE"):
            S.trace = tuple(int(v) for v in _os.environ["KTRACE"].split(","))
        if _os.environ.get("KLIMIT"):
            S.limit = int(_os.environ["KLIMIT"])
        okind = "ExternalOutput" if debug else "Internal"
        x_d = S.dram("x", [NB, SEQ, D], F32, "ExternalInput")
        ctx_d = S.dram("ctx", [NB, LCTX, D], F32, "ExternalInput")
        cT_d = S.dram("cT", [128, KD, 3], F32, "ExternalInput")
        w_ada_d = S.dram("w_ada", [depth, D, 6 * D], F32, "ExternalInput")
        b_adaT_d = S.dram("b_adaT", [depth, 128, 48], F32, "ExternalInput")
        b_ada_d = S.dram("b_ada", [depth, 6 * D], F32, "ExternalInput")
        g1T_d = S.dram("g1T", [depth, 128, KD], F32, "ExternalInput")
        g2T_d = S.dram("g2T", [depth, 128, KD], F32, "ExternalInput")
        w_in_d = S.dram("w_in_p", [depth, D, 1952], F32, "ExternalInput")
        g_cqT_d = S.dram("g_cqT", [depth, 128, 2], F32, "ExternalInput")
        g_ckvT_d = S.dram("g_ckvT", [depth, 128, 1], F32, "ExternalInput")
        w_uq_d = S.dram("w_uq_p", [depth, 256, 384], F32, "ExternalInput")
        w_ukv_d = S.dram("w_ukv_p", [depth, 128, 512], F32, "ExternalInput")
        sink_d = S.dram("sink", [depth, 4], F32, "ExternalInput")
        conv_wT_d = S.dram("conv_wT", [depth, 128, 4, 4], F32, "ExternalInput")
        conv_bT_d = S.dram("conv_bT", [depth, 128, 4], F32, "ExternalInput")
        lru_wa_d = S.dram("lru_wa", [depth, 2, 8, 64, 64], F32, "ExternalInput")
        lru_wx_d = S.dram("lru_wx", [depth, 2, 8, 64, 64], F32, "ExternalInput")
        lru_vT_d = S.dram("lru_vT", [depth, 128, 3, 2, 4], F32, "ExternalInput")
        g_grpT_d = S.dram("g_grpT", [depth, 128, KD], F32, "ExternalInput")
        w_out_d = S.dram("w_out", [depth, D, D], F32, "ExternalInput")
        wr_d = S.dram("wr", [depth, D, 36], F32, "ExternalInput")
        rb_d = S.dram("rb", [depth, 36], F32, "ExternalInput")
        weg_d = S.dram("w_e_gate", [depth, NEXP, D, DEXP], F32, "ExternalInput")
        weu_d = S.dram("w_e_up", [depth, NEXP, D, DEXP], F32, "ExternalInput")
        wed_d = S.dram("w_e_down", [depth, NEXP, DEXP, D], F32, "ExternalInput")
        gfin_d = S.dram("g_final", [D], F32, "ExternalInput")
        ident_d = S.dram("ident", [128, 128], F32, "ExternalInput")
        masks_d = S.dram("masks", [128, 2, 128], F32, "ExternalInput")
        rope_d = S.dram("rope", [NLT + 1, 128, 96], F32, "ExternalInput")
        out_d = S.dram("out", [NB, SEQ, D], F32, "ExternalOutput")
        GATES = S.dram("s_gates", [depth, 3, 2, D], F32, okind)
        XS = S.dram("s_xs", [NB, T, D], F32, okind)
        X1S = S.dram("s_x1s", [NB, T, D], F32, okind)
        ZL = S.dram("s_zl", [NB, 1024, T], F32, okind)
        QT = S.dram("s_qt", [NB, 96, 4, T], BF16, okind)
        KT = S.dram("s_kt", [NB, 96, 4, T], BF16, okind)
        VA = S.dram("s_va", [NB, T, 260], BF16, okind)
        SQKT = S.dram("s_sqkt", [NB, 64, 6, T], BF16, okind)
        SVA = S.dram("s_sva", [NB, T, 130], BF16, okind)
        OMIX = S.dram("s_omix", [NB, T, 512], F32, okind)
        OLRU = S.dram("s_olru", [NB, 512, T], BF16, okind)

        ARENA_BYTES = 190 * 1024
        arena_t = es.enter_context(nc.sbuf_tensor("arena", [128, ARENA_BYTES // 4], F32))
        arena = arena_t[:]
        PERS_BYTES = 12 * 1024
        P = Region(arena, 0, PERS_BYTES)
        R = Region(arena, PERS_BYTES, ARENA_BYTES)
        psum = []
        for i in range(4):
            t = es.enter_context(nc.psum_tensor("ps%d" % i, [128, 1024], F32))
            a = t[:]
            b0, b1 = Buf("ps%da" % i, excl=True), Buf("ps%db" % i, excl=True)
            psum.append((V(a, [b0, b1]), V(a[:, 0:512], [b0]), V(a[:, 512:1024], [b1])))
        PB = []
        for pr in psum:
            PB.append(pr[1])
            PB.append(pr[2])

        def pbf(bank):
            return V(bank.ap.bitcast(BF16), bank.bufs)

        identF = P.alloc("identF", [128], F32)
        identB = P.alloc("identB", [128], BF16)
        maskB = P.alloc("maskB", [2, 128], BF16)
        ones1 = P.alloc("ones1", [1], F32)
        modT = P.alloc("modT", [depth, 48, 3], F32)
        AB = P.alloc("AB", [depth * 3, 4, KD], F32)
        sT = P.alloc("sT", [KD, 3], F32)
        S.dma(identF, ident_d)
        S.copy(identB, identF, eng="dve")
        S.dma(sT, cT_d)
        S.memset(ones1, 1.0, eng="dve")
        R.reset()
        mtmp = R.alloc("mtmp", [2, 128], F32)
        S.dma(mtmp, masks_d)
        S.copy(maskB, mtmp, eng="dve")
        S.act(sT, sT, AF.Silu)
        wab = [R.alloc("wab%d" % i, [KD, 512], F32) for i in range(2)]
        brow = [R.alloc("brow%d" % i, [512], F32) for i in range(2)]
        grow = [R.alloc("grow%d" % i, [512], F32) for i in range(2)]
        badaT = R.alloc("badaT", [depth, 48], F32)
        gT = R.alloc("gT", [depth, 2, KD], F32)
        for l in range(depth):
            S.dma(badaT[:, l, :], b_adaT_d[l])
            S.dma(gT[:, l, 0, :], g1T_d[l])
            S.dma(gT[:, l, 1, :], g2T_d[l])
        it = 0
        for l in range(depth):
            for j in range(12):
                w = wab[it % 2]
                S.dma(w, w_ada_d[l][:, j * 512:(j + 1) * 512].re("(k p) n -> p k n", p=128))
                vec = j // 2
                if vec in (2, 5):
                    br = brow[it % 2]
                    S.dma(br[0:3, :], V(b_ada_d.ap[l, j * 512:(j + 1) * 512].partition_broadcast(3), []))
                    pm = PB[it % 2]
                    for k in range(KD):
                        S.mm(pm[0:3, :], sT[:, k, :], w[:, k, :], start=(k == 0), stop=(k == KD - 1))
                    gr = grow[it % 2]
                    S.tt(gr[0:3, :], pm[0:3, :], br[0:3, :], ALU.add)
                    S.dma(GATES[l, :, 0 if vec == 2 else 1, (j % 2) * 512:(j % 2 + 1) * 512], gr[0:3, :])
                else:
                    for m in range(4):
                        pm = PB[2 + (m % 2)]
                        for k in range(KD):
                            S.mm(pm[:, 0:3], w[:, k, m * 128:(m + 1) * 128], sT[:, k, :],
                                 start=(k == 0), stop=(k == KD - 1))
                        S.act(modT[:, l, j * 4 + m, :], pm[:, 0:3], AF.Identity,
                              bias=badaT[:, l, j * 4 + m:j * 4 + m + 1], scale=1.0)
                it += 1
            for r in range(3):
                ab = AB[:, l * 3 + r]
                S.ts(ab[:, 0, :], modT[:, l, 8:16, r], 1.0, None, ALU.add)
                S.tt(ab[:, 0, :], ab[:, 0, :], gT[:, l, 0, :], ALU.mult)
                S.copy(ab[:, 1, :], modT[:, l, 0:8, r])
                S.ts(ab[:, 2, :], modT[:, l, 32:40, r], 1.0, None, ALU.add)
                S.tt(ab[:, 2, :], ab[:, 2, :], gT[:, l, 1, :], ALU.mult)
                S.copy(ab[:, 3, :], modT[:, l, 24:32, r])
        S.barrier()
        if debug:
            print("ops after prologue", S.nadd)

        def x_src(b, l, t):
            if l == 0:
                if t < 2:
                    return ctx_d[b, t * 128:(t + 1) * 128, :]
                return x_d[b, (t - 2) * 128:(t - 1) * 128, :]
            return XS[b, t * 128:(t + 1) * 128, :]

        def norm_transpose(xt, A, Bc, hT_dst, tmpR, junk, pbanks, hTf_dst=None):
            ss = tmpR.alloc("ss", [1], F32)
            S.act(junk, xt, AF.Square, accum=ss)
            rstd = rms_rstd(S, tmpR, ss, D, 1, "x")
            xn = tmpR.alloc("xn", [D], F32)
            S.ts(xn, xt, rstd, None, ALU.mult)
            for half in range(2):
                pb = pbanks[half]
                for kk in range(4):
                    k = half * 4 + kk
                    S.transpose(pb[:, kk * 128:(kk + 1) * 128], xn[:, k * 128:(k + 1) * 128], identF)
                for kk in range(4):
                    k = half * 4 + kk
                    S.act(hT_dst[:, k, :], pb[:, kk * 128:(kk + 1) * 128], AF.Identity,
                          bias=Bc[:, k:k + 1], scale=A[:, k:k + 1])
                    if hTf_dst is not None:
                        S.ts(hTf_dst[:, k, :], pb[:, kk * 128:(kk + 1) * 128], A[:, k:k + 1], Bc[:, k:k + 1],
                             ALU.mult, ALU.add)

        def phase_A(b, l):
            R.reset()
            rowl, rowc = l * 3 + b, l * 3 + 2
            w_in = R.alloc("w_in", [KD, 1952], BF16)
            for k in range(KD):
                S.dma(w_in[:, k, :], w_in_d[l][k * 128:(k + 1) * 128, :], q="pool")
            wq_f = R.alloc("wq_f", [2, 384], F32)
            wkv_f = R.alloc("wkv_f", [512], F32)
            gq = R.alloc("gq", [3], F32)
            S.dma(wq_f, w_uq_d[l].re("(j p) n -> p j n", p=128))
            S.dma(wkv_f, w_ukv_d[l])
            S.dma(gq[:, 0:2], g_cqT_d[l])
            S.dma(gq[:, 2:3], g_ckvT_d[l])
            wq = R.alloc("wq", [2, 384], BF16)
            wkv = R.alloc("wkv", [512], BF16)
            for j in range(2):
                S.ts(wq[:, j, :], wq_f[:, j, :], gq[:, j:j + 1], None, ALU.mult)
            S.ts(wkv, wkv_f, gq[:, 2:3], None, ALU.mult)
            NS = 4
            xt = [R.alloc("xt%d" % i, [D], F32) for i in range(NS)]
            rp = [R.alloc("rp%d" % i, [96], F32) for i in range(NS)]
            hTs = [R.alloc("hTs%d" % i, [KD, 512], BF16) for i in range(2)]
            qa = [R.alloc("qa%d" % i, [4, 96], BF16) for i in range(NS)]
            ka = [R.alloc("ka%d" % i, [4, 96], BF16) for i in range(NS)]
            va = [R.alloc("va%d" % i, [4, 65], BF16) for i in range(NS)]
            sqk = [R.alloc("sqk%d" % i, [6, 64], BF16) for i in range(NS)]
            sva = [R.alloc("sva%d" % i, [2, 65], BF16) for i in range(NS)]
            qTt = [R.alloc("qTt%d" % i, [4, 128], BF16) for i in range(NS)]
            kTt = [R.alloc("kTt%d" % i, [4, 128], BF16) for i in range(NS)]
            sqkT = [R.alloc("sqkT%d" % i, [6, 128], BF16) for i in range(NS)]
            zlt = [R.alloc("zlt%d" % i, [512], F32) for i in range(2)]
            for i in range(NS):
                S.memset(va[i][:, :, 64:65], 1.0)
                S.memset(sva[i][:, :, 64:65], 1.0)
            TSZ = 15 * 1024
            TRs = [Region(arena, R.off + i * TSZ, R.off + (i + 1) * TSZ, cached=True) for i in range(NS)]
            assert R.off + NS * TSZ <= ARENA_BYTES, ("phase A sbuf", R.off + NS * TSZ)
            groups = [[0, 1]] + [list(range(2 + 4 * g, 2 + 4 * g + 4)) for g in range(NLT // 4)]
            loaded = set()

            def load_tile(t, slot, what="xr"):
                for w_ in what:
                    if (t, w_) in loaded:
                        continue
                    loaded.add((t, w_))
                    if w_ == "x":
                        S.dma(xt[slot], x_src(b, l, t))
                    else:
                        S.dma(rp[slot], rope_d[NLT if t < 2 else t - 2])

            for ti, t in enumerate(groups[0]):
                load_tile(t, ti)
            if len(groups) > 1:
                for ti, t in enumerate(groups[1]):
                    if ti >= len(groups[0]):
                        load_tile(t, ti)

            def a_tile(gi, ti, t, hT):
                TR = TRs[ti]
                bA, bB = PB[2 * ti], PB[2 * ti + 1]
                is_ctx = t < 2
                A = AB[:, rowc if is_ctx else rowl]
                x_t, rpt = xt[ti], rp[ti]
                hTt = hT[:, :, ti * 128:(ti + 1) * 128]
                junk = TR.alloc("junk", [D], BF16)
                ss = TR.alloc("ss", [1], F32)
                S.act(junk, x_t, AF.Square, accum=ss)
                yield
                rstd = TR.alloc("rstd_x", [1], F32)
                S.act(rstd, ss, AF.Sqrt, bias=EPS, scale=1.0 / D)
                yield
                S.recip(rstd, rstd)
                yield
                xn = TR.alloc("xn", [D], F32)
                S.ts(xn, x_t, rstd, None, ALU.mult)
                if gi + 1 < len(groups) and ti < len(groups[gi + 1]):
                    load_tile(groups[gi + 1][ti], ti, "x")
                yield
                for half, pb in enumerate((bA, bB)):
                    for kk in range(4):
                        k = half * 4 + kk
                        S.transpose(pb[:, kk * 128:(kk + 1) * 128], xn[:, k * 128:(k + 1) * 128], identF)
                yield
                for half, pb in enumerate((bA, bB)):
                    for kk in range(4):
                        k = half * 4 + kk
                        S.act(hTt[:, k, :], pb[:, kk * 128:(kk + 1) * 128], AF.Identity,
                              bias=A[:, 1, k:k + 1], scale=A[:, 0, k:k + 1])
                yield
                for k in range(KD):
                    S.mm(bA[:, 0:416], hTt[:, k, :], w_in[:, k, 0:416], start=(k == 0), stop=(k == KD - 1))
                for k in range(KD):
                    S.mm(bB[:, 0:512], hTt[:, k, :], w_in[:, k, 416:928], start=(k == 0), stop=(k == KD - 1))
                yield
                ss2 = TR.alloc("ss2", [2], F32)
                S.act(junk[:, 0:256], bA[:, 0:256], AF.Square, accum=ss2[:, 0:1])
                S.act(junk[:, 256:384], bA[:, 256:384], AF.Square, accum=ss2[:, 1:2])
                krr = TR.alloc("krr", [32], F32)
                S.act(krr, bA[:, 384:416], AF.Identity)
                z1s = TR.alloc("z1s", [512], F32)
                S.copy(z1s, bB[:, 0:512], eng="dve")
                yield
                rs2 = TR.alloc("rs2", [2], F32)
                S.act(rs2[:, 0:1], ss2[:, 0:1], AF.Sqrt, bias=EPS, scale=1.0 / 256)
                S.act(rs2[:, 1:2], ss2[:, 1:2], AF.Sqrt, bias=EPS, scale=1.0 / 128)
                yield
                S.recip(rs2, rs2)
                yield
                cn = TR.alloc("cn", [384], BF16)
                S.act(cn[:, 0:256], bA[:, 0:256], AF.Identity, scale=rs2[:, 0:1])
                S.act(cn[:, 256:384], bA[:, 256:384], AF.Identity, scale=rs2[:, 1:2])
                q_a, k_a, v_a, sqk_a, sv_a = qa[ti], ka[ti], va[ti], sqk[ti], sva[ti]
                Cm, Sm = rpt[:, 0:16], rpt[:, 16:32]
                Cs, Ss = rpt[:, 32:64], rpt[:, 64:96]
                z1h = z1s[:, 0:384].re("p (h d) -> p h d", h=6)
                S.copy(sv_a[:, :, 0:64], z1s[:, 384:512].re("p (h d) -> p h d", h=2), eng="act")
                Cs6, Ss6 = Cs.bcast(1, [128, 6, 32]), Ss.bcast(1, [128, 6, 32])
                s1 = TR.alloc("s1", [6, 32], F32)
                s2 = TR.alloc("s2", [6, 32], F32)
                s3 = TR.alloc("s3", [6, 32], F32)
                s4 = TR.alloc("s4", [6, 32], F32)
                S.tt(s1, z1h[:, :, 0:32], Cs6, ALU.mult)
                S.tt(s2, z1h[:, :, 32:64], Ss6, ALU.mult)
                S.tt(s3, z1h[:, :, 32:64], Cs6, ALU.mult)
                S.tt(s4, z1h[:, :, 0:32], Ss6, ALU.mult)
                k1 = TR.alloc("k1", [16], F32)
                k2 = TR.alloc("k2", [16], F32)
                k3 = TR.alloc("k3", [16], F32)
                k4 = TR.alloc("k4", [16], F32)
                S.tt(k1, krr[:, 0:16], Cm, ALU.mult, eng="pool")
                S.tt(k2, krr[:, 16:32], Sm, ALU.mult, eng="pool")
                S.tt(k3, krr[:, 16:32], Cm, ALU.mult)
                S.tt(k4, krr[:, 0:16], Sm, ALU.mult)
                yield
                pT = pbf(bA)
                for j in range(3):
                    S.transpose(pT[:, j * 128:(j + 1) * 128], cn[:, j * 128:(j + 1) * 128], identB)
                S.tt(sqk_a[:, :, 0:32], s1, s2, ALU.subtract, eng="pool")
                S.tt(sqk_a[:, :, 32:64], s3, s4, ALU.add)
                kr = TR.alloc("kr", [32], F32)
                S.tt(kr[:, 0:16], k1, k2, ALU.subtract, eng="pool")
                S.tt(kr[:, 16:32], k3, k4, ALU.add)
                yield
                cT = TR.alloc("cTt", [3, 128], BF16)
                S.copy(cT, pT[:, 0:384].re("p (j t) -> p j t", j=3), eng="dve")
                S.copy(k_a[:, :, 64:96], kr.bcast(1, [128, 4, 32]), eng="dve")
                yield
                pq, pkv = bA, bB
                for j in range(2):
                    S.mm(pq[:, 0:384], cT[:, j, :], wq[:, j, :], start=(j == 0), stop=(j == 1))
                S.mm(pkv[:, 0:512], cT[:, 2, :], wkv, start=True, stop=True)
                yield
                pq3 = pq[:, 0:384].re("p (h d) -> p h d", h=4)
                S.copy(q_a[:, :, 0:64], pq3[:, :, 0:64], eng="act")
                qr = TR.alloc("qr", [4, 32], F32)
                S.copy(qr, pq3[:, :, 64:96], eng="act")
                S.copy(k_a[:, :, 0:64], pkv[:, 0:256].re("p (h d) -> p h d", h=4), eng="dve")
                S.copy(v_a[:, :, 0:64], pkv[:, 256:512].re("p (h d) -> p h d", h=4), eng="dve")
                yield
                Cm4, Sm4 = Cm.bcast(1, [128, 4, 16]), Sm.bcast(1, [128, 4, 16])
                q1 = TR.alloc("q1", [4, 16], F32)
                q2 = TR.alloc("q2", [4, 16], F32)
                q3 = TR.alloc("q3", [4, 16], F32)
                q4 = TR.alloc("q4", [4, 16], F32)
                S.tt(q1, qr[:, :, 0:16], Cm4, ALU.mult)
                S.tt(q2, qr[:, :, 16:32], Sm4, ALU.mult)
                S.tt(q3, qr[:, :, 16:32], Cm4, ALU.mult)
                S.tt(q4, qr[:, :, 0:16], Sm4, ALU.mult)
                pTk = pbf(bB)
                for h in range(4):
                    S.transpose(pTk[0:96, h * 128:(h + 1) * 128], k_a[:, h, :], identB)
                yield
                S.tt(q_a[:, :, 64:80], q1, q2, ALU.subtract, eng="pool")
                S.tt(q_a[:, :, 80:96], q3, q4, ALU.add)
                kT_t = kTt[ti]
                S.copy(kT_t[0:96], pTk[0:96, 0:512].re("p (h t) -> p h t", h=4), eng="act")
                yield
                pTq = pbf(bA)
                for h in range(4):
                    S.transpose(pTq[0:96, h * 128:(h + 1) * 128], q_a[:, h, :], identB)
                pTs = pbf(bB)
                for h in range(6):
                    S.transpose(pTs[0:64, h * 128:(h + 1) * 128], sqk_a[:, h, :], identB)
                yield
                qT_t = qTt[ti]
                S.copy(qT_t[0:96], pTq[0:96, 0:512].re("p (h t) -> p h t", h=4), eng="dve")
                sT_t = sqkT[ti]
                S.copy(sT_t[0:64], pTs[0:64, 0:768].re("p (h t) -> p h t", h=6), eng="act")
                tok = slice(t * 128, (t + 1) * 128)
                S.dma(KT[b, :, :, tok], kT_t[0:96])
                S.dma(VA[b, tok, :], v_a.re("p h d -> p (h d)"))
                S.dma(SVA[b, tok, :], sv_a.re("p h d -> p (h d)"))
                yield
                S.dma(QT[b, :, :, tok], qT_t[0:96])
                S.dma(SQKT[b, :, :, tok], sT_t[0:64])
                if gi + 1 < len(groups) and ti < len(groups[gi + 1]):
                    load_tile(groups[gi + 1][ti], ti, "r")

            for gi, grp in enumerate(groups):
                hT = hTs[gi % 2]
                active = [a_tile(gi, ti, t, hT) for ti, t in enumerate(grp)]
                while active:
                    for g_ in list(active):
                        try:
                            next(g_)
                        except StopIteration:
                            active.remove(g_)
                ntok = 128 * len(grp)
                tok0 = grp[0] * 128
                for m in range(8):
                    pz = PB[m]
                    for k in range(KD):
                        S.mm(pz[:, 0:ntok], w_in[:, k, 928 + m * 128:928 + (m + 1) * 128], hT[:, k, 0:ntok],
                             start=(k == 0), stop=(k == KD - 1))
                    zt = zlt[m % 2]
                    S.copy(zt[:, 0:ntok], pz[:, 0:ntok], eng=("act" if m % 2 else "dve"))
                    S.dma(ZL[b, m * 128:(m + 1) * 128, tok0:tok0 + ntok], zt[:, 0:ntok])
            S.barrier()

        def phase_B(b, l):
            R.reset()
            kT = R.alloc("kT", [4, T], BF16)
            vA = R.alloc("vA", [NT, 260], BF16)
            for h in range(4):
                S.dma(kT[0:96, h, :], KT[b, :, h, :])
            for c0 in range(0, NT, 8):
                c1 = min(NT, c0 + 8)
                S.dma(vA[:, c0:c1, :], VA[b, c0 * 128:c1 * 128, :].re("(c p) n -> p c n", p=128))
            qTb = [R.alloc("qTb%d" % i, [4, 512], BF16) for i in range(2)]
            pt = [R.alloc("pt%d" % i, [512], BF16) for i in range(4)]
            usb = [R.alloc("usb%d" % i, [512], F32) for i in range(2)]
            ot = [R.alloc("ot%d" % i, [4, 64], F32) for i in range(2)]
            rc = [R.alloc("rc%d" % i, [4], F32) for i in range(2)]
            chunks = []
            if l < depth - 1:
                chunks.append((0, 256, 2))
            for c in range(SEQ // 512):
                chunks.append((256 + c * 512, 512, NT))
            Sb = [PB[0], PB[1], PB[2], PB[3]]
            Ub = [PB[4], PB[5]]
            Tb = [PB[6], PB[7]]
            S.dma(qTb[0][0:96, :, 0:chunks[0][1]], QT[b, :, :, chunks[0][0]:chunks[0][0] + chunks[0][1]])
            cnt = 0
            ui = 0
            for ci, (q0, nq, nkc) in enumerate(chunks):
                qt = qTb[ci % 2]
                if ci + 1 < len(chunks):
                    nq0, nnq, _ = chunks[ci + 1]
                    S.dma(qTb[(ci + 1) % 2][0:96, :, 0:nnq], QT[b, :, :, nq0:nq0 + nnq])
                for h in range(4):
                    U = Ub[ui % 2]
                    def score(c):
                        sb = Sb[(cnt + c) % 4]
                        S.mm(sb[:, 0:nq], kT[0:96, h, c * 128:(c + 1) * 128], qt[0:96, h, 0:nq])
                        p = pt[(cnt + c) % 4]
                        S.act(p[:, 0:nq], sb[:, 0:nq], AF.Exp, scale=MLA_SCALE)
                        return p
                    ps = {0: score(0)}
                    if nkc > 1:
                        ps[1] = score(1)
                    for c in range(nkc):
                        if c + 2 < nkc:
                            ps[c + 2] = score(c + 2)
                        S.mm(U[0:65, 0:nq], vA[:, c, h * 65:(h + 1) * 65], ps[c][:, 0:nq],
                             start=(c == 0), stop=(c == nkc - 1))
                        del ps[c]
                    cnt += nkc
                    us = usb[ui % 2]
                    S.copy(us[0:65, 0:nq], U[0:65, 0:nq], eng="dve")
                    tb = Tb[ui % 2]
                    nsub = nq // 128
                    for s in range(nsub):
                        S.transpose(tb[:, s * 65:(s + 1) * 65], us[0:65, s * 128:(s + 1) * 128], identF[0:65, 0:65])
                    t3 = tb[:, 0:nsub * 65].re("p (s d) -> p s d", s=nsub)
                    r = rc[ui % 2]
                    S.recip(r[:, 0:nsub], t3[:, :, 64])
                    o = ot[ui % 2]
                    S.tt(o[:, 0:nsub, :], t3[:, :, 0:64], r[:, 0:nsub].bcast(2, [128, nsub, 64]), ALU.mult)
                    S.dma(OMIX[b, q0:q0 + nq, h * 64:(h + 1) * 64].re("(s p) d -> p s d", p=128), o[:, 0:nsub, :])
                    ui += 1
            S.barrier()

        def phase_B2(b, l):
            R.reset()
            sT_ = R.alloc("sqkT_all", [6, T], BF16)
            svA = R.alloc("svA", [NT, 130], BF16)
            for h in range(6):
                S.dma(sT_[0:64, h, :], SQKT[b, :, h, :])
            for c0 in range(0, NT, 8):
                c1 = min(NT, c0 + 8)
                S.dma(svA[:, c0:c1, :], SVA[b, c0 * 128:c1 * 128, :].re("(c p) n -> p c n", p=128))
            esink = R.alloc("esink", [4], F32)
            S.dma(esink, V(sink_d.ap[l].partition_broadcast(128), []))
            S.act(esink, esink, AF.Exp)
            pt = [R.alloc("spt%d" % i, [2, 128], BF16) for i in range(4)]
            usb = [R.alloc("susb%d" % i, [256], F32) for i in range(2)]
            ot = [R.alloc("sot%d" % i, [2, 64], F32) for i in range(2)]
            rc = [R.alloc("src%d" % i, [2], F32) for i in range(2)]
            Sb = [PB[0], PB[1], PB[2], PB[3]]
            Ub = [PB[4], PB[5]]
            Tb = [PB[6], PB[7]]
            qtiles = list(range(2, NT)) if l == depth - 1 else list(range(NT))
            cnt = 0
            ui = 0
            for tq in qtiles:
                if tq < 2:
                    keys = [(0, None), (1, None)]
                else:
                    keys = [(0, None), (1, None)]
                    if tq - 1 >= 2:
                        keys.append((tq - 1, 0))
                    keys.append((tq, None))
                    if tq + 1 < NT:
                        keys.append((tq + 1, 1))
                for g in range(2):
                    U = Ub[ui % 2]
                    q = sT_[0:64, 2 * g:2 * g + 2, tq * 128:(tq + 1) * 128]
                    plist = []
                    for (kc, mk) in keys:
                        sb = Sb[cnt % 4]
                        S.mm(sb[:, 0:256].re("p (h t) -> p h t", h=2), sT_[0:64, 4 + g, kc * 128:(kc + 1) * 128], q)
                        p = pt[cnt % 4]
                        S.act(p, sb[:, 0:256].re("p (h t) -> p h t", h=2), AF.Exp, scale=SWA_SCALE)
                        if mk is not None:
                            S.tt(p, p, maskB[:, mk, :].bcast(1, [128, 2, 128]), ALU.mult)
                        plist.append(p)
                        cnt += 1
                        if len(plist) >= 2:
                            idx = len(plist) - 2
                            kc2 = keys[idx][0]
                            S.mm(U[0:65, 0:256], svA[:, kc2, g * 65:(g + 1) * 65], plist[idx].re("p h t -> p (h t)"),
                                 start=(idx == 0), stop=False)
                    idx = len(plist) - 1
                    S.mm(U[0:65, 0:256], svA[:, keys[idx][0], g * 65:(g + 1) * 65], plist[idx].re("p h t -> p (h t)"),
                         start=(idx == 0), stop=True)
                    us = usb[ui % 2]
                    S.copy(us[0:65, :], U[0:65, 0:256], eng="dve")
                    tb = Tb[ui % 2]
                    for s in range(2):
                        S.transpose(tb[:, s * 65:(s + 1) * 65], us[0:65, s * 128:(s + 1) * 128], identF[0:65, 0:65])
                    t3 = tb[:, 0:130].re("p (s d) -> p s d", s=2)
                    r = rc[ui % 2]
                    S.tt(r, t3[:, :, 64], esink[:, 2 * g:2 * g + 2], ALU.add)
                    S.recip(r, r)
                    o = ot[ui % 2]
                    S.tt(o, t3[:, :, 0:64], r.bcast(2, [128, 2, 64]), ALU.mult)
                    S.dma(OMIX[b, tq * 128:(tq + 1) * 128, 256 + g * 128:256 + (g + 1) * 128], o.re("p s d -> p (s d)"))
                    ui += 1
            S.barrier()

        def phase_C(b, l):
            R.reset()
            ZW = T + 8
            CO, LO = 2, 261
            wst = R.alloc("wst", [4, 4, 128], F32)
            S.memset(wst, 0.0, eng="dve")
            for ty, wd_ in enumerate((lru_wa_d, lru_wx_d)):
                for d in range(2):
                    for half in range(2):
                        src = wd_[l, d].re("(m two) c e -> two c m e", two=2)[half]
                        S.dma(wst[half * 64:(half + 1) * 64, ty * 2 + d, :, half * 64:(half + 1) * 64], src)
            wbd = R.alloc("wbd", [4, 4, 128], BF16)
            S.copy(wbd, wst, eng="dve")
            vT = R.alloc("vT", [3, 2, 4], F32)
            S.dma(vT, lru_vT_d[l])
            cw = R.alloc("cw", [4, 4], F32)
            cb = R.alloc("cb", [4], F32)
            S.dma(cw, conv_wT_d[l])
            S.dma(cb, conv_bT_d[l])
            cneg = R.alloc("cneg", [2, 4], F32)
            S.act(cneg, vT[:, 2], AF.Exp, scale=-1.0)
            S.act(cneg, cneg, AF.Ln, bias=1.0, scale=1.0)
            S.ts(cneg, cneg, -8.0, None, ALU.mult)
            cnh = R.alloc("cnh", [2, 4], F32)
            S.ts(cnh, cneg, 0.5, None, ALU.mult)
            zb = R.alloc("zb", [ZW], F32)
            zbd = zb.sub(Buf("zbdata"))
            S.memset(V(zb.ap, zb.bufs + zbd.bufs), 0.0, eng="pool")
            u = R.alloc("u", [T], F32)
            ub = R.alloc("ub", [T], BF16)
            rr = R.alloc("rr", [T], F32)
            ii = R.alloc("ii", [T], F32)
            tq_ = R.alloc("tq", [T], F32)
            hf = R.alloc("hf", [T], F32)
            hb = R.alloc("hb", [T], F32)
            gz = R.alloc("gz", [T], F32)
            ob = R.alloc("ob", [T], BF16)
            tchunks = [(0, 256)] + [(256 + c * 512, 512) for c in range(SEQ // 512)]
            for m in range(4):
                S.dma(zbd[:, CO:CO + 256], ZL[b, m * 128:(m + 1) * 128, 0:256])
                S.dma(zbd[:, LO:LO + SEQ], ZL[b, m * 128:(m + 1) * 128, 256:T])
                S.dma(gz, ZL[b, 512 + m * 128:512 + (m + 1) * 128, :])
                zr = V(zb.ap, zb.bufs + zbd.bufs)
                for (o0, o1, n) in ((0, 0, 256), (256, 259, SEQ)):
                    S.ts(u[:, o0:o0 + n], zr[:, o1:o1 + n], cw[:, m, 0:1], cb[:, m:m + 1], ALU.mult, ALU.add)
                    for tap in range(1, 4):
                        S.stt(u[:, o0:o0 + n], zr[:, o1 + tap:o1 + tap + n], cw[:, m, tap:tap + 1], u[:, o0:o0 + n],
                              ALU.mult, ALU.add)
                S.copy(ub, u, eng="act")
                S.act(gz, gz, AF.Gelu_apprx_tanh)
                for d in range(2):
                    for ci, (t0, n) in enumerate(tchunks):
                        pa, px = PB[(ci % 2) * 2], PB[(ci % 2) * 2 + 1]
                        S.mm(pa[:, 0:n], wbd[:, 0 * 2 + d, m, :], ub[:, t0:t0 + n])
                        S.mm(px[:, 0:n], wbd[:, 1 * 2 + d, m, :], ub[:, t0:t0 + n])
                        S.act(rr[:, t0:t0 + n], pa[:, 0:n], AF.Sigmoid, bias=vT[:, 0, d, m:m + 1], scale=1.0)
                        S.act(ii[:, t0:t0 + n], px[:, 0:n], AF.Sigmoid, bias=vT[:, 1, d, m:m + 1], scale=1.0)
                    S.act(tq_, rr, AF.Tanh, scale=cnh[:, d, m:m + 1])
                    S.act(rr, rr, AF.Exp, scale=cneg[:, d, m:m + 1])
                    S.act(tq_, tq_, AF.Sqrt, scale=-1.0)
                    S.stt(tq_, rr, 1.0, tq_, ALU.add, ALU.mult)
                    S.tt(ii, ii, u, ALU.mult)
                    S.tt(ii, ii, tq_, ALU.mult)
                    if d == 0:
                        S.scan(hf, rr, ii, 0.0)
                    else:
                        S.scan(hb[:, 0:256][:, ::-1], rr[:, 0:256][:, ::-1], ii[:, 0:256][:, ::-1], 0.0)
                        S.scan(hb[:, 256:T][:, ::-1], rr[:, 256:T][:, ::-1], ii[:, 256:T][:, ::-1], hb[:, 0:1])
                S.tt(hf, hf, hb, ALU.add)
                S.tt(ob, hf, gz, ALU.mult)
                S.dma(OLRU[b, m * 128:(m + 1) * 128, :], ob)
            S.barrier()

        def phase_DE(b, l, tiles):
            R.reset()
            last = l == depth - 1
            ntl = len(tiles)
            ntok = ntl * 128
            H2T = R.alloc("H2T", [KD, ntok], BF16)
            COMB = R.alloc("COMB", [ntl, 32], F32)
            mark = R.off
            wo_f = R.alloc("wo_f", [KD, D], F32)
            S.dma(wo_f, w_out_d[l].re("(k p) n -> p k n", p=128))
            gg = R.alloc("gg", [KD], F32)
            S.dma(gg, g_grpT_d[l])
            wo = R.alloc("wo", [KD, D], BF16)
            for k in range(KD):
                if k % 2:
                    S.act(wo[:, k, :], wo_f[:, k, :], AF.Identity, scale=gg[:, k:k + 1])
                else:
                    S.ts(wo[:, k, :], wo_f[:, k, :], gg[:, k:k + 1], None, ALU.mult)
            wrt = R.alloc("wrt", [KD, 36], F32)
            S.dma(wrt, wr_d[l].re("(k p) n -> p k n", p=128))
            rbt = R.alloc("rbt", [36], F32)
            S.dma(rbt, V(rb_d.ap[l].partition_broadcast(128), []))
            G1 = [R.alloc("G1_%d" % i, [D], F32) for i in range(2)]
            S.dma(G1[0], V(GATES.ap[l, b, 0].partition_broadcast(128), []))
            S.dma(G1[1], V(GATES.ap[l, 2, 0].partition_broadcast(128), []))
            NBUF = 4
            xt = [R.alloc("dxt%d" % i, [D], F32) for i in range(NBUF)]
            om = [R.alloc("om%d" % i, [512], F32) for i in range(NBUF)]
            ol = [R.alloc("ol%d" % i, [4, 128], BF16) for i in range(NBUF)]
            LG = R.alloc("LG", [ntl, 36], F32)
            rowl, rowc = l * 3 + b, l * 3 + 2
            TRs = [Region(arena, R.off + pp * 24576, R.off + (pp + 1) * 24576, cached=True) for pp in range(2)]
            RB = Region(arena, R.off + 2 * 24576, ARENA_BYTES, cached=True)

            def loads(i):
                t = tiles[i]
                tok = slice(t * 128, (t + 1) * 128)
                S.dma(xt[i % NBUF], x_src(b, l, t))
                S.dma(om[i % NBUF], OMIX[b, tok, :])
                S.dma(ol[i % NBUF], OLRU[b, :, tok].re("(m p) t -> p m t", p=128))

            def d_tile(i, t):
                pp = i % 2
                TR = TRs[pp]
                Q = PB[4 * pp:4 * pp + 4]
                PA, PL = psum[2 * pp], psum[2 * pp + 1]
                is_ctx = t < 2
                tok = slice(t * 128, (t + 1) * 128)
                if i + 2 < ntl:
                    loads(i + 2)
                x_t, o_m, o_l = xt[i % NBUF], om[i % NBUF], ol[i % NBUF]
                junk = TR.alloc("djunk", [D], BF16)
                ss2 = TR.alloc("dss2", [2], F32)
                S.act(junk[:, 0:256], o_m[:, 0:256], AF.Square, accum=ss2[:, 0:1])
                S.act(junk[:, 256:512], o_m[:, 256:512], AF.Square, accum=ss2[:, 1:2])
                sq = TR.alloc("sq", [4, 128], F32)
                S.act(sq, o_l, AF.Square)
                yield
                rs2 = TR.alloc("rstd_g", [2], F32)
                S.act(rs2, ss2, AF.Sqrt, bias=EPS, scale=1.0 / 256)
                pss = Q[1]
                for m in range(4):
                    S.mm(pss[:, 0:1], sq[:, m, :], ones1, start=(m == 0), stop=(m == 3))
                yield
                S.recip(rs2, rs2)
                rsl = TR.alloc("rsl", [1], F32)
                S.act(rsl, pss[:, 0:1], AF.Sqrt, bias=EPS, scale=1.0 / 512)
                yield
                on = TR.alloc("on", [512], BF16)
                S.act(on[:, 0:256], o_m[:, 0:256], AF.Identity, scale=rs2[:, 0:1])
                S.act(on[:, 256:512], o_m[:, 256:512], AF.Identity, scale=rs2[:, 1:2])
                S.recip(rsl, rsl)
                yield
                pT = pbf(Q[0])
                for j in range(4):
                    S.transpose(pT[:, j * 128:(j + 1) * 128], on[:, j * 128:(j + 1) * 128], identB)
                yield
                mT = TR.alloc("mT", [4, 128], BF16)
                S.copy(mT, pT[:, 0:512].re("p (j t) -> p j t", j=4), eng="dve")
                yield
                for nh in range(2):
                    for m in range(4):
                        S.mm(PL[nh + 1], o_l[:, m, :], wo[:, 4 + m, nh * 512:(nh + 1) * 512], start=(m == 0), stop=(m == 3))
                for nh in range(2):
                    for j in range(4):
                        S.mm(PA[nh + 1], mT[:, j, :], wo[:, j, nh * 512:(nh + 1) * 512], start=(j == 0), stop=(j == 3))
                yield
                tA = TR.alloc("tA", [D], F32)
                S.copy(tA, PA[0], eng="act")
                yield
                S.stt(tA, PL[0], rsl, tA, ALU.mult, ALU.add)
                yield
                S.tt(tA, tA, G1[1 if is_ctx else 0], ALU.mult)
                yield
                S.tt(x_t, x_t, tA, ALU.add)
                yield
                S.dma(X1S[b, tok, :], x_t)
                ss = TR.alloc("ss", [1], F32)
                S.act(junk, x_t, AF.Square, accum=ss)
                yield
                rstd = TR.alloc("rstd_x", [1], F32)
                S.act(rstd, ss, AF.Sqrt, bias=EPS, scale=1.0 / D)
                yield
                S.recip(rstd, rstd)
                yield
                xn = TR.alloc("xn", [D], F32)
                S.act(xn, x_t, AF.Identity, scale=rstd)
                yield
                A = AB[:, rowc if is_ctx else rowl]
                h2f = TR.alloc("h2f", [KD, 128], F32)
                for half in range(2):
                    pb = Q[half]
                    for kk in range(4):
                        k = half * 4 + kk
                        S.transpose(pb[:, kk * 128:(kk + 1) * 128], xn[:, k * 128:(k + 1) * 128], identF)
                yield
                for half in range(2):
                    pb = Q[half]
                    for kk in range(4):
                        k = half * 4 + kk
                        S.act(h2f[:, k, :], pb[:, kk * 128:(kk + 1) * 128], AF.Identity,
                              bias=A[:, 3, k:k + 1], scale=A[:, 2, k:k + 1])
                yield
                S.copy(H2T[:, :, i * 128:(i + 1) * 128], h2f, eng="dve")
                pr = Q[2]
                for k in range(KD):
                    S.mm(pr[:, 0:36], h2f[:, k, :], wrt[:, k, :], start=(k == 0), stop=(k == KD - 1))
                yield
                S.tt(LG[:, i, :], pr[:, 0:36], rbt, ALU.add)

            for i in range(min(2, ntl)):
                loads(i)
            active = []
            nxt = 0
            while nxt < ntl or active:
                while len(active) < 2 and nxt < ntl:
                    active.append(d_tile(nxt, tiles[nxt]))
                    nxt += 1
                for g_ in list(active):
                    try:
                        next(g_)
                    except StopIteration:
                        active.remove(g_)
            lgG = LG[:, :, 0:4]
            lgE = LG[:, :, 4:36].re("p t (g e) -> p t g e", g=4)
            gmax = RB.alloc("gmax", [ntl], F32)
            S.reduce(gmax, lgG, ALU.max)
            mg = RB.alloc("mg", [ntl, 4], F32)
            S.tt(mg, lgG, gmax.bcast(2, [128, ntl, 4]), ALU.is_equal)
            eg = RB.alloc("eg", [ntl, 4], F32)
            S.tt(eg, lgG, gmax.bcast(2, [128, ntl, 4]), ALU.subtract)
            S.act(eg, eg, AF.Exp)
            pgt = RB.alloc("pgt", [ntl], F32)
            S.reduce(pgt, eg, ALU.add)
            S.recip(pgt, pgt)
            les = RB.alloc("les", [ntl, 8], F32)
            tmp8 = RB.alloc("tmp8", [ntl, 8], F32)
            S.tt(les, lgE[:, :, 0, :], mg[:, :, 0].bcast(2, [128, ntl, 8]), ALU.mult)
            for g in range(1, 4):
                S.tt(tmp8, lgE[:, :, g, :], mg[:, :, g].bcast(2, [128, ntl, 8]), ALU.mult)
                S.tt(les, les, tmp8, ALU.add)
            m1v = RB.alloc("m1v", [ntl], F32)
            S.reduce(m1v, les, ALU.max)
            k1 = RB.alloc("k1", [ntl, 8], F32)
            S.tt(k1, les, m1v.bcast(2, [128, ntl, 8]), ALU.is_equal)
            les2 = RB.alloc("les2", [ntl, 8], F32)
            S.stt(les2, k1, -1e30, les, ALU.mult, ALU.add)
            m2v = RB.alloc("m2v", [ntl], F32)
            S.reduce(m2v, les2, ALU.max)
            k2 = RB.alloc("k2", [ntl, 8], F32)
            S.tt(k2, les2, m2v.bcast(2, [128, ntl, 8]), ALU.is_equal)
            e2 = RB.alloc("e2", [ntl], F32)
            S.tt(e2, m2v, m1v, ALU.subtract)
            S.act(e2, e2, AF.Exp)
            w1 = RB.alloc("w1", [ntl], F32)
            S.ts(w1, e2, 1.0, None, ALU.add)
            S.recip(w1, w1)
            S.tt(w1, w1, pgt, ALU.mult)
            w2 = RB.alloc("w2", [ntl], F32)
            S.tt(w2, w1, e2, ALU.mult)
            cwt = RB.alloc("cwt", [ntl, 8], F32)
            S.tt(cwt, k1, w1.bcast(2, [128, ntl, 8]), ALU.mult)
            S.tt(tmp8, k2, w2.bcast(2, [128, ntl, 8]), ALU.mult)
            S.tt(cwt, cwt, tmp8, ALU.add)
            for g in range(4):
                S.tt(COMB[:, :, g * 8:(g + 1) * 8], cwt, mg[:, :, g].bcast(2, [128, ntl, 8]), ALU.mult)
            S.barrier()
            R.off = mark
            Y = R.alloc("Y", [ntl, D], F32)
            wg = [R.alloc("wg%d" % i, [KD, DEXP], BF16) for i in range(2)]
            wu = [R.alloc("wu%d" % i, [KD, DEXP], BF16) for i in range(2)]
            wd = [R.alloc("wd%d" % i, [2, D], BF16) for i in range(2)]
            sgl = [R.alloc("sgl%d" % i, [512], F32) for i in range(2)]
            aT = [R.alloc("aT%d" % i, [2, 512], BF16) for i in range(2)]
            Yb = [Y[:, i, :].sub(Buf("Y%d" % i)) for i in range(ntl)]

            def wload(e):
                S.dma(wg[e % 2], weg_d[l, e].re("(k p) n -> p k n", p=128), q="pool")
                S.dma(wu[e % 2], weu_d[l, e].re("(k p) n -> p k n", p=128), q="pool")
                S.dma(wd[e % 2], wed_d[l, e].re("(k p) n -> p k n", p=128), q="pool")
            wload(0)
            chunks = []
            c0 = 0
            while c0 < ntok:
                n = min(512, ntok - c0)
                chunks.append((c0, n))
                c0 += n
            it = 0
            for e in range(NEXP):
                if e + 1 < NEXP:
                    wload(e + 1)
                g_w, u_w, d_w = wg[e % 2], wu[e % 2], wd[e % 2]
                for (c0, n) in chunks:
                    a_t = aT[it % 2]
                    for half in range(2):
                        pg_, pu_ = PB[half * 2], PB[half * 2 + 1]
                        for k in range(KD):
                            S.mm(pg_[:, 0:n], g_w[:, k, half * 128:(half + 1) * 128], H2T[:, k, c0:c0 + n],
                                 start=(k == 0), stop=(k == KD - 1))
                        for k in range(KD):
                            S.mm(pu_[:, 0:n], u_w[:, k, half * 128:(half + 1) * 128], H2T[:, k, c0:c0 + n],
                                 start=(k == 0), stop=(k == KD - 1))
                        sg_ = sgl[half]
                        S.act(sg_[:, 0:n], pg_[:, 0:n], AF.Silu)
                        S.tt(a_t[:, half, 0:n], sg_[:, 0:n], pu_[:, 0:n], ALU.mult)
                    for s in range(n // 128):
                        ti = c0 // 128 + s
                        pd = psum[2 + (ti % 2)]
                        for nh in range(2):
                            for half in range(2):
                                S.mm(pd[nh + 1], a_t[:, half, s * 128:(s + 1) * 128], d_w[:, half, nh * 512:(nh + 1) * 512],
                                     start=(half == 0), stop=(half == 1))
                        if e == 0:
                            S.ts(Yb[ti], pd[0], COMB[:, ti, e:e + 1], None, ALU.mult)
                        else:
                            S.stt(Yb[ti], pd[0], COMB[:, ti, e:e + 1], Yb[ti], ALU.mult, ALU.add)
                    it += 1
            G2 = [R.alloc("G2_%d" % i, [D], F32) for i in range(2)]
            S.dma(G2[0], V(GATES.ap[l, b, 1].partition_broadcast(128), []))
            S.dma(G2[1], V(GATES.ap[l, 2, 1].partition_broadcast(128), []))
            x1 = [R.alloc("x1_%d" % i, [D], F32) for i in range(2)]
            ejunk = R.alloc("ejunk", [D], BF16)
            if last:
                GF = R.alloc("GF", [D], F32)
                S.dma(GF, V(gfin_d.ap.partition_broadcast(128), []))
            TR = Region(arena, R.off, ARENA_BYTES, cached=True)
            S.dma(x1[0], X1S[b, tiles[0] * 128:(tiles[0] + 1) * 128, :])
            for i, t in enumerate(tiles):
                TR.reset()
                tok = slice(t * 128, (t + 1) * 128)
                if i + 1 < ntl:
                    S.dma(x1[(i + 1) % 2], X1S[b, tiles[i + 1] * 128:(tiles[i + 1] + 1) * 128, :])
                x_t = x1[i % 2]
                S.tt(Yb[i], Yb[i], G2[1 if t < 2 else 0], ALU.mult)
                S.tt(x_t, x_t, Yb[i], ALU.add)
                if not last:
                    S.dma(XS[b, tok, :], x_t)
                else:
                    ss = TR.alloc("ess", [1], F32)
                    S.act(ejunk, x_t, AF.Square, accum=ss)
                    rstd = rms_rstd(S, TR, ss, D, 1, "f")
                    S.stt(x_t, x_t, rstd, GF, ALU.mult, ALU.mult)
                    S.dma(out_d[b, (t - 2) * 128:(t - 1) * 128, :], x_t, is_out=True)
            S.barrier()

        for b in range(NB):
            for l in range(depth if nlayers is None else nlayers):
                if "A" in plan:
                    phase_A(b, l)
                if "B" in plan:
                    phase_B(b, l)
                if "S" in plan:
                    phase_B2(b, l)
                if "C" in plan:
                    phase_C(b, l)
                if "D" not in plan:
                    continue
                if l == depth - 1:
                    tl = list(range(2, NT))
                else:
                    tl = list(range(NT))
                nblk = (len(tl) + 16) // 17
                per = (len(tl) + nblk - 1) // nblk
                for i in range(nblk):
                    blk = tl[i * per:(i + 1) * per]
                    if blk:
                        phase_DE(b, l, blk)
        if debug:
            print("total ops", S.nadd)
        S.emit()
    return nc


def _perm_rot(nheads, hd, base=0):
    q = hd // 4
    idx = []
    for h in range(nheads):
        o = base + h * hd
        idx += list(range(o, o + q)) + list(range(o + 2 * q, o + 3 * q)) + list(range(o + q, o + 2 * q)) + \
            list(range(o + 3 * q, o + 4 * q))
    return idx


def _chan_major(v, nch):
    return np.ascontiguousarray(np.swapaxes(v.reshape(v.shape[:-1] + (nch, 128)), -1, -2))


def host_shared(inp, SEQ):
    f = np.float32
    depth = inp["w_ada"].shape[0]
    sh = {}
    sh["w_ada"] = np.ascontiguousarray(inp["w_ada"], f)
    sh["b_ada"] = np.ascontiguousarray(inp["b_ada"], f)
    sh["b_adaT"] = _chan_major(inp["b_ada"].astype(f), 48)
    sh["g1T"] = _chan_major(inp["g_norm1"].astype(f), 8)
    sh["g2T"] = _chan_major(inp["g_norm2"].astype(f), 8)
    cols = list(range(0, 384)) + _perm_rot(1, 32, 384) + _perm_rot(4, 64, 416) + _perm_rot(2, 64, 672) + \
        list(range(800, 1952))
    sh["w_in_p"] = np.ascontiguousarray(inp["w_in"][:, :, cols], f)
    sh["g_cqT"] = _chan_major(inp["g_cq"].astype(f), 2)
    sh["g_ckvT"] = _chan_major(inp["g_ckv"].astype(f), 1)
    qcols = []
    for h in range(4):
        qcols += list(range(h * 96, h * 96 + 64)) + _perm_rot(1, 32, h * 96 + 64)
    sh["w_uq_p"] = np.ascontiguousarray(inp["w_uq"][:, :, qcols], f)
    kvcols = [h * 128 + i for h in range(4) for i in range(64)] + [h * 128 + 64 + i for h in range(4) for i in range(64)]
    sh["w_ukv_p"] = np.ascontiguousarray(inp["w_ukv"][:, :, kvcols], f)
    sh["sink"] = np.ascontiguousarray(inp["swa_sink"], f)
    cw = inp["conv_w"].astype(f)
    sh["conv_wT"] = np.ascontiguousarray(np.transpose(cw.reshape(depth, 4, 4, 128), (0, 3, 2, 1)))
    sh["conv_bT"] = _chan_major(inp["conv_b"].astype(f), 4)
    sh["lru_wa"] = np.ascontiguousarray(inp["lru_wa"], f)
    sh["lru_wx"] = np.ascontiguousarray(inp["lru_wx"], f)
    v = np.stack([inp["lru_ba"], inp["lru_bx"], inp["lru_lam"]], axis=1).astype(f)
    sh["lru_vT"] = np.ascontiguousarray(np.transpose(v.reshape(depth, 3, 2, 4, 128), (0, 4, 1, 2, 3)))
    sh["g_grpT"] = _chan_major(inp["g_grp"].astype(f), 8)
    sh["w_out"] = np.ascontiguousarray(inp["w_out"], f)
    wg2 = np.transpose(inp["w_g2"], (0, 2, 1, 3)).reshape(depth, D, 32)
    sh["wr"] = np.ascontiguousarray(np.concatenate([inp["w_g1"], wg2], axis=-1), f)
    sh["rb"] = np.ascontiguousarray(np.concatenate([inp["b_g1"], inp["b_g2"].reshape(depth, 32)], axis=-1), f)
    sh["w_e_gate"] = np.ascontiguousarray(inp["w_e_gate"], f)
    sh["w_e_up"] = np.ascontiguousarray(inp["w_e_up"], f)
    sh["w_e_down"] = np.ascontiguousarray(inp["w_e_down"], f)
    sh["g_final"] = np.ascontiguousarray(inp["g_final"], f)
    sh["ident"] = np.eye(128, dtype=f)
    j = np.arange(128)[:, None]
    i = np.arange(128)[None, :]
    sh["masks"] = np.ascontiguousarray(np.stack([(j >= i), (j <= i)], axis=1).astype(f))
    pos = np.arange(SEQ)
    rows = (pos // 64).astype(np.float32)
    colsp = (pos % 64).astype(np.float32)

    def tab(half):
        fr = (np.float32(10000.0) ** (-np.arange(half, dtype=np.float32) / np.float32(half))).astype(np.float32)
        ar = rows[:, None] * fr[None, :]
        ac = colsp[:, None] * fr[None, :]
        C = np.concatenate([np.cos(ar), np.cos(ac)], axis=1)
        Sn = np.concatenate([np.sin(ar), np.sin(ac)], axis=1)
        return C.astype(f), Sn.astype(f)
    Cm, Sm = tab(8)
    Cs, Ss = tab(16)
    tabs = np.concatenate([Cm, Sm, Cs, Ss], axis=1).reshape(SEQ // 128, 128, 96)
    ident_tab = np.concatenate([np.ones((128, 16), f), np.zeros((128, 16), f), np.ones((128, 32), f), np.zeros((128, 32), f)], axis=1)
    sh["rope"] = np.ascontiguousarray(np.concatenate([tabs, ident_tab[None]], axis=0))
    return sh


def host_core(inp, b0, NB):
    f = np.float32
    cv = np.stack([inp["c"][b0], inp["c"][min(b0 + 1, inp["c"].shape[0] - 1)], inp["c_ctx"]], axis=0).astype(f)
    cT = np.ascontiguousarray(np.transpose(cv.reshape(3, KD, 128), (2, 1, 0)))
    return {
        "x": np.ascontiguousarray(inp["x"][b0:b0 + NB], f),
        "ctx": np.ascontiguousarray(inp["ctx"][b0:b0 + NB], f),
        "cT": cT,
    }


_NC_CACHE = {}


def kernel(**inputs):
    inp = {k: np.asarray(v) for k, v in inputs.items()}
    B, SEQ, _ = inp["x"].shape
    ncores = 8
    NB = B // ncores
    key = (SEQ, NB)
    if key not in _NC_CACHE:
        _NC_CACHE[key] = build(SEQ, NB)
    nc = _NC_CACHE[key]
    sh = host_shared(inp, SEQ)
    in_maps = []
    for c in range(ncores):
        m = dict(sh)
        m.update(host_core(inp, c * NB, NB))
        in_maps.append(m)
    res = run_bass_kernel_spmd(nc, in_maps, core_ids=list(range(ncores)))
    out = np.concatenate([np.asarray(r["out"]) for r in res.results], axis=0)
    return out.astype(np.float32)
```

```python
import numpy as np
from contextlib import ExitStack
import concourse.bass as bass
import concourse.mybir as mybir
from concourse.bass_utils import run_bass_kernel_spmd

F32 = mybir.dt.float32
BF16 = mybir.dt.bfloat16
AF = mybir.ActivationFunctionType
ALU = mybir.AluOpType
AX = mybir.AxisListType

D = 1024
KD = 8
LCTX = 256
EPS = 1e-6
NEXP = 32
DEXP = 256
MLA_SCALE = 96 ** -0.5
SWA_SCALE = 0.125

COMPUTE = ("pe", "act", "dve", "pool")
DMAQ = ("sp", "pool")
NDMASEM = 12


class Buf:
    __slots__ = ("w", "r", "name", "excl")

    def __init__(self, name="", excl=False):
        self.w = None
        self.r = {}
        self.name = name
        self.excl = excl


class V:
    __slots__ = ("ap", "bufs")

    def __init__(self, ap, bufs):
        self.ap = ap
        self.bufs = bufs

    def __getitem__(self, idx):
        return V(self.ap[idx], self.bufs)

    def re(self, pat, **kw):
        return V(self.ap.rearrange(pat, **kw), self.bufs)

    def bcast(self, axis, shape):
        return V(self.ap.unsqueeze(axis).broadcast_to(list(shape)), self.bufs)

    def sub(self, buf):
        return V(self.ap, [buf])


class Op:
    __slots__ = ("eng", "fn", "deps", "sig", "cnt", "dma", "dsem", "dtgt", "idx")

    def __init__(self, eng, fn, dma):
        self.eng = eng
        self.fn = fn
        self.dma = dma
        self.deps = None
        self.sig = False
        self.cnt = 0
        self.dsem = None
        self.dtgt = 0


class Sched:
    def __init__(self, nc, es):
        self.nc = nc
        self.es = es
        self.ops = {e: [] for e in ("pe", "act", "dve", "pool", "sp")}
        self.dma_hist = {q: [None] * NDMASEM for q in DMAQ}
        self.dma_cnt = {q: [0] * NDMASEM for q in DMAQ}
        self.dma_rr = {q: 0 for q in DMAQ}
        self.sems = {}
        self.dsems = {}
        self.out_dmas = []
        self.bar = set()
        self.last = {e: None for e in COMPUTE}
        self.nadd = 0
        self.limit = None
        self.trace = None

    def dram(self, name, shape, dtype, kind="Internal"):
        t = self.nc.dram_tensor(name, list(shape), dtype, kind=kind)
        return V(t.ap(), [])

    def barrier(self):
        b = set()
        for e in COMPUTE:
            if self.last[e] is not None:
                b.add(self.last[e])
        for q in DMAQ:
            for o in self.dma_hist[q]:
                if o is not None:
                    b.add(o)
        for o in b:
            o.sig = True
        self.bar = b

    def add(self, eng, fn, reads=(), writes=(), dma=False):
        op = Op(eng, fn, dma)
        self.nadd += 1
        op.idx = self.nadd
        if self.trace is not None and self.trace[0] <= self.nadd <= self.trace[1]:
            import traceback
            fr = [f for f in traceback.extract_stack() if f.name not in ("add",)][-2:]
            print("OP", self.nadd, eng, "dma" if dma else "", [(f.lineno, f.line) for f in fr][-1])
        if self.limit is not None and self.nadd > self.limit:
            op.deps = set()
            return op
        deps = set(self.bar)
        for v in reads:
            for b in v.bufs:
                if b.w is not None:
                    deps.add(b.w)
                if b.excl:
                    for key, r in b.r.items():
                        if key != eng and not isinstance(r, list):
                            deps.add(r)
        for v in writes:
            for b in v.bufs:
                if b.w is not None:
                    deps.add(b.w)
                for r in b.r.values():
                    if isinstance(r, list):
                        deps.update(r)
                    elif r.eng != eng or r.dma or dma:
                        deps.add(r)
        if dma:
            q = eng
            slot = self.dma_rr[q] % NDMASEM
            self.dma_rr[q] += 1
            prev = self.dma_hist[q][slot]
            if prev is not None:
                deps.add(prev)
            self.dma_cnt[q][slot] += 1
            op.dsem = (q, slot)
            op.dtgt = 16 * self.dma_cnt[q][slot]
            self.dma_hist[q][slot] = op
        deps.discard(op)
        if eng == "pe":
            deps = {d for d in deps if d.dma or d.eng != "pe"}
        op.deps = deps
        for d in deps:
            d.sig = True
        for v in reads:
            for b in v.bufs:
                if dma:
                    b.r.setdefault(("dma", eng), []).append(op)
                else:
                    b.r[eng] = op
        for v in writes:
            for b in v.bufs:
                b.w = op
                b.r = {}
        self.ops[eng].append(op)
        if not dma:
            self.last[eng] = op
        return op

    def emit(self):
        nc = self.nc
        es = self.es
        import os as _os2
        for _i in range(int(_os2.environ.get("KDUMMYSEM", "0"))):
            es.enter_context(nc.semaphore("dummy%d" % _i))
        for e in COMPUTE:
            self.sems[e] = es.enter_context(nc.semaphore("s_" + e))
        for q in DMAQ:
            for i in range(NDMASEM):
                self.dsems[(q, i)] = es.enter_context(nc.semaphore("d_%s%d" % (q, i)))
        for e in COMPUTE:
            c = 0
            for op in self.ops[e]:
                if op.dma:
                    continue
                if op.sig:
                    c += 1
                    op.cnt = c
            assert c < 65000, (e, c)
        final_waits = list(self.out_dmas)
        block = es.enter_context(nc.Block())
        sched = self

        def run(ename, eng):
            waited = {e: 0 for e in COMPUTE}
            dwaited = {}
            for op in sched.ops[ename]:
                need = {}
                if sched.trace is not None and sched.trace[0] <= op.idx <= sched.trace[1]:
                    print("EMIT", op.idx, ename, "cnt", op.cnt, "sig", op.sig, "deps", sorted((d.eng, d.idx, d.cnt, d.dtgt) for d in op.deps))
                for d in op.deps:
                    if d.dma:
                        if dwaited.get(d.dsem, 0) < d.dtgt:
                            dwaited[d.dsem] = d.dtgt
                            eng.wait_ge(sched.dsems[d.dsem], d.dtgt)
                    else:
                        if d.cnt > need.get(d.eng, 0):
                            need[d.eng] = d.cnt
                for se, c in need.items():
                    if c > waited[se]:
                        waited[se] = c
                        eng.wait_ge(sched.sems[se], c)
                inst = op.fn(eng)
                if op.dma:
                    inst.then_inc(sched.dsems[op.dsem], 16)
                elif op.sig:
                    inst.then_inc(sched.sems[ename], 1)
            if ename == "sp":
                for d in final_waits:
                    eng.wait_ge(sched.dsems[d.dsem], d.dtgt)

        @block.tensor
        def _(eng):
            run("pe", eng)

        @block.scalar
        def _(eng):
            run("act", eng)

        @block.vector
        def _(eng):
            run("dve", eng)

        @block.gpsimd
        def _(eng):
            run("pool", eng)

        @block.sync
        def _(eng):
            run("sp", eng)

    def dma(self, out, in_, q="sp", is_out=False):
        op = self.add(q, lambda e: e.dma_start(out=out.ap, in_=in_.ap), reads=[in_], writes=[out], dma=True)
        if is_out:
            self.out_dmas.append(op)
        return op

    def mm(self, out, lhsT, rhs, start=True, stop=True):
        return self.add("pe", lambda e: e.matmul(out.ap, lhsT.ap, rhs.ap, start=start, stop=stop),
                        reads=[lhsT, rhs], writes=[out])

    def transpose(self, out, in_, ident):
        return self.add("pe", lambda e: e.transpose(out.ap, in_.ap, ident.ap), reads=[in_, ident], writes=[out])

    def act(self, out, in_, func, bias=None, scale=None, accum=None):
        reads = [in_]
        kw = {}
        if bias is not None:
            if isinstance(bias, V):
                reads.append(bias)
                kw["bias"] = bias.ap
            else:
                kw["bias"] = bias
        if scale is not None:
            if isinstance(scale, V):
                reads.append(scale)
                kw["scale"] = scale.ap
            else:
                kw["scale"] = scale
        writes = [out]
        if accum is not None:
            kw["accum_out"] = accum.ap
            writes.append(accum)
        return self.add("act", lambda e: e.activation(out.ap, in_.ap, func, **kw), reads=reads, writes=writes)

    def tt(self, out, a, b, op, eng="dve"):
        return self.add(eng, lambda e: e.tensor_tensor(out.ap, a.ap, b.ap, op), reads=[a, b], writes=[out])

    def ts(self, out, a, s1, s2, op0, op1=None, eng="dve"):
        reads = [a]
        a1 = s1.ap if isinstance(s1, V) else s1
        a2 = s2.ap if isinstance(s2, V) else s2
        if isinstance(s1, V):
            reads.append(s1)
        if isinstance(s2, V):
            reads.append(s2)
        if op1 is None:
            return self.add(eng, lambda e: e.tensor_scalar(out.ap, a.ap, a1, None, op0), reads=reads, writes=[out])
        return self.add(eng, lambda e: e.tensor_scalar(out.ap, a.ap, a1, a2, op0, op1), reads=reads, writes=[out])

    def stt(self, out, a, s, b, op0, op1):
        reads = [a, b]
        a1 = s.ap if isinstance(s, V) else s
        if isinstance(s, V):
            reads.append(s)
        return self.add("dve", lambda e: e.scalar_tensor_tensor(out.ap, a.ap, a1, b.ap, op0, op1),
                        reads=reads, writes=[out])

    def copy(self, out, in_, eng="dve"):
        if eng == "act":
            return self.add("act", lambda e: e.copy(out.ap, in_.ap), reads=[in_], writes=[out])
        return self.add(eng, lambda e: e.tensor_copy(out.ap, in_.ap), reads=[in_], writes=[out])

    def memset(self, out, val, eng="pool"):
        return self.add(eng, lambda e: e.memset(out.ap, val), reads=[], writes=[out])

    def scan(self, out, d0, d1, init):
        reads = [d0, d1]
        i = init.ap if isinstance(init, V) else init
        if isinstance(init, V):
            reads.append(init)
        return self.add("dve", lambda e: e.tensor_tensor_scan(out.ap, d0.ap, d1.ap, i, ALU.mult, ALU.add),
                        reads=reads, writes=[out])

    def recip(self, out, in_):
        return self.add("dve", lambda e: e.reciprocal(out.ap, in_.ap), reads=[in_], writes=[out])

    def reduce(self, out, in_, op):
        return self.add("dve", lambda e: e.tensor_reduce(out.ap, in_.ap, AX.X, op), reads=[in_], writes=[out])

    def max8(self, out, in_):
        return self.add("dve", lambda e: e.max(out.ap, in_.ap), reads=[in_], writes=[out])


class Region:
    def __init__(self, arena_ap, start, end, cached=False):
        self.ap = arena_ap
        self.cached = cached
        self.cache = {}
        self.start = start
        self.end = end
        self.off = start

    def reset(self):
        if not self.cached:
            self.off = self.start

    def alloc(self, name, free_shape, dtype):
        if self.cached and name in self.cache:
            return self.cache[name]
        v = self._alloc(name, free_shape, dtype)
        if self.cached:
            self.cache[name] = v
        return v

    def _alloc(self, name, free_shape, dtype):
        n = 1
        for s in free_shape:
            n *= s
        esz = 4 if dtype == F32 else 2
        nb = (n * esz + 63) // 64 * 64
        assert self.off + nb <= self.end, ("sbuf region overflow", name, self.off, nb, self.end)
        a = self.ap[:, self.off // 4:(self.off + nb) // 4]
        self.off += nb
        if dtype != F32:
            a = a.bitcast(dtype)
        a = a[:, 0:n]
        if len(free_shape) == 2:
            a = a.rearrange("p (a b) -> p a b", a=free_shape[0])
        elif len(free_shape) == 3:
            a = a.rearrange("p (a b c) -> p a b c", a=free_shape[0], b=free_shape[1])
        return V(a, [Buf(name)])


def rms_rstd(S, R, ss, n, width, tag):
    r = R.alloc("rstd_" + tag, [width], F32)
    S.act(r, ss, AF.Sqrt, bias=EPS, scale=1.0 / n)
    S.recip(r, r)
    return r


def build(SEQ, NB, depth=2, debug=False, plan="ABSCD", nlayers=None):
    T = LCTX + SEQ
    NT = T // 128
    NLT = SEQ // 128
    nc = bass.Bass("TRN2", target_bir_lowering=False)
    es = ExitStack()
    with es:
        S = Sched(nc, es)
        import os as _os
        if _os.environ.get("KTRACE"):
            S.trace = tuple(int(v) for v in _os.environ["KTRACE"].split(","))
        if _os.environ.get("KLIMIT"):
            S.limit = int(_os.environ["KLIMIT"])
        okind = "ExternalOutput" if debug else "Internal"
        x_d = S.dram("x", [NB, SEQ, D], F32, "ExternalInput")
        ctx_d = S.dram("ctx", [NB, LCTX, D], F32, "ExternalInput")
        cT_d = S.dram("cT", [128, KD, 3], F32, "ExternalInput")
        w_ada_d = S.dram("w_ada", [depth, D, 6 * D], F32, "ExternalInput")
        b_adaT_d = S.dram("b_adaT", [depth, 128, 48], F32, "ExternalInput")
        b_ada_d = S.dram("b_ada", [depth, 6 * D], F32, "ExternalInput")
        g1T_d = S.dram("g1T", [depth, 128, KD], F32, "ExternalInput")
        g2T_d = S.dram("g2T", [depth, 128, KD], F32, "ExternalInput")
        w_in_d = S.dram("w_in_p", [depth, D, 1952], F32, "ExternalInput")
        g_cqT_d = S.dram("g_cqT", [depth, 128, 2], F32, "ExternalInput")
        g_ckvT_d = S.dram("g_ckvT", [depth, 128, 1], F32, "ExternalInput")
        w_uq_d = S.dram("w_uq_p", [depth, 256, 384], F32, "ExternalInput")
        w_ukv_d = S.dram("w_ukv_p", [depth, 128, 512], F32, "ExternalInput")
        sink_d = S.dram("sink", [depth, 4], F32, "ExternalInput")
        conv_wT_d = S.dram("conv_wT", [depth, 128, 4, 4], F32, "ExternalInput")
        conv_bT_d = S.dram("conv_bT", [depth, 128, 4], F32, "ExternalInput")
        lru_wa_d = S.dram("lru_wa", [depth, 2, 8, 64, 64], F32, "ExternalInput")
        lru_wx_d = S.dram("lru_wx", [depth, 2, 8, 64, 64], F32, "ExternalInput")
        lru_vT_d = S.dram("lru_vT", [depth, 128, 3, 2, 4], F32, "ExternalInput")
        g_grpT_d = S.dram("g_grpT", [depth, 128, KD], F32, "ExternalInput")
        w_out_d = S.dram("w_out", [depth, D, D], F32, "ExternalInput")
        wr_d = S.dram("wr", [depth, D, 36], F32, "ExternalInput")
        rb_d = S.dram("rb", [depth, 36], F32, "ExternalInput")
        weg_d = S.dram("w_e_gate", [depth, NEXP, D, DEXP], F32, "ExternalInput")
        weu_d = S.dram("w_e_up", [depth, NEXP, D, DEXP], F32, "ExternalInput")
        wed_d = S.dram("w_e_down", [depth, NEXP, DEXP, D], F32, "ExternalInput")
        gfin_d = S.dram("g_final", [D], F32, "ExternalInput")
        ident_d = S.dram("ident", [128, 128], F32, "ExternalInput")
        masks_d = S.dram("masks", [128, 2, 128], F32, "ExternalInput")
        rope_d = S.dram("rope", [NLT + 1, 128, 96], F32, "ExternalInput")
        out_d = S.dram("out", [NB, SEQ, D], F32, "ExternalOutput")
        GATES = S.dram("s_gates", [depth, 3, 2, D], F32, okind)
        XS = S.dram("s_xs", [NB, T, D], F32, okind)
        X1S = S.dram("s_x1s", [NB, T, D], F32, okind)
        ZL = S.dram("s_zl", [NB, 1024, T], F32, okind)
        QT = S.dram("s_qt", [NB, 96, 4, T], BF16, okind)
        KT = S.dram("s_kt", [NB, 96, 4, T], BF16, okind)
        VA = S.dram("s_va", [NB, T, 260], BF16, okind)
        SQKT = S.dram("s_sqkt", [NB, 64, 6, T], BF16, okind)
        SVA = S.dram("s_sva", [NB, T, 130], BF16, okind)
        OMIX = S.dram("s_omix", [NB, T, 512], F32, okind)
        OLRU = S.dram("s_olru", [NB, 512, T], BF16, okind)

        ARENA_BYTES = 190 * 1024
        arena_t = es.enter_context(nc.sbuf_tensor("arena", [128, ARENA_BYTES // 4], F32))
        arena = arena_t[:]
        PERS_BYTES = 12 * 1024
        P = Region(arena, 0, PERS_BYTES)
        R = Region(arena, PERS_BYTES, ARENA_BYTES)
        psum = []
        for i in range(4):
            t = es.enter_context(nc.psum_tensor("ps%d" % i, [128, 1024], F32))
            a = t[:]
            b0, b1 = Buf("ps%da" % i, excl=True), Buf("ps%db" % i, excl=True)
            psum.append((V(a, [b0, b1]), V(a[:, 0:512], [b0]), V(a[:, 512:1024], [b1])))
        PB = []
        for pr in psum:
            PB.append(pr[1])
            PB.append(pr[2])

        def pbf(bank):
            return V(bank.ap.bitcast(BF16), bank.bufs)

        identF = P.alloc("identF", [128], F32)
        identB = P.alloc("identB", [128], BF16)
        maskB = P.alloc("maskB", [2, 128], BF16)
        ones1 = P.alloc("ones1", [1], F32)
        modT = P.alloc("modT", [depth, 48, 3], F32)
        AB = P.alloc("AB", [depth * 3, 4, KD], F32)
        sT = P.alloc("sT", [KD, 3], F32)
        S.dma(identF, ident_d)
        S.copy(identB, identF, eng="dve")
        S.dma(sT, cT_d)
        S.memset(ones1, 1.0, eng="dve")
        R.reset()
        mtmp = R.alloc("mtmp", [2, 128], F32)
        S.dma(mtmp, masks_d)
        S.copy(maskB, mtmp, eng="dve")
        S.act(sT, sT, AF.Silu)
        wab = [R.alloc("wab%d" % i, [KD, 512], F32) for i in range(2)]
        brow = [R.alloc("brow%d" % i, [512], F32) for i in range(2)]
        grow = [R.alloc("grow%d" % i, [512], F32) for i in range(2)]
        badaT = R.alloc("badaT", [depth, 48], F32)
        gT = R.alloc("gT", [depth, 2, KD], F32)
        for l in range(depth):
            S.dma(badaT[:, l, :], b_adaT_d[l])
            S.dma(gT[:, l, 0, :], g1T_d[l])
            S.dma(gT[:, l, 1, :], g2T_d[l])
        it = 0
        for l in range(depth):
            for j in range(12):
                w = wab[it % 2]
                S.dma(w, w_ada_d[l][:, j * 512:(j + 1) * 512].re("(k p) n -> p k n", p=128))
                vec = j // 2
                if vec in (2, 5):
                    br = brow[it % 2]
                    S.dma(br[0:3, :], V(b_ada_d.ap[l, j * 512:(j + 1) * 512].partition_broadcast(3), []))
                    pm = PB[it % 2]
                    for k in range(KD):
                        S.mm(pm[0:3, :], sT[:, k, :], w[:, k, :], start=(k == 0), stop=(k == KD - 1))
                    gr = grow[it % 2]
                    S.tt(gr[0:3, :], pm[0:3, :], br[0:3, :], ALU.add)
                    S.dma(GATES[l, :, 0 if vec == 2 else 1, (j % 2) * 512:(j % 2 + 1) * 512], gr[0:3, :])
                else:
                    for m in range(4):
                        pm = PB[2 + (m % 2)]
                        for k in range(KD):
                            S.mm(pm[:, 0:3], w[:, k, m * 128:(m + 1) * 128], sT[:, k, :],
                                 start=(k == 0), stop=(k == KD - 1))
                        S.act(modT[:, l, j * 4 + m, :], pm[:, 0:3], AF.Identity,
                              bias=badaT[:, l, j * 4 + m:j * 4 + m + 1], scale=1.0)
                it += 1
            for r in range(3):
                ab = AB[:, l * 3 + r]
                S.ts(ab[:, 0, :], modT[:, l, 8:16, r], 1.0, None, ALU.add)
                S.tt(ab[:, 0, :], ab[:, 0, :], gT[:, l, 0, :], ALU.mult)
                S.copy(ab[:, 1, :], modT[:, l, 0:8, r])
                S.ts(ab[:, 2, :], modT[:, l, 32:40, r], 1.0, None, ALU.add)
                S.tt(ab[:, 2, :], ab[:, 2, :], gT[:, l, 1, :], ALU.mult)
                S.copy(ab[:, 3, :], modT[:, l, 24:32, r])
        S.barrier()
        if debug:
            print("ops after prologue", S.nadd)

        def x_src(b, l, t):
            if l == 0:
                if t < 2:
                    return ctx_d[b, t * 128:(t + 1) * 128, :]
                return x_d[b, (t - 2) * 128:(t - 1) * 128, :]
            return XS[b, t * 128:(t + 1) * 128, :]

        def norm_transpose(xt, A, Bc, hT_dst, tmpR, junk, pbanks, hTf_dst=None):
            ss = tmpR.alloc("ss", [1], F32)
            S.act(junk, xt, AF.Square, accum=ss)
            rstd = rms_rstd(S, tmpR, ss, D, 1, "x")
            xn = tmpR.alloc("xn", [D], F32)
            S.ts(xn, xt, rstd, None, ALU.mult)
            for half in range(2):
                pb = pbanks[half]
                for kk in range(4):
                    k = half * 4 + kk
                    S.transpose(pb[:, kk * 128:(kk + 1) * 128], xn[:, k * 128:(k + 1) * 128], identF)
                for kk in range(4):
                    k = half * 4 + kk
                    S.act(hT_dst[:, k, :], pb[:, kk * 128:(kk + 1) * 128], AF.Identity,
                          bias=Bc[:, k:k + 1], scale=A[:, k:k + 1])
                    if hTf_dst is not None:
                        S.ts(hTf_dst[:, k, :], pb[:, kk * 128:(kk + 1) * 128], A[:, k:k + 1], Bc[:, k:k + 1],
                             ALU.mult, ALU.add)

        def phase_A(b, l):
            R.reset()
            rowl, rowc = l * 3 + b, l * 3 + 2
            w_in = R.alloc("w_in", [KD, 1952], BF16)
            for k in range(KD):
                S.dma(w_in[:, k, :], w_in_d[l][k * 128:(k + 1) * 128, :], q="pool")
            wq_f = R.alloc("wq_f", [2, 384], F32)
            wkv_f = R.alloc("wkv_f", [512], F32)
            gq = R.alloc("gq", [3], F32)
            S.dma(wq_f, w_uq_d[l].re("(j p) n -> p j n", p=128))
            S.dma(wkv_f, w_ukv_d[l])
            S.dma(gq[:, 0:2], g_cqT_d[l])
            S.dma(gq[:, 2:3], g_ckvT_d[l])
            wq = R.alloc("wq", [2, 384], BF16)
            wkv = R.alloc("wkv", [512], BF16)
            for j in range(2):
                S.ts(wq[:, j, :], wq_f[:, j, :], gq[:, j:j + 1], None, ALU.mult)
            S.ts(wkv, wkv_f, gq[:, 2:3], None, ALU.mult)
            NS = 4
            xt = [R.alloc("xt%d" % i, [D], F32) for i in range(NS)]
            rp = [R.alloc("rp%d" % i, [96], F32) for i in range(NS)]
            hTs = [R.alloc("hTs%d" % i, [KD, 512], BF16) for i in range(2)]
            qa = [R.alloc("qa%d" % i, [4, 96], BF16) for i in range(NS)]
            ka = [R.alloc("ka%d" % i, [4, 96], BF16) for i in range(NS)]
            va = [R.alloc("va%d" % i, [4, 65], BF16) for i in range(NS)]
            sqk = [R.alloc("sqk%d" % i, [6, 64], BF16) for i in range(NS)]
            sva = [R.alloc("sva%d" % i, [2, 65], BF16) for i in range(NS)]
            qTt = [R.alloc("qTt%d" % i, [4, 128], BF16) for i in range(NS)]
            kTt = [R.alloc("kTt%d" % i, [4, 128], BF16) for i in range(NS)]
            sqkT = [R.alloc("sqkT%d" % i, [6, 128], BF16) for i in range(NS)]
            zlt = [R.alloc("zlt%d" % i, [512], F32) for i in range(2)]
            for i in range(NS):
                S.memset(va[i][:, :, 64:65], 1.0)
                S.memset(sva[i][:, :, 64:65], 1.0)
            TSZ = 15 * 1024
            TRs = [Region(arena, R.off + i * TSZ, R.off + (i + 1) * TSZ, cached=True) for i in range(NS)]
            assert R.off + NS * TSZ <= ARENA_BYTES, ("phase A sbuf", R.off + NS * TSZ)
            groups = [[0, 1]] + [list(range(2 + 4 * g, 2 + 4 * g + 4)) for g in range(NLT // 4)]
            loaded = set()

            def load_tile(t, slot, what="xr"):
                for w_ in what:
                    if (t, w_) in loaded:
                        continue
                    loaded.add((t, w_))
                    if w_ == "x":
                        S.dma(xt[slot], x_src(b, l, t))
                    else:
                        S.dma(rp[slot], rope_d[NLT if t < 2 else t - 2])

            for ti, t in enumerate(groups[0]):
                load_tile(t, ti)
            if len(groups) > 1:
                for ti, t in enumerate(groups[1]):
                    if ti >= len(groups[0]):
                        load_tile(t, ti)

            def a_tile(gi, ti, t, hT):
                TR = TRs[ti]
                bA, bB = PB[2 * ti], PB[2 * ti + 1]
                is_ctx = t < 2
                A = AB[:, rowc if is_ctx else rowl]
                x_t, rpt = xt[ti], rp[ti]
                hTt = hT[:, :, ti * 128:(ti + 1) * 128]
                junk = TR.alloc("junk", [D], BF16)
                ss = TR.alloc("ss", [1], F32)
                S.act(junk, x_t, AF.Square, accum=ss)
                yield
                rstd = TR.alloc("rstd_x", [1], F32)
                S.act(rstd, ss, AF.Sqrt, bias=EPS, scale=1.0 / D)
                yield
                S.recip(rstd, rstd)
                yield
                xn = TR.alloc("xn", [D], F32)
                S.ts(xn, x_t, rstd, None, ALU.mult)
                if gi + 1 < len(groups) and ti < len(groups[gi + 1]):
                    load_tile(groups[gi + 1][ti], ti, "x")
                yield
                for half, pb in enumerate((bA, bB)):
                    for kk in range(4):
                        k = half * 4 + kk
                        S.transpose(pb[:, kk * 128:(kk + 1) * 128], xn[:, k * 128:(k + 1) * 128], identF)
                yield
                for half, pb in enumerate((bA, bB)):
                    for kk in range(4):
                        k = half * 4 + kk
                        if half == 0:
                            S.act(hTt[:, k, :], pb[:, kk * 128:(kk + 1) * 128], AF.Identity,
                                  bias=A[:, 1, k:k + 1], scale=A[:, 0, k:k + 1])
                        else:
                            S.ts(hTt[:, k, :], pb[:, kk * 128:(kk + 1) * 128], A[:, 0, k:k + 1], A[:, 1, k:k + 1],
                                 ALU.mult, ALU.add)
                yield
                for k in range(KD):
                    S.mm(bA[:, 0:416], hTt[:, k, :], w_in[:, k, 0:416], start=(k == 0), stop=(k == KD - 1))
                for k in range(KD):
                    S.mm(bB[:, 0:512], hTt[:, k, :], w_in[:, k, 416:928], start=(k == 0), stop=(k == KD - 1))
                yield
                ss2 = TR.alloc("ss2", [2], F32)
                S.act(junk[:, 0:256], bA[:, 0:256], AF.Square, accum=ss2[:, 0:1])
                S.act(junk[:, 256:384], bA[:, 256:384], AF.Square, accum=ss2[:, 1:2])
                krr = TR.alloc("krr", [32], F32)
                S.act(krr, bA[:, 384:416], AF.Identity)
                z1s = TR.alloc("z1s", [512], F32)
                S.copy(z1s, bB[:, 0:512], eng="dve")
                yield
                rs2 = TR.alloc("rs2", [2], F32)
                S.act(rs2[:, 0:1], ss2[:, 0:1], AF.Sqrt, bias=EPS, scale=1.0 / 256)
                S.act(rs2[:, 1:2], ss2[:, 1:2], AF.Sqrt, bias=EPS, scale=1.0 / 128)
                yield
                S.recip(rs2, rs2)
                yield
                cn = TR.alloc("cn", [384], BF16)
                S.act(cn[:, 0:256], bA[:, 0:256], AF.Identity, scale=rs2[:, 0:1])
                S.act(cn[:, 256:384], bA[:, 256:384], AF.Identity, scale=rs2[:, 1:2])
                q_a, k_a, v_a, sqk_a, sv_a = qa[ti], ka[ti], va[ti], sqk[ti], sva[ti]
                Cm, Sm = rpt[:, 0:16], rpt[:, 16:32]
                Cs, Ss = rpt[:, 32:64], rpt[:, 64:96]
                z1h = z1s[:, 0:384].re("p (h d) -> p h d", h=6)
                S.copy(sv_a[:, :, 0:64], z1s[:, 384:512].re("p (h d) -> p h d", h=2), eng="act")
                Cs6, Ss6 = Cs.bcast(1, [128, 6, 32]), Ss.bcast(1, [128, 6, 32])
                s1 = TR.alloc("s1", [6, 32], F32)
                s2 = TR.alloc("s2", [6, 32], F32)
                s3 = TR.alloc("s3", [6, 32], F32)
                s4 = TR.alloc("s4", [6, 32], F32)
                S.tt(s1, z1h[:, :, 0:32], Cs6, ALU.mult)
                S.tt(s2, z1h[:, :, 32:64], Ss6, ALU.mult)
                S.tt(s3, z1h[:, :, 32:64], Cs6, ALU.mult)
                S.tt(s4, z1h[:, :, 0:32], Ss6, ALU.mult)
                k1 = TR.alloc("k1", [16], F32)
                k2 = TR.alloc("k2", [16], F32)
                k3 = TR.alloc("k3", [16], F32)
                k4 = TR.alloc("k4", [16], F32)
                S.tt(k1, krr[:, 0:16], Cm, ALU.mult, eng="pool")
                S.tt(k2, krr[:, 16:32], Sm, ALU.mult, eng="pool")
                S.tt(k3, krr[:, 16:32], Cm, ALU.mult)
                S.tt(k4, krr[:, 0:16], Sm, ALU.mult)
                yield
                pT = pbf(bA)
                for j in range(3):
                    S.transpose(pT[:, j * 128:(j + 1) * 128], cn[:, j * 128:(j + 1) * 128], identB)
                S.tt(sqk_a[:, :, 0:32], s1, s2, ALU.subtract, eng="pool")
                S.tt(sqk_a[:, :, 32:64], s3, s4, ALU.add)
                kr = TR.alloc("kr", [32], F32)
                S.tt(kr[:, 0:16], k1, k2, ALU.subtract, eng="pool")
                S.tt(kr[:, 16:32], k3, k4, ALU.add)
                yield
                cT = TR.alloc("cTt", [3, 128], BF16)
                S.copy(cT, pT[:, 0:384].re("p (j t) -> p j t", j=3), eng="dve")
                S.copy(k_a[:, :, 64:96], kr.bcast(1, [128, 4, 32]), eng="dve")
                yield
                pq, pkv = bA, bB
                for j in range(2):
                    S.mm(pq[:, 0:384], cT[:, j, :], wq[:, j, :], start=(j == 0), stop=(j == 1))
                S.mm(pkv[:, 0:512], cT[:, 2, :], wkv, start=True, stop=True)
                yield
                pq3 = pq[:, 0:384].re("p (h d) -> p h d", h=4)
                S.copy(q_a[:, :, 0:64], pq3[:, :, 0:64], eng="act")
                qr = TR.alloc("qr", [4, 32], F32)
                S.copy(qr, pq3[:, :, 64:96], eng="act")
                S.copy(k_a[:, :, 0:64], pkv[:, 0:256].re("p (h d) -> p h d", h=4), eng="dve")
                S.copy(v_a[:, :, 0:64], pkv[:, 256:512].re("p (h d) -> p h d", h=4), eng="dve")
                yield
                Cm4, Sm4 = Cm.bcast(1, [128, 4, 16]), Sm.bcast(1, [128, 4, 16])
                q1 = TR.alloc("q1", [4, 16], F32)
                q2 = TR.alloc("q2", [4, 16], F32)
                q3 = TR.alloc("q3", [4, 16], F32)
                q4 = TR.alloc("q4", [4, 16], F32)
                S.tt(q1, qr[:, :, 0:16], Cm4, ALU.mult)
                S.tt(q2, qr[:, :, 16:32], Sm4, ALU.mult)
                S.tt(q3, qr[:, :, 16:32], Cm4, ALU.mult)
                S.tt(q4, qr[:, :, 0:16], Sm4, ALU.mult)
                pTk = pbf(bB)
                for h in range(4):
                    S.transpose(pTk[0:96, h * 128:(h + 1) * 128], k_a[:, h, :], identB)
                yield
                S.tt(q_a[:, :, 64:80], q1, q2, ALU.subtract, eng="pool")
                S.tt(q_a[:, :, 80:96], q3, q4, ALU.add)
                kT_t = kTt[ti]
                S.copy(kT_t[0:96], pTk[0:96, 0:512].re("p (h t) -> p h t", h=4), eng="act")
                yield
                pTq = pbf(bA)
                for h in range(4):
                    S.transpose(pTq[0:96, h * 128:(h + 1) * 128], q_a[:, h, :], identB)
                pTs = pbf(bB)
                for h in range(6):
                    S.transpose(pTs[0:64, h * 128:(h + 1) * 128], sqk_a[:, h, :], identB)
                yield
                qT_t = qTt[ti]
                S.copy(qT_t[0:96], pTq[0:96, 0:512].re("p (h t) -> p h t", h=4), eng="dve")
                sT_t = sqkT[ti]
                S.copy(sT_t[0:64], pTs[0:64, 0:768].re("p (h t) -> p h t", h=6), eng="act")
                tok = slice(t * 128, (t + 1) * 128)
                S.dma(KT[b, :, :, tok], kT_t[0:96])
                S.dma(VA[b, tok, :], v_a.re("p h d -> p (h d)"))
                S.dma(SVA[b, tok, :], sv_a.re("p h d -> p (h d)"))
                yield
                S.dma(QT[b, :, :, tok], qT_t[0:96])
                S.dma(SQKT[b, :, :, tok], sT_t[0:64])
                if gi + 1 < len(groups) and ti < len(groups[gi + 1]):
                    load_tile(groups[gi + 1][ti], ti, "r")

            for gi, grp in enumerate(groups):
                hT = hTs[gi % 2]
                active = [a_tile(gi, ti, t, hT) for ti, t in enumerate(grp)]
                while active:
                    for g_ in list(active):
                        try:
                            next(g_)
                        except StopIteration:
                            active.remove(g_)
                ntok = 128 * len(grp)
                tok0 = grp[0] * 128
                for m in range(8):
                    pz = PB[m]
                    for k in range(KD):
                        S.mm(pz[:, 0:ntok], w_in[:, k, 928 + m * 128:928 + (m + 1) * 128], hT[:, k, 0:ntok],
                             start=(k == 0), stop=(k == KD - 1))
                    zt = zlt[m % 2]
                    S.copy(zt[:, 0:ntok], pz[:, 0:ntok], eng=("act" if m % 2 else "dve"))
                    S.dma(ZL[b, m * 128:(m + 1) * 128, tok0:tok0 + ntok], zt[:, 0:ntok])
            S.barrier()

        def phase_B(b, l):
            R.reset()
            kT = R.alloc("kT", [4, T], BF16)
            vA = R.alloc("vA", [NT, 260], BF16)
            for h in range(4):
                S.dma(kT[0:96, h, :], KT[b, :, h, :])
            for c0 in range(0, NT, 8):
                c1 = min(NT, c0 + 8)
                S.dma(vA[:, c0:c1, :], VA[b, c0 * 128:c1 * 128, :].re("(c p) n -> p c n", p=128))
            qTb = [R.alloc("qTb%d" % i, [4, 512], BF16) for i in range(2)]
            pt = [R.alloc("pt%d" % i, [512], BF16) for i in range(4)]
            usb = [R.alloc("usb%d" % i, [512], F32) for i in range(2)]
            ot = [R.alloc("ot%d" % i, [4, 64], F32) for i in range(2)]
            rc = [R.alloc("rc%d" % i, [4], F32) for i in range(2)]
            chunks = []
            if l < depth - 1:
                chunks.append((0, 256, 2))
            for c in range(SEQ // 512):
                chunks.append((256 + c * 512, 512, NT))
            Sb = [PB[0], PB[1], PB[2], PB[3]]
            Ub = [PB[4], PB[5]]
            Tb = [PB[6], PB[7]]
            S.dma(qTb[0][0:96, :, 0:chunks[0][1]], QT[b, :, :, chunks[0][0]:chunks[0][0] + chunks[0][1]])
            cnt = 0
            ui = 0
            for ci, (q0, nq, nkc) in enumerate(chunks):
                qt = qTb[ci % 2]
                if ci + 1 < len(chunks):
                    nq0, nnq, _ = chunks[ci + 1]
                    S.dma(qTb[(ci + 1) % 2][0:96, :, 0:nnq], QT[b, :, :, nq0:nq0 + nnq])
                for h in range(4):
                    U = Ub[ui % 2]
                    def score(c):
                        sb = Sb[(cnt + c) % 4]
                        S.mm(sb[:, 0:nq], kT[0:96, h, c * 128:(c + 1) * 128], qt[0:96, h, 0:nq])
                        p = pt[(cnt + c) % 4]
                        S.act(p[:, 0:nq], sb[:, 0:nq], AF.Exp, scale=MLA_SCALE)
                        return p
                    ps = {0: score(0)}
                    if nkc > 1:
                        ps[1] = score(1)
                    for c in range(nkc):
                        if c + 2 < nkc:
                            ps[c + 2] = score(c + 2)
                        S.mm(U[0:65, 0:nq], vA[:, c, h * 65:(h + 1) * 65], ps[c][:, 0:nq],
                             start=(c == 0), stop=(c == nkc - 1))
                        del ps[c]
                    cnt += nkc
                    us = usb[ui % 2]
                    S.copy(us[0:65, 0:nq], U[0:65, 0:nq], eng="dve")
                    tb = Tb[ui % 2]
                    nsub = nq // 128
                    for s in range(nsub):
                        S.transpose(tb[:, s * 65:(s + 1) * 65], us[0:65, s * 128:(s + 1) * 128], identF[0:65, 0:65])
                    t3 = tb[:, 0:nsub * 65].re("p (s d) -> p s d", s=nsub)
                    r = rc[ui % 2]
                    S.recip(r[:, 0:nsub], t3[:, :, 64])
                    o = ot[ui % 2]
                    S.tt(o[:, 0:nsub, :], t3[:, :, 0:64], r[:, 0:nsub].bcast(2, [128, nsub, 64]), ALU.mult)
                    S.dma(OMIX[b, q0:q0 + nq, h * 64:(h + 1) * 64].re("(s p) d -> p s d", p=128), o[:, 0:nsub, :])
                    ui += 1
            S.barrier()

        def phase_B2(b, l):
            R.reset()
            sT_ = R.alloc("sqkT_all", [6, T], BF16)
            svA = R.alloc("svA", [NT, 130], BF16)
            for h in range(6):
                S.dma(sT_[0:64, h, :], SQKT[b, :, h, :])
            for c0 in range(0, NT, 8):
                c1 = min(NT, c0 + 8)
                S.dma(svA[:, c0:c1, :], SVA[b, c0 * 128:c1 * 128, :].re("(c p) n -> p c n", p=128))
            esink = R.alloc("esink", [4], F32)
            S.dma(esink, V(sink_d.ap[l].partition_broadcast(128), []))
            S.act(esink, esink, AF.Exp)
            pt = [R.alloc("spt%d" % i, [2, 128], BF16) for i in range(4)]
            usb = [R.alloc("susb%d" % i, [256], F32) for i in range(2)]
            ot = [R.alloc("sot%d" % i, [2, 64], F32) for i in range(2)]
            rc = [R.alloc("src%d" % i, [2], F32) for i in range(2)]
            Sb = [PB[0], PB[1], PB[2], PB[3]]
            Ub = [PB[4], PB[5]]
            Tb = [PB[6], PB[7]]
            qtiles = list(range(2, NT)) if l == depth - 1 else list(range(NT))
            cnt = 0
            ui = 0
            for tq in qtiles:
                if tq < 2:
                    keys = [(0, None), (1, None)]
                else:
                    keys = [(0, None), (1, None)]
                    if tq - 1 >= 2:
                        keys.append((tq - 1, 0))
                    keys.append((tq, None))
                    if tq + 1 < NT:
                        keys.append((tq + 1, 1))
                for g in range(2):
                    U = Ub[ui % 2]
                    q = sT_[0:64, 2 * g:2 * g + 2, tq * 128:(tq + 1) * 128]
                    plist = []
                    for (kc, mk) in keys:
                        sb = Sb[cnt % 4]
                        S.mm(sb[:, 0:256].re("p (h t) -> p h t", h=2), sT_[0:64, 4 + g, kc * 128:(kc + 1) * 128], q)
                        p = pt[cnt % 4]
                        S.act(p, sb[:, 0:256].re("p (h t) -> p h t", h=2), AF.Exp, scale=SWA_SCALE)
                        if mk is not None:
                            S.tt(p, p, maskB[:, mk, :].bcast(1, [128, 2, 128]), ALU.mult)
                        plist.append(p)
                        cnt += 1
                        if len(plist) >= 2:
                            idx = len(plist) - 2
                            kc2 = keys[idx][0]
                            S.mm(U[0:65, 0:256], svA[:, kc2, g * 65:(g + 1) * 65], plist[idx].re("p h t -> p (h t)"),
                                 start=(idx == 0), stop=False)
                    idx = len(plist) - 1
                    S.mm(U[0:65, 0:256], svA[:, keys[idx][0], g * 65:(g + 1) * 65], plist[idx].re("p h t -> p (h t)"),
                         start=(idx == 0), stop=True)
                    us = usb[ui % 2]
                    S.copy(us[0:65, :], U[0:65, 0:256], eng="dve")
                    tb = Tb[ui % 2]
                    for s in range(2):
                        S.transpose(tb[:, s * 65:(s + 1) * 65], us[0:65, s * 128:(s + 1) * 128], identF[0:65, 0:65])
                    t3 = tb[:, 0:130].re("p (s d) -> p s d", s=2)
                    r = rc[ui % 2]
                    S.tt(r, t3[:, :, 64], esink[:, 2 * g:2 * g + 2], ALU.add)
                    S.recip(r, r)
                    o = ot[ui % 2]
                    S.tt(o, t3[:, :, 0:64], r.bcast(2, [128, 2, 64]), ALU.mult)
                    S.dma(OMIX[b, tq * 128:(tq + 1) * 128, 256 + g * 128:256 + (g + 1) * 128], o.re("p s d -> p (s d)"))
                    ui += 1
            S.barrier()

        def phase_C(b, l):
            R.reset()
            ZW = T + 8
            CO, LO = 2, 261
            wst = R.alloc("wst", [4, 4, 128], F32)
            S.memset(wst, 0.0, eng="dve")
            for ty, wd_ in enumerate((lru_wa_d, lru_wx_d)):
                for d in range(2):
                    for half in range(2):
                        src = wd_[l, d].re("(m two) c e -> two c m e", two=2)[half]
                        S.dma(wst[half * 64:(half + 1) * 64, ty * 2 + d, :, half * 64:(half + 1) * 64], src)
            wbd = R.alloc("wbd", [4, 4, 128], BF16)
            S.copy(wbd, wst, eng="dve")
            vT = R.alloc("vT", [3, 2, 4], F32)
            S.dma(vT, lru_vT_d[l])
            cw = R.alloc("cw", [4, 4], F32)
            cb = R.alloc("cb", [4], F32)
            S.dma(cw, conv_wT_d[l])
            S.dma(cb, conv_bT_d[l])
            cneg = R.alloc("cneg", [2, 4], F32)
            S.act(cneg, vT[:, 2], AF.Exp, scale=-1.0)
            S.act(cneg, cneg, AF.Ln, bias=1.0, scale=1.0)
            S.ts(cneg, cneg, -8.0, None, ALU.mult)
            cnh = R.alloc("cnh", [2, 4], F32)
            S.ts(cnh, cneg, 0.5, None, ALU.mult)
            zb = R.alloc("zb", [ZW], F32)
            zbd = zb.sub(Buf("zbdata"))
            S.memset(V(zb.ap, zb.bufs + zbd.bufs), 0.0, eng="pool")
            u = R.alloc("u", [T], F32)
            ub = R.alloc("ub", [T], BF16)
            rr = R.alloc("rr", [T], F32)
            ii = R.alloc("ii", [T], F32)
            tq_ = R.alloc("tq", [T], F32)
            hf = R.alloc("hf", [T], F32)
            hb = R.alloc("hb", [T], F32)
            gz = R.alloc("gz", [T], F32)
            ob = R.alloc("ob", [T], BF16)
            tchunks = [(0, 256)] + [(256 + c * 512, 512) for c in range(SEQ // 512)]
            for m in range(4):
                S.dma(zbd[:, CO:CO + 256], ZL[b, m * 128:(m + 1) * 128, 0:256])
                S.dma(zbd[:, LO:LO + SEQ], ZL[b, m * 128:(m + 1) * 128, 256:T])
                S.dma(gz, ZL[b, 512 + m * 128:512 + (m + 1) * 128, :])
                zr = V(zb.ap, zb.bufs + zbd.bufs)
                for (o0, o1, n) in ((0, 0, 256), (256, 259, SEQ)):
                    S.ts(u[:, o0:o0 + n], zr[:, o1:o1 + n], cw[:, m, 0:1], cb[:, m:m + 1], ALU.mult, ALU.add)
                    for tap in range(1, 4):
                        S.stt(u[:, o0:o0 + n], zr[:, o1 + tap:o1 + tap + n], cw[:, m, tap:tap + 1], u[:, o0:o0 + n],
                              ALU.mult, ALU.add)
                S.copy(ub, u, eng="act")
                S.act(gz, gz, AF.Gelu_apprx_tanh)
                for d in range(2):
                    for ci, (t0, n) in enumerate(tchunks):
                        pa, px = PB[(ci % 2) * 2], PB[(ci % 2) * 2 + 1]
                        S.mm(pa[:, 0:n], wbd[:, 0 * 2 + d, m, :], ub[:, t0:t0 + n])
                        S.mm(px[:, 0:n], wbd[:, 1 * 2 + d, m, :], ub[:, t0:t0 + n])
                        S.act(rr[:, t0:t0 + n], pa[:, 0:n], AF.Sigmoid, bias=vT[:, 0, d, m:m + 1], scale=1.0)
                        S.act(ii[:, t0:t0 + n], px[:, 0:n], AF.Sigmoid, bias=vT[:, 1, d, m:m + 1], scale=1.0)
                    S.act(tq_, rr, AF.Tanh, scale=cnh[:, d, m:m + 1])
                    S.act(rr, rr, AF.Exp, scale=cneg[:, d, m:m + 1])
                    S.act(tq_, tq_, AF.Sqrt, scale=-1.0)
                    S.stt(tq_, rr, 1.0, tq_, ALU.add, ALU.mult)
                    S.tt(ii, ii, u, ALU.mult)
                    S.tt(ii, ii, tq_, ALU.mult)
                    if d == 0:
                        S.scan(hf, rr, ii, 0.0)
                    else:
                        S.scan(hb[:, 0:256][:, ::-1], rr[:, 0:256][:, ::-1], ii[:, 0:256][:, ::-1], 0.0)
                        S.scan(hb[:, 256:T][:, ::-1], rr[:, 256:T][:, ::-1], ii[:, 256:T][:, ::-1], hb[:, 0:1])
                S.tt(hf, hf, hb, ALU.add)
                S.tt(ob, hf, gz, ALU.mult)
                S.dma(OLRU[b, m * 128:(m + 1) * 128, :], ob)
            S.barrier()

        def phase_DE(b, l, tiles):
            R.reset()
            last = l == depth - 1
            ntl = len(tiles)
            ntok = ntl * 128
            H2T = R.alloc("H2T", [KD, ntok], BF16)
            COMB = R.alloc("COMB", [ntl, 32], F32)
            mark = R.off
            wo_f = R.alloc("wo_f", [KD, D], F32)
            S.dma(wo_f, w_out_d[l].re("(k p) n -> p k n", p=128))
            gg = R.alloc("gg", [KD], F32)
            S.dma(gg, g_grpT_d[l])
            wo = R.alloc("wo", [KD, D], BF16)
            for k in range(KD):
                if k % 2:
                    S.act(wo[:, k, :], wo_f[:, k, :], AF.Identity, scale=gg[:, k:k + 1])
                else:
                    S.ts(wo[:, k, :], wo_f[:, k, :], gg[:, k:k + 1], None, ALU.mult)
            wrt = R.alloc("wrt", [KD, 36], F32)
            S.dma(wrt, wr_d[l].re("(k p) n -> p k n", p=128))
            rbt = R.alloc("rbt", [36], F32)
            S.dma(rbt, V(rb_d.ap[l].partition_broadcast(128), []))
            G1 = [R.alloc("G1_%d" % i, [D], F32) for i in range(2)]
            S.dma(G1[0], V(GATES.ap[l, b, 0].partition_broadcast(128), []))
            S.dma(G1[1], V(GATES.ap[l, 2, 0].partition_broadcast(128), []))
            NBUF = 4
            xt = [R.alloc("dxt%d" % i, [D], F32) for i in range(NBUF)]
            om = [R.alloc("om%d" % i, [512], F32) for i in range(NBUF)]
            ol = [R.alloc("ol%d" % i, [4, 128], BF16) for i in range(NBUF)]
            LG = R.alloc("LG", [ntl, 36], F32)
            rowl, rowc = l * 3 + b, l * 3 + 2
            TRs = [Region(arena, R.off + pp * 24576, R.off + (pp + 1) * 24576, cached=True) for pp in range(2)]
            RB = Region(arena, R.off + 2 * 24576, ARENA_BYTES, cached=True)

            def loads(i):
                t = tiles[i]
                tok = slice(t * 128, (t + 1) * 128)
                S.dma(xt[i % NBUF], x_src(b, l, t))
                S.dma(om[i % NBUF], OMIX[b, tok, :])
                S.dma(ol[i % NBUF], OLRU[b, :, tok].re("(m p) t -> p m t", p=128))

            def d_tile(i, t):
                pp = i % 2
                TR = TRs[pp]
                Q = PB[4 * pp:4 * pp + 4]
                PA, PL = psum[2 * pp], psum[2 * pp + 1]
                is_ctx = t < 2
                tok = slice(t * 128, (t + 1) * 128)
                if i + 2 < ntl:
                    loads(i + 2)
                x_t, o_m, o_l = xt[i % NBUF], om[i % NBUF], ol[i % NBUF]
                junk = TR.alloc("djunk", [D], BF16)
                ss2 = TR.alloc("dss2", [2], F32)
                S.act(junk[:, 0:256], o_m[:, 0:256], AF.Square, accum=ss2[:, 0:1])
                S.act(junk[:, 256:512], o_m[:, 256:512], AF.Square, accum=ss2[:, 1:2])
                sq = TR.alloc("sq", [4, 128], F32)
                S.act(sq, o_l, AF.Square)
                yield
                rs2 = TR.alloc("rstd_g", [2], F32)
                S.act(rs2, ss2, AF.Sqrt, bias=EPS, scale=1.0 / 256)
                pss = Q[1]
                for m in range(4):
                    S.mm(pss[:, 0:1], sq[:, m, :], ones1, start=(m == 0), stop=(m == 3))
                yield
                S.recip(rs2, rs2)
                rsl = TR.alloc("rsl", [1], F32)
                S.act(rsl, pss[:, 0:1], AF.Sqrt, bias=EPS, scale=1.0 / 512)
                yield
                on = TR.alloc("on", [512], BF16)
                S.act(on[:, 0:256], o_m[:, 0:256], AF.Identity, scale=rs2[:, 0:1])
                S.act(on[:, 256:512], o_m[:, 256:512], AF.Identity, scale=rs2[:, 1:2])
                S.recip(rsl, rsl)
                yield
                pT = pbf(Q[0])
                for j in range(4):
                    S.transpose(pT[:, j * 128:(j + 1) * 128], on[:, j * 128:(j + 1) * 128], identB)
                yield
                mT = TR.alloc("mT", [4, 128], BF16)
                S.copy(mT, pT[:, 0:512].re("p (j t) -> p j t", j=4), eng="dve")
                yield
                for nh in range(2):
                    for m in range(4):
                        S.mm(PL[nh + 1], o_l[:, m, :], wo[:, 4 + m, nh * 512:(nh + 1) * 512], start=(m == 0), stop=(m == 3))
                for nh in range(2):
                    for j in range(4):
                        S.mm(PA[nh + 1], mT[:, j, :], wo[:, j, nh * 512:(nh + 1) * 512], start=(j == 0), stop=(j == 3))
                yield
                tA = TR.alloc("tA", [D], F32)
                S.copy(tA, PA[0], eng="act")
                yield
                S.stt(tA, PL[0], rsl, tA, ALU.mult, ALU.add)
                yield
                S.tt(tA, tA, G1[1 if is_ctx else 0], ALU.mult)
                yield
                S.tt(x_t, x_t, tA, ALU.add)
                yield
                S.dma(X1S[b, tok, :], x_t)
                ss = TR.alloc("ss", [1], F32)
                S.act(junk, x_t, AF.Square, accum=ss)
                yield
                rstd = TR.alloc("rstd_x", [1], F32)
                S.act(rstd, ss, AF.Sqrt, bias=EPS, scale=1.0 / D)
                yield
                S.recip(rstd, rstd)
                yield
                xn = TR.alloc("xn", [D], F32)
                S.act(xn, x_t, AF.Identity, scale=rstd)
                yield
                A = AB[:, rowc if is_ctx else rowl]
                h2f = TR.alloc("h2f", [KD, 128], F32)
                for half in range(2):
                    pb = Q[half]
                    for kk in range(4):
                        k = half * 4 + kk
                        S.transpose(pb[:, kk * 128:(kk + 1) * 128], xn[:, k * 128:(k + 1) * 128], identF)
                yield
                for half in range(2):
                    pb = Q[half]
                    for kk in range(4):
                        k = half * 4 + kk
                        S.act(h2f[:, k, :], pb[:, kk * 128:(kk + 1) * 128], AF.Identity,
                              bias=A[:, 3, k:k + 1], scale=A[:, 2, k:k + 1])
                yield
                S.copy(H2T[:, :, i * 128:(i + 1) * 128], h2f, eng="dve")
                pr = Q[2]
                for k in range(KD):
                    S.mm(pr[:, 0:36], h2f[:, k, :], wrt[:, k, :], start=(k == 0), stop=(k == KD - 1))
                yield
                S.tt(LG[:, i, :], pr[:, 0:36], rbt, ALU.add)

            for i in range(min(2, ntl)):
                loads(i)
            active = []
            nxt = 0
            while nxt < ntl or active:
                while len(active) < 2 and nxt < ntl:
                    active.append(d_tile(nxt, tiles[nxt]))
                    nxt += 1
                for g_ in list(active):
                    try:
                        next(g_)
                    except StopIteration:
                        active.remove(g_)
            lgG = LG[:, :, 0:4]
            lgE = LG[:, :, 4:36].re("p t (g e) -> p t g e", g=4)
            gmax = RB.alloc("gmax", [ntl], F32)
            S.reduce(gmax, lgG, ALU.max)
            mg = RB.alloc("mg", [ntl, 4], F32)
            S.tt(mg, lgG, gmax.bcast(2, [128, ntl, 4]), ALU.is_equal)
            eg = RB.alloc("eg", [ntl, 4], F32)
            S.tt(eg, lgG, gmax.bcast(2, [128, ntl, 4]), ALU.subtract)
            S.act(eg, eg, AF.Exp)
            pgt = RB.alloc("pgt", [ntl], F32)
            S.reduce(pgt, eg, ALU.add)
            S.recip(pgt, pgt)
            les = RB.alloc("les", [ntl, 8], F32)
            tmp8 = RB.alloc("tmp8", [ntl, 8], F32)
            S.tt(les, lgE[:, :, 0, :], mg[:, :, 0].bcast(2, [128, ntl, 8]), ALU.mult)
            for g in range(1, 4):
                S.tt(tmp8, lgE[:, :, g, :], mg[:, :, g].bcast(2, [128, ntl, 8]), ALU.mult)
                S.tt(les, les, tmp8, ALU.add)
            m1v = RB.alloc("m1v", [ntl], F32)
            S.reduce(m1v, les, ALU.max)
            k1 = RB.alloc("k1", [ntl, 8], F32)
            S.tt(k1, les, m1v.bcast(2, [128, ntl, 8]), ALU.is_equal)
            les2 = RB.alloc("les2", [ntl, 8], F32)
            S.stt(les2, k1, -1e30, les, ALU.mult, ALU.add)
            m2v = RB.alloc("m2v", [ntl], F32)
            S.reduce(m2v, les2, ALU.max)
            k2 = RB.alloc("k2", [ntl, 8], F32)
            S.tt(k2, les2, m2v.bcast(2, [128, ntl, 8]), ALU.is_equal)
            e2 = RB.alloc("e2", [ntl], F32)
            S.tt(e2, m2v, m1v, ALU.subtract)
            S.act(e2, e2, AF.Exp)
            w1 = RB.alloc("w1", [ntl], F32)
            S.ts(w1, e2, 1.0, None, ALU.add)
            S.recip(w1, w1)
            S.tt(w1, w1, pgt, ALU.mult)
            w2 = RB.alloc("w2", [ntl], F32)
            S.tt(w2, w1, e2, ALU.mult)
            cwt = RB.alloc("cwt", [ntl, 8], F32)
            S.tt(cwt, k1, w1.bcast(2, [128, ntl, 8]), ALU.mult)
            S.tt(tmp8, k2, w2.bcast(2, [128, ntl, 8]), ALU.mult)
            S.tt(cwt, cwt, tmp8, ALU.add)
            for g in range(4):
                S.tt(COMB[:, :, g * 8:(g + 1) * 8], cwt, mg[:, :, g].bcast(2, [128, ntl, 8]), ALU.mult)
            S.barrier()
            R.off = mark
            Y = R.alloc("Y", [ntl, D], F32)
            wg = [R.alloc("wg%d" % i, [KD, DEXP], BF16) for i in range(2)]
            wu = [R.alloc("wu%d" % i, [KD, DEXP], BF16) for i in range(2)]
            wd = [R.alloc("wd%d" % i, [2, D], BF16) for i in range(2)]
            sgl = [R.alloc("sgl%d" % i, [512], F32) for i in range(2)]
            aT = [R.alloc("aT%d" % i, [2, 512], BF16) for i in range(2)]
            Yb = [Y[:, i, :].sub(Buf("Y%d" % i)) for i in range(ntl)]

            def wload(e):
                S.dma(wg[e % 2], weg_d[l, e].re("(k p) n -> p k n", p=128), q="pool")
                S.dma(wu[e % 2], weu_d[l, e].re("(k p) n -> p k n", p=128), q="pool")
                S.dma(wd[e % 2], wed_d[l, e].re("(k p) n -> p k n", p=128), q="pool")
            wload(0)
            chunks = []
            c0 = 0
            while c0 < ntok:
                n = min(512, ntok - c0)
                chunks.append((c0, n))
                c0 += n
            it = 0
            for e in range(NEXP):
                if e + 1 < NEXP:
                    wload(e + 1)
                g_w, u_w, d_w = wg[e % 2], wu[e % 2], wd[e % 2]
                for (c0, n) in chunks:
                    a_t = aT[it % 2]
                    for half in range(2):
                        pg_, pu_ = PB[half * 2], PB[half * 2 + 1]
                        for k in range(KD):
                            S.mm(pg_[:, 0:n], g_w[:, k, half * 128:(half + 1) * 128], H2T[:, k, c0:c0 + n],
                                 start=(k == 0), stop=(k == KD - 1))
                        for k in range(KD):
                            S.mm(pu_[:, 0:n], u_w[:, k, half * 128:(half + 1) * 128], H2T[:, k, c0:c0 + n],
                                 start=(k == 0), stop=(k == KD - 1))
                        sg_ = sgl[half]
                        S.act(sg_[:, 0:n], pg_[:, 0:n], AF.Silu)
                        S.tt(a_t[:, half, 0:n], sg_[:, 0:n], pu_[:, 0:n], ALU.mult)
                    for s in range(n // 128):
                        ti = c0 // 128 + s
                        pd = psum[2 + (ti % 2)]
                        for nh in range(2):
                            for half in range(2):
                                S.mm(pd[nh + 1], a_t[:, half, s * 128:(s + 1) * 128], d_w[:, half, nh * 512:(nh + 1) * 512],
                                     start=(half == 0), stop=(half == 1))
                        if e == 0:
                            S.ts(Yb[ti], pd[0], COMB[:, ti, e:e + 1], None, ALU.mult)
                        else:
                            S.stt(Yb[ti], pd[0], COMB[:, ti, e:e + 1], Yb[ti], ALU.mult, ALU.add)
                    it += 1
            G2 = [R.alloc("G2_%d" % i, [D], F32) for i in range(2)]
            S.dma(G2[0], V(GATES.ap[l, b, 1].partition_broadcast(128), []))
            S.dma(G2[1], V(GATES.ap[l, 2, 1].partition_broadcast(128), []))
            x1 = [R.alloc("x1_%d" % i, [D], F32) for i in range(2)]
            ejunk = R.alloc("ejunk", [D], BF16)
            if last:
                GF = R.alloc("GF", [D], F32)
                S.dma(GF, V(gfin_d.ap.partition_broadcast(128), []))
            TR = Region(arena, R.off, ARENA_BYTES, cached=True)
            S.dma(x1[0], X1S[b, tiles[0] * 128:(tiles[0] + 1) * 128, :])
            for i, t in enumerate(tiles):
                TR.reset()
                tok = slice(t * 128, (t + 1) * 128)
                if i + 1 < ntl:
                    S.dma(x1[(i + 1) % 2], X1S[b, tiles[i + 1] * 128:(tiles[i + 1] + 1) * 128, :])
                x_t = x1[i % 2]
                S.tt(Yb[i], Yb[i], G2[1 if t < 2 else 0], ALU.mult)
                S.tt(x_t, x_t, Yb[i], ALU.add)
                if not last:
                    S.dma(XS[b, tok, :], x_t)
                else:
                    ss = TR.alloc("ess", [1], F32)
                    S.act(ejunk, x_t, AF.Square, accum=ss)
                    rstd = rms_rstd(S, TR, ss, D, 1, "f")
                    S.stt(x_t, x_t, rstd, GF, ALU.mult, ALU.mult)
                    S.dma(out_d[b, (t - 2) * 128:(t - 1) * 128, :], x_t, is_out=True)
            S.barrier()

        for b in range(NB):
            for l in range(depth if nlayers is None else nlayers):
                if "A" in plan:
                    phase_A(b, l)
                if "B" in plan:
                    phase_B(b, l)
                if "S" in plan:
                    phase_B2(b, l)
                if "C" in plan:
                    phase_C(b, l)
                if "D" not in plan:
                    continue
                if l == depth - 1:
                    tl = list(range(2, NT))
                else:
                    tl = list(range(NT))
                nblk = (len(tl) + 16) // 17
                per = (len(tl) + nblk - 1) // nblk
                for i in range(nblk):
                    blk = tl[i * per:(i + 1) * per]
                    if blk:
                        phase_DE(b, l, blk)
        if debug:
            print("total ops", S.nadd)
        S.emit()
    return nc


def _perm_rot(nheads, hd, base=0):
    q = hd // 4
    idx = []
    for h in range(nheads):
        o = base + h * hd
        idx += list(range(o, o + q)) + list(range(o + 2 * q, o + 3 * q)) + list(range(o + q, o + 2 * q)) + \
            list(range(o + 3 * q, o + 4 * q))
    return idx


def _chan_major(v, nch):
    return np.ascontiguousarray(np.swapaxes(v.reshape(v.shape[:-1] + (nch, 128)), -1, -2))


def host_shared(inp, SEQ):
    f = np.float32
    depth = inp["w_ada"].shape[0]
    sh = {}
    sh["w_ada"] = np.ascontiguousarray(inp["w_ada"], f)
    sh["b_ada"] = np.ascontiguousarray(inp["b_ada"], f)
    sh["b_adaT"] = _chan_major(inp["b_ada"].astype(f), 48)
    sh["g1T"] = _chan_major(inp["g_norm1"].astype(f), 8)
    sh["g2T"] = _chan_major(inp["g_norm2"].astype(f), 8)
    cols = list(range(0, 384)) + _perm_rot(1, 32, 384) + _perm_rot(4, 64, 416) + _perm_rot(2, 64, 672) + \
        list(range(800, 1952))
    sh["w_in_p"] = np.ascontiguousarray(inp["w_in"][:, :, cols], f)
    sh["g_cqT"] = _chan_major(inp["g_cq"].astype(f), 2)
    sh["g_ckvT"] = _chan_major(inp["g_ckv"].astype(f), 1)
    qcols = []
    for h in range(4):
        qcols += list(range(h * 96, h * 96 + 64)) + _perm_rot(1, 32, h * 96 + 64)
    sh["w_uq_p"] = np.ascontiguousarray(inp["w_uq"][:, :, qcols], f)
    kvcols = [h * 128 + i for h in range(4) for i in range(64)] + [h * 128 + 64 + i for h in range(4) for i in range(64)]
    sh["w_ukv_p"] = np.ascontiguousarray(inp["w_ukv"][:, :, kvcols], f)
    sh["sink"] = np.ascontiguousarray(inp["swa_sink"], f)
    cw = inp["conv_w"].astype(f)
    sh["conv_wT"] = np.ascontiguousarray(np.transpose(cw.reshape(depth, 4, 4, 128), (0, 3, 2, 1)))
    sh["conv_bT"] = _chan_major(inp["conv_b"].astype(f), 4)
    sh["lru_wa"] = np.ascontiguousarray(inp["lru_wa"], f)
    sh["lru_wx"] = np.ascontiguousarray(inp["lru_wx"], f)
    v = np.stack([inp["lru_ba"], inp["lru_bx"], inp["lru_lam"]], axis=1).astype(f)
    sh["lru_vT"] = np.ascontiguousarray(np.transpose(v.reshape(depth, 3, 2, 4, 128), (0, 4, 1, 2, 3)))
    sh["g_grpT"] = _chan_major(inp["g_grp"].astype(f), 8)
    sh["w_out"] = np.ascontiguousarray(inp["w_out"], f)
    wg2 = np.transpose(inp["w_g2"], (0, 2, 1, 3)).reshape(depth, D, 32)
    sh["wr"] = np.ascontiguousarray(np.concatenate([inp["w_g1"], wg2], axis=-1), f)
    sh["rb"] = np.ascontiguousarray(np.concatenate([inp["b_g1"], inp["b_g2"].reshape(depth, 32)], axis=-1), f)
    sh["w_e_gate"] = np.ascontiguousarray(inp["w_e_gate"], f)
    sh["w_e_up"] = np.ascontiguousarray(inp["w_e_up"], f)
    sh["w_e_down"] = np.ascontiguousarray(inp["w_e_down"], f)
    sh["g_final"] = np.ascontiguousarray(inp["g_final"], f)
    sh["ident"] = np.eye(128, dtype=f)
    j = np.arange(128)[:, None]
    i = np.arange(128)[None, :]
    sh["masks"] = np.ascontiguousarray(np.stack([(j >= i), (j <= i)], axis=1).astype(f))
    pos = np.arange(SEQ)
    rows = (pos // 64).astype(np.float32)
    colsp = (pos % 64).astype(np.float32)

    def tab(half):
        fr = (np.float32(10000.0) ** (-np.arange(half, dtype=np.float32) / np.float32(half))).astype(np.float32)
        ar = rows[:, None] * fr[None, :]
        ac = colsp[:, None] * fr[None, :]
        C = np.concatenate([np.cos(ar), np.cos(ac)], axis=1)
        Sn = np.concatenate([np.sin(ar), np.sin(ac)], axis=1)
        return C.astype(f), Sn.astype(f)
    Cm, Sm = tab(8)
    Cs, Ss = tab(16)
    tabs = np.concatenate([Cm, Sm, Cs, Ss], axis=1).reshape(SEQ // 128, 128, 96)
    ident_tab = np.concatenate([np.ones((128, 16), f), np.zeros((128, 16), f), np.ones((128, 32), f), np.zeros((128, 32), f)], axis=1)
    sh["rope"] = np.ascontiguousarray(np.concatenate([tabs, ident_tab[None]], axis=0))
    return sh


def host_core(inp, b0, NB):
    f = np.float32
    cv = np.stack([inp["c"][b0], inp["c"][min(b0 + 1, inp["c"].shape[0] - 1)], inp["c_ctx"]], axis=0).astype(f)
    cT = np.ascontiguousarray(np.transpose(cv.reshape(3, KD, 128), (2, 1, 0)))
    return {
        "x": np.ascontiguousarray(inp["x"][b0:b0 + NB], f),
        "ctx": np.ascontiguousarray(inp["ctx"][b0:b0 + NB], f),
        "cT": cT,
    }


_NC_CACHE = {}


def kernel(**inputs):
    inp = {k: np.asarray(v) for k, v in inputs.items()}
    B, SEQ, _ = inp["x"].shape
    ncores = 8
    NB = B // ncores
    key = (SEQ, NB)
    if key not in _NC_CACHE:
        _NC_CACHE[key] = build(SEQ, NB)
    nc = _NC_CACHE[key]
    sh = host_shared(inp, SEQ)
    in_maps = []
    for c in range(ncores):
        m = dict(sh)
        m.update(host_core(inp, c * NB, NB))
        in_maps.append(m)
    res = run_bass_kernel_spmd(nc, in_maps, core_ids=list(range(ncores)))
    out = np.concatenate([np.asarray(r["out"]) for r in res.results], axis=0)
    return out.astype(np.float32)
```
